# Optimizing a Trainium2 kernel written in Bass

```python
import jax, jax.numpy as jnp
from jax import lax
import numpy as np

D_MODEL = 1024
BATCH = 16
SEQ = 2048
DEPTH = 1

CHUNK = 64
D_MIX = D_MODEL
CONV_WIDTH = D_MIX // 2
CONV_GROUPS = 8
CONV_K = 3
HGRN_WIDTH = D_MIX - CONV_WIDTH
HGRN_HEADS = 4
HGRN_DK = HGRN_WIDTH // HGRN_HEADS
HGRN_DV = HGRN_WIDTH // HGRN_HEADS
N_PROJ_SLOTS = 7
PROJ_WIDTH = 3 * CONV_WIDTH + 4 * HGRN_WIDTH
MEM_LEN = 256
XATTN_HEADS = 4
XATTN_HEAD_DIM = D_MODEL // XATTN_HEADS
N_GROUPS = 4
EXPERTS_PER_GROUP = 4
N_EXPERTS = N_GROUPS * EXPERTS_PER_GROUP
TOPK_IN_GROUP = 2
D_EXPERT = D_MODEL // 2
EPS = 1e-6

kernel_name = "hymba_conv_hgrn2_xattn_hiermoe_block"


def rms_norm(x, g):
    xf = x.astype(jnp.float32)
    y = xf * lax.rsqrt(jnp.mean(xf * xf, axis=-1, keepdims=True) + EPS)
    return (y * g.astype(jnp.float32)).astype(x.dtype)


def hgrn2_chunkwise(q, k, v, log_f):
    bsz, seq, heads, dk = q.shape
    dv = v.shape[-1]
    n_chunks = seq // CHUNK

    def to_chunks(t):
        return t.reshape(bsz, n_chunks, CHUNK, heads, t.shape[-1]).transpose(1, 0, 3, 2, 4)

    tri = jnp.tril(jnp.ones((CHUNK, CHUNK), dtype=bool))

    def step(state, inp):
        qc, kc, vc, ac = inp
        b = jnp.cumsum(ac, axis=2)
        diff = b[:, :, :, None, :] - b[:, :, None, :, :]
        decay = jnp.exp(jnp.where(tri[None, None, :, :, None], diff, -jnp.inf))
        scores = jnp.einsum('bhtsk,bhsk->bhts', qc[:, :, :, None, :] * decay, kc)
        o = (jnp.einsum('bhts,bhsv->bhtv', scores, vc)
             + jnp.einsum('bhtk,bhkv->bhtv', qc * jnp.exp(b), state))
        b_last = b[:, :, -1:, :]
        state = (jnp.exp(b_last[:, :, 0, :])[..., None] * state
                 + jnp.einsum('bhsk,bhsv->bhkv', kc * jnp.exp(b_last - b), vc))
        return state, o

    s0 = jnp.zeros((bsz, heads, dk, dv), jnp.float32)
    _, o = lax.scan(step, s0, (to_chunks(q), to_chunks(k), to_chunks(v), to_chunks(log_f)))
    return o.transpose(1, 0, 3, 2, 4).reshape(bsz, seq, heads, dv)


def parallel_mixer(h, w_in, conv_w, lb, hgrn_norm, w_out):
    bsz, seq, _ = h.shape
    proj = jnp.einsum('bsd,dp->bsp', h, w_in)
    cb, cc, cx, hq, hf, hi, hg = jnp.split(proj, N_PROJ_SLOTS, axis=-1)
    u = jnp.pad(cc * cx, ((0, 0), (CONV_K - 1, 0), (0, 0)))
    conv = sum(u[:, j:j + seq] * conv_w[j] for j in range(CONV_K))
    y_a = cb * conv
    z = hf.astype(jnp.float32).reshape(bsz, seq, HGRN_HEADS, HGRN_DK)
    lbh = lb.astype(jnp.float32).reshape(HGRN_HEADS, HGRN_DK)
    log_f = jnp.log(lbh + (1.0 - lbh) * jax.nn.sigmoid(z))
    k = (1.0 - lbh) * jax.nn.sigmoid(-z)
    q = hq.astype(jnp.float32).reshape(bsz, seq, HGRN_HEADS, HGRN_DK)
    v = hi.astype(jnp.float32).reshape(bsz, seq, HGRN_HEADS, HGRN_DV)
    o = hgrn2_chunkwise(q, k, v, log_f)
    o = o * lax.rsqrt(jnp.mean(o * o, axis=-1, keepdims=True) + EPS)
    o = o * hgrn_norm.astype(jnp.float32).reshape(HGRN_HEADS, HGRN_DV)
    g = hg.astype(jnp.float32).reshape(bsz, seq, HGRN_HEADS, HGRN_DV)
    y_b = (o * jax.nn.silu(g)).reshape(bsz, seq, HGRN_WIDTH).astype(h.dtype)
    y = jnp.concatenate([y_a, y_b], axis=-1)
    return jnp.einsum('bsm,md->bsd', y, w_out)


def memory_cross_attention(h, m, w_q, w_kv, w_o):
    bsz, seq, _ = h.shape
    q = jnp.einsum('bsd,de->bse', h, w_q).reshape(bsz, seq, XATTN_HEADS, XATTN_HEAD_DIM)
    kv = jnp.einsum('bmd,de->bme', m, w_kv)
    k, v = jnp.split(kv, 2, axis=-1)
    k = k.reshape(bsz, m.shape[1], XATTN_HEADS, XATTN_HEAD_DIM)
    v = v.reshape(bsz, m.shape[1], XATTN_HEADS, XATTN_HEAD_DIM)
    s = jnp.einsum('bshd,bmhd->bhsm', q, k).astype(jnp.float32) * (XATTN_HEAD_DIM ** -0.5)
    p = jax.nn.softmax(s, axis=-1).astype(v.dtype)
    o = jnp.einsum('bhsm,bmhd->bshd', p, v).reshape(bsz, seq, D_MODEL)
    return jnp.einsum('bse,ed->bsd', o, w_o)


def hierarchical_moe(h, w_group, b_group, w_expert, b_expert, w_gate, w_up, w_down):
    bsz, seq, d = h.shape
    t = h.reshape(-1, d)
    g_prob = jax.nn.softmax((t @ w_group + b_group).astype(jnp.float32), axis=-1)
    g_p, g_idx = lax.top_k(g_prob, 1)
    e_logits = (t @ w_expert + b_expert).astype(jnp.float32).reshape(-1, N_GROUPS, EXPERTS_PER_GROUP)
    e_logits = jnp.take_along_axis(e_logits, g_idx[:, :, None], axis=1)[:, 0]
    e_prob = jax.nn.softmax(e_logits, axis=-1)
    e_p, e_idx = lax.top_k(e_prob, TOPK_IN_GROUP)
    weights = g_p * e_p / jnp.sum(e_p, axis=-1, keepdims=True)
    expert_id = g_idx * EXPERTS_PER_GROUP + e_idx
    combine = jnp.sum(jax.nn.one_hot(expert_id, N_EXPERTS, dtype=jnp.float32) * weights[..., None], axis=1)
    out = jnp.zeros(t.shape, jnp.float32)
    for e in range(N_EXPERTS):
        hid = jax.nn.silu(t @ w_gate[e]) * (t @ w_up[e])
        out = out + combine[:, e:e + 1] * (hid @ w_down[e])
    return out.astype(h.dtype).reshape(bsz, seq, d)


def setup_inputs(seed: int = 0) -> dict:
    key = jax.random.key(seed)
    ks = jax.random.split(key, 24)
    f32 = jnp.float32

    def nrm(k, shape, scale):
        return jax.random.normal(k, shape, f32) * scale

    def gain(k, shape):
        return 1.0 + 0.02 * jax.random.normal(k, shape, f32)

    return {
        "x": jax.random.normal(ks[0], (BATCH, SEQ, D_MODEL), f32),
        "mem": jax.random.normal(ks[1], (BATCH, MEM_LEN, D_MODEL), f32),
        "mix_norm": gain(ks[2], (DEPTH, D_MODEL)),
        "w_in": nrm(ks[3], (DEPTH, D_MODEL, PROJ_WIDTH), D_MODEL ** -0.5),
        "conv_w": nrm(ks[4], (DEPTH, CONV_K, CONV_WIDTH), CONV_K ** -0.5),
        "hgrn_lb": jax.random.normal(ks[5], (DEPTH + 1, HGRN_WIDTH), f32),
        "hgrn_norm": gain(ks[6], (DEPTH, HGRN_WIDTH)),
        "w_out": nrm(ks[7], (DEPTH, D_MIX, D_MODEL), D_MIX ** -0.5),
        "xattn_norm": gain(ks[8], (DEPTH, D_MODEL)),
        "mem_norm": gain(ks[9], (DEPTH, D_MODEL)),
        "w_q": nrm(ks[10], (DEPTH, D_MODEL, D_MODEL), D_MODEL ** -0.5),
        "w_kv": nrm(ks[11], (DEPTH, D_MODEL, 2 * D_MODEL), D_MODEL ** -0.5),
        "w_o": nrm(ks[12], (DEPTH, D_MODEL, D_MODEL), D_MODEL ** -0.5),
        "ffn_norm": gain(ks[13], (DEPTH, D_MODEL)),
        "w_group": nrm(ks[14], (DEPTH, D_MODEL, N_GROUPS), D_MODEL ** -0.5),
        "b_group": nrm(ks[15], (DEPTH, N_GROUPS), 0.01),
        "w_expert": nrm(ks[16], (DEPTH, D_MODEL, N_EXPERTS), D_MODEL ** -0.5),
        "b_expert": nrm(ks[17], (DEPTH, N_EXPERTS), 0.01),
        "w_gate": nrm(ks[18], (DEPTH, N_EXPERTS, D_MODEL, D_EXPERT), D_MODEL ** -0.5),
        "w_up": nrm(ks[19], (DEPTH, N_EXPERTS, D_MODEL, D_EXPERT), D_MODEL ** -0.5),
        "w_down": nrm(ks[20], (DEPTH, N_EXPERTS, D_EXPERT, D_MODEL), D_EXPERT ** -0.5),
        "final_norm": gain(ks[21], (D_MODEL,)),
    }


def reference(x, mem, mix_norm, w_in, conv_w, hgrn_lb, hgrn_norm, w_out,
              xattn_norm, mem_norm, w_q, w_kv, w_o,
              ffn_norm, w_group, b_group, w_expert, b_expert, w_gate, w_up, w_down,
              final_norm):
    lb_all = jnp.cumsum(jax.nn.softmax(hgrn_lb.astype(jnp.float32), axis=0), axis=0)
    for l in range(DEPTH):
        x = x + parallel_mixer(rms_norm(x, mix_norm[l]), w_in[l], conv_w[l], lb_all[l],
                               hgrn_norm[l], w_out[l])
        x = x + memory_cross_attention(rms_norm(x, xattn_norm[l]), rms_norm(mem, mem_norm[l]),
                                       w_q[l], w_kv[l], w_o[l])
        x = x + hierarchical_moe(rms_norm(x, ffn_norm[l]), w_group[l], b_group[l], w_expert[l],
                                 b_expert[l], w_gate[l], w_up[l], w_down[l])
    return rms_norm(x, final_norm)
```

```python
import contextlib
import numpy as np
import concourse.bass as bass
import concourse.mybir as mybir
from concourse.bass_utils import run_bass_kernel_spmd

F32 = mybir.dt.float32
BF16 = mybir.dt.bfloat16
AF = mybir.ActivationFunctionType
ALU = mybir.AluOpType
AX = mybir.AxisListType

ENGS = ("pe", "act", "dve", "pool", "sp")
EPS = 1e-6
NCORES = 8
SEQ = 2048
D = 1024
NT = 16
NJ = 4
NEXP = 16


class Prog:
    NDMA_SEMS = 24

    def __init__(self, nc):
        self.nc = nc
        self.ops = []
        self.last_w = {}
        self.readers = {}
        self.dma_rr = [0, 0]
        self.dma_last = {}
        self.last_on = {}

    def alias(self, old_keys, new_key):
        s = set()
        for k in old_keys:
            w = self.last_w.get(k)
            if w is not None:
                s.add(w)
            s.update(self.readers.get(k, ()))
        self.last_w[new_key] = None
        self.readers[new_key] = list(s)

    def op(self, eng, fn, reads=(), writes=(), dma=False, extra=()):
        oid = len(self.ops)
        deps = set(extra)
        for k in reads:
            w = self.last_w.get(k)
            if w is not None:
                deps.add(w)
        for k in writes:
            w = self.last_w.get(k)
            if w is not None:
                deps.add(w)
            for r in self.readers.get(k, ()):
                deps.add(r)
        deps.discard(oid)
        rec = dict(id=oid, eng=eng, fn=fn, deps=deps, dma=dma, sig=False, dsem=None)
        if dma:
            half = self.NDMA_SEMS // 2
            q = 0 if eng == "pool" else 1
            s = q * half + (self.dma_rr[q] % half)
            self.dma_rr[q] += 1
            rec["dsem"] = s
            prev = self.dma_last.get(s)
            if prev is not None:
                deps.add(prev)
            self.dma_last[s] = oid
        self.ops.append(rec)
        if fn is not None:
            self.last_on[eng] = oid
        for k in writes:
            self.last_w[k] = oid
            self.readers[k] = []
        for k in reads:
            if k not in writes:
                self.readers.setdefault(k, []).append(oid)
        return oid

    def barrier(self):
        tails = set(self.last_on.values()) | set(self.dma_last.values())
        for e in ENGS:
            self.op(e, None, extra=set(tails))
        self.last_w = {}
        self.readers = {}

    def emit(self):
        nc = self.nc
        ops = self.ops
        for o in ops:
            if o["eng"] == "pe" and not o["dma"]:
                o["deps"] = {d for d in o["deps"] if ops[d]["dma"] or ops[d]["eng"] != "pe"}
        for o in ops:
            for d in o["deps"]:
                ops[d]["sig"] = True
        cnt = {e: 0 for e in ENGS}
        dcnt = {}
        for o in ops:
            if o["dma"]:
                s = o["dsem"]
                dcnt[s] = dcnt.get(s, 0) + 16
                o["ev"] = ("d%d" % s, dcnt[s])
            elif o["sig"]:
                cnt[o["eng"]] += 1
                o["ev"] = (o["eng"], cnt[o["eng"]])
        with contextlib.ExitStack() as st:
            sems = {}
            for e in ENGS:
                sems[e] = st.enter_context(nc.semaphore("sem_" + e))
            for s in range(self.NDMA_SEMS):
                sems["d%d" % s] = st.enter_context(nc.semaphore("sem_d%d" % s))
            block = st.enter_context(nc.Block())
            per = {e: [o for o in ops if o["eng"] == e] for e in ENGS}

            def run(e, handle):
                waited = {}
                for o in per[e]:
                    need = {}
                    for d in o["deps"]:
                        sname, val = ops[d]["ev"]
                        if need.get(sname, 0) < val:
                            need[sname] = val
                    for sname, val in need.items():
                        if waited.get(sname, 0) >= val:
                            continue
                        handle.wait_ge(sems[sname], val)
                        waited[sname] = val
                    if o["fn"] is None:
                        continue
                    ins = o["fn"](handle)
                    if o["dma"]:
                        ins.then_inc(sems[o["ev"][0]], 16)
                    elif o["sig"]:
                        ins.then_inc(sems[e], 1)

            @block.tensor
            def _(h):
                run("pe", h)

            @block.scalar
            def _(h):
                run("act", h)

            @block.vector
            def _(h):
                run("dve", h)

            @block.gpsimd
            def _(h):
                run("pool", h)

            @block.sync
            def _(h):
                run("sp", h)
                for s, v in dcnt.items():
                    h.wait_ge(sems["d%d" % s], v)


PC_G = 0
PC_CONV = 32
PC_LB = 44
PC_HN = 52
PC_N = 56
CN_ID = 0
CN_ONE = 128
CN_MASK = 256
CN_RM = 320
CN_N = 832


def build_program(stop_after=None, nhalves=2):
    nc = bass.Bass("TRN2", target_bir_lowering=False)

    def dram(name, shape, kind="ExternalInput"):
        return nc.dram_tensor(name, shape, F32, kind=kind).ap()

    x_d = dram("x", [2 * SEQ, D])
    mem_d = dram("mem", [512, D])
    w_in_d = dram("w_in", [D, 3584])
    w_out_d = dram("w_out", [D, D])
    w_q_d = dram("w_q", [D, D])
    w_kv_d = dram("w_kv", [D, 2 * D])
    w_o_d = dram("w_o", [D, D])
    w_gate_d = dram("w_gate", [NEXP, D, 512])
    w_up_d = dram("w_up", [NEXP, D, 512])
    w_down_d = dram("w_down", [NEXP, 512, D])
    pcols_d = dram("pcols", [128, PC_N])
    wr_d = dram("wr", [128, 160])
    rows_d = dram("rows", [1, 1044])
    consts_d = dram("consts", [128, CN_N])
    out_d = dram("out", [2 * SEQ, D], kind="ExternalOutput")
    dbg_d = dram("dbg", [128, 8192], kind="ExternalOutput") if stop_after else None

    st = contextlib.ExitStack()
    with st:
        NW = 51 * 1024
        big = st.enter_context(nc.sbuf_tensor("big", [128, NW], F32))
        psf = [st.enter_context(nc.psum_tensor("psf%d" % i, [128, 512], F32)) for i in range(6)]
        psb = [st.enter_context(nc.psum_tensor("psb%d" % i, [128, 1024], BF16)) for i in range(2)]
        P = Prog(nc)

        top = [0]

        def carve(words, dtype=F32, pattern=None, **kw):
            off = top[0]
            top[0] += words
            assert top[0] <= NW, "SBUF arena overflow %d" % top[0]
            ap = big[:, off:off + words]
            if dtype == BF16:
                ap = ap.bitcast(BF16)
            if pattern:
                ap = ap.rearrange(pattern, **kw)
            return ap

        bank_state = {}
        rr = {"f": 0, "b": 0}

        held = set()

        class Bank:
            def __init__(self, kind, hold=False):
                n = 6 if kind == "f" else 2
                for _ in range(n):
                    self.idx = rr[kind] % n
                    rr[kind] += 1
                    if (kind, self.idx) not in held:
                        break
                else:
                    raise RuntimeError("all PSUM banks held")
                self.kind = kind
                self.gen = rr[kind]
                self.t = psf[self.idx] if kind == "f" else psb[self.idx]
                self.name = "%s%d" % (kind, self.idx)
                self.old = bank_state.get(self.name, [])
                self.keys = {}
                bank_state[self.name] = []
                if hold:
                    held.add((kind, self.idx))

            def done(self):
                held.discard((self.kind, self.idx))

            def k(self, sub=0):
                if sub not in self.keys:
                    key = "ps.%s.%d.%s" % (self.name, self.gen, sub)
                    P.alias(self.old, key)
                    self.keys[sub] = key
                    bank_state[self.name].append(key)
                return self.keys[sub]

        pc = carve(PC_N)
        cn = carve(CN_N)
        identb = carve(64, BF16)
        onesb = carve(64, BF16)
        maskb = carve(32, BF16)
        wr = carve(160, F32, "p (c n) -> p c n", c=8)
        wrg = carve(160, F32, "p (c n) -> p c n", c=8)
        rows = carve(1044)
        hp = carve(16)
        ident = cn[:, CN_ID:CN_ID + 128]
        rmask = cn[:, CN_RM:CN_RM + 512]
        base_top = top[0]

        P.op("sp", lambda h: h.dma_start(out=pc, in_=pcols_d), writes=["pc"], dma=True)
        P.op("sp", lambda h: h.dma_start(out=cn, in_=consts_d), writes=["cn"], dma=True)
        P.op("sp", lambda h: h.dma_start(out=wr.rearrange("p c n -> p (c n)"), in_=wr_d), writes=["wr"], dma=True)
        P.op("sp", lambda h: h.dma_start(out=rows, in_=rows_d.partition_broadcast(128)), writes=["rows"], dma=True)
        P.op("dve", lambda h: h.tensor_copy(out=identb, in_=cn[:, CN_ID:CN_ID + 128]), reads=["cn"], writes=["identb"])
        P.op("dve", lambda h: h.tensor_copy(out=onesb, in_=cn[:, CN_ONE:CN_ONE + 128]), reads=["cn"], writes=["onesb"])
        P.op("dve", lambda h: h.tensor_copy(out=maskb, in_=cn[:, CN_MASK:CN_MASK + 64]), reads=["cn"], writes=["maskb"])
        P.op("dve", lambda h: h.tensor_tensor(out=hp[:, 12:16], in0=pc[:, PC_LB + 4:PC_LB + 8], in1=pc[:, PC_LB:PC_LB + 4],
                                              op=ALU.subtract), reads=["pc"], writes=["hp_t"])
        P.op("act", lambda h: h.activation(out=hp[:, 12:16], in_=hp[:, 12:16], func=AF.Exp), reads=["hp_t"], writes=["hp_t"])
        P.op("dve", lambda h: h.tensor_scalar_add(out=hp[:, 12:16], in0=hp[:, 12:16], scalar1=1.0), reads=["hp_t"], writes=["hp_t"])
        P.op("dve", lambda h: h.reciprocal(out=hp[:, 0:4], in_=hp[:, 12:16]), reads=["hp_t"], writes=["hp_lb"])
        P.op("dve", lambda h: h.tensor_scalar(out=hp[:, 4:8], in0=hp[:, 0:4], scalar1=-1.0, scalar2=1.0, op0=ALU.mult, op1=ALU.add),
             reads=["hp_lb"], writes=["hp_oml"])
        P.op("act", lambda h: h.activation(out=hp[:, 8:12], in_=hp[:, 4:8], func=AF.Ln), reads=["hp_oml"], writes=["hp_ln"])
        for c in range(8):
            P.op("dve", lambda h, c=c: h.tensor_scalar(out=wrg[:, c, :], in0=wr[:, c, :], scalar1=pc[:, PC_G + 24 + c:PC_G + 25 + c],
                                                       scalar2=None, op0=ALU.mult), reads=["wr", "pc"], writes=["wrg"])

        def wview(w2d, c0, c1):
            return w2d.rearrange("(c p) n -> p c n", p=128)[:, :, c0:c1]

        def gbc(gi):
            return pc[:, PC_G + gi * 8:PC_G + gi * 8 + 8].unsqueeze(2).to_broadcast([128, 8, 128])

        def norm_tile(src, src_key, gi, dst3, dst_key, stat, stat_key, xn, xn_key, ti):
            ss = stat[:, 0:1]
            rs = stat[:, 1:2]
            P.op("act", lambda h: h.activation(out=xn, in_=src, func=AF.Square, accum_out=ss),
                 reads=[src_key], writes=[xn_key, stat_key])
            P.op("act", lambda h: h.activation(out=rs, in_=ss, func=AF.Ln, scale=1.0 / D, bias=eps_ap), reads=[stat_key, "eps"], writes=[stat_key])
            P.op("act", lambda h: h.activation(out=rs, in_=rs, func=AF.Exp, scale=-0.5), reads=[stat_key], writes=[stat_key])
            P.op("dve", lambda h: h.tensor_scalar(out=xn, in0=src, scalar1=rs, scalar2=None, op0=ALU.mult),
                 reads=[src_key, stat_key], writes=[xn_key])
            bk = Bank("b")
            for c in range(8):
                P.op("pe", lambda h, c=c: h.transpose(out=bk.t[:, c * 128:(c + 1) * 128], in_=xn[:, c * 128:(c + 1) * 128], identity=identb),
                     reads=[xn_key, "identb"], writes=[bk.k()])
            P.op("dve", lambda h: h.tensor_tensor(out=dst3[:, :, ti * 128:(ti + 1) * 128],
                                                  in0=bk.t[:, :].rearrange("p (c n) -> p c n", c=8), in1=gbc(gi), op=ALU.mult),
                 reads=[bk.k(), "pc"], writes=[dst_key])

        eps_ap = carve(1)
        P.op("dve", lambda h: h.memset(eps_ap, EPS), writes=["eps"])
        one_ap = carve(1)
        P.op("dve", lambda h: h.memset(one_ap, 1.0), writes=["one"])
        base_top = top[0]

        if stop_after == "S":
            P.barrier()
            P.op("sp", lambda h: h.dma_start(out=dbg_d[:, 0:16], in_=hp), dma=True)
            P.op("sp", lambda h: h.dma_start(out=dbg_d[:, 16:1060], in_=rows), dma=True)
            P.op("sp", lambda h: h.dma_start(out=dbg_d[:, 1060:1220], in_=wrg.rearrange("p c n -> p (c n)")), dma=True)
            nhalves = 0

        wrb = carve(80, BF16, "p (c n) -> p c n", c=8)
        P.op("dve", lambda h: h.tensor_copy(out=wrb, in_=wr), reads=["wr"], writes=["wrb"])
        base_top = top[0]

        def carve_at(off, words, dtype=F32, pattern=None, **kw):
            sv = top[0]
            top[0] = off
            ap = carve(words, dtype, pattern, **kw)
            top[0] = sv
            return ap

        def do_half(hf):
            top[0] = base_top
            bufA = carve(8192, BF16, "p (c n) -> p c n", c=8)
            bufY_off = top[0]
            bufY = carve(8192, BF16, "p (c n) -> p c n", c=8)
            xres_off = top[0]
            xres = carve(16384, F32, "p (i n) -> p i n", i=16)
            rstd3 = carve(32, F32, "p (i n) -> p i n", i=16)
            RT0 = top[0]
            tok0 = hf * SEQ

            def akey(i):
                return "A.%d" % i

            def ykey(c, j):
                return "Y.%d.%d" % (c, j)

            def xkey(i):
                return "X.%d" % i

            win = carve_at(xres_off, 14336, BF16, "p (c n) -> p c n", c=8)
            vtok = carve_at(xres_off + 14336, 2048, BF16, "p (c n) -> p c n", c=8)
            for s in (0, 1, 2, 5, 4, 3, 6):
                P.op("pool", lambda h, s=s: h.dma_start(out=win[:, :, s * 512:(s + 1) * 512], in_=wview(w_in_d, s * 512, (s + 1) * 512)),
                     writes=["win%d" % s], dma=True)

            top[0] = RT0
            xs = [carve(1024) for _ in range(3)]
            xn2 = [carve(512, BF16) for _ in range(2)]
            for i in range(NT):
                xk = "xs%d" % (i % 3)
                P.op("sp", lambda h, i=i: h.dma_start(out=xs[i % 3], in_=x_d[tok0 + i * 128:tok0 + (i + 1) * 128, :]),
                     writes=[xk], dma=True)
                norm_tile(xs[i % 3], xk, 0, bufA, akey(i), rstd3[:, i, :], "st.%d" % i, xn2[i % 2], "xn%d" % (i % 2), i)
            P.barrier()
            if stop_after == "A1":
                for c in range(8):
                    P.op("pool", lambda h, c=c: h.dma_start(out=dbg_d[:, c * 1024:(c + 1) * 1024], in_=bufA[:, c, 0:1024]), dma=True)
                return True

            top[0] = RT0
            wout = carve(4096, BF16, "p (c n) -> p c n", c=8)
            P.op("pool", lambda h: h.dma_start(out=wout, in_=wview(w_out_d, 0, D)), writes=["wout"], dma=True)
            Sst = carve(512, F32, "p (h n) -> p h n", h=4)
            ubuf = carve(4 * 514, F32, "p (c n) -> p c n", c=4)
            P.op("pool", lambda h: h.memset(Sst.rearrange("p h n -> p (h n)"), 0.0), writes=["S0", "S1", "S2", "S3"])
            P.op("pool", lambda h: h.memset(ubuf.rearrange("p c n -> p (c n)"), 0.0), writes=["u0", "u1", "u2", "u3"])
            f_t1 = carve(512)
            f_t2 = carve(512)
            DSb = [carve(64, BF16) for _ in range(8)]
            NSET = 2
            TS = []
            for q in range(NSET):
                TS.append(dict(
                    e=carve(512), l2=carve(512), l1=carve(512), lnk=carve(512), d=carve(512), dec=carve(8),
                    qt=carve(256, BF16), khT=carve(256, BF16),
                    khtok=carve(512, BF16, "p (c n) -> p c n", c=8), scm=carve(256, BF16, "p (c n) -> p c n", c=8)))

            def conv_chunk(j, cch):
                T0 = j * 512
                rdA = [akey(4 * j + t) for t in range(4)]
                th = []
                bks = [None, None, None]

                def mm(s):
                    def f():
                        bks[s] = Bank("f")
                        bk = bks[s]
                        for c in range(8):
                            P.op("pe", lambda h, c=c: h.matmul(
                                bk.t[:, :], lhsT=win[:, c, s * 512 + cch * 128:s * 512 + (cch + 1) * 128],
                                rhs=bufA[:, c, T0:T0 + 512], start=(c == 0), stop=(c == 7)),
                                reads=rdA + ["win%d" % s], writes=[bk.k()])
                    return f
                uk = "u%d" % cch
                cw0 = PC_CONV + cch * 3

                def ew1():
                    if j > 0:
                        P.op("pool", lambda h: h.tensor_copy(out=ubuf[:, cch, 0:2], in_=ubuf[:, cch, 512:514]), reads=[uk], writes=[uk])
                    P.op("act", lambda h: h.activation(out=ubuf[:, cch, 2:514], in_=bks[1].t[:, :], func=AF.Copy), reads=[bks[1].k()], writes=[uk])

                def ew2():
                    P.op("dve", lambda h: h.tensor_tensor(out=ubuf[:, cch, 2:514], in0=bks[2].t[:, :], in1=ubuf[:, cch, 2:514], op=ALU.mult),
                         reads=[bks[2].k(), uk], writes=[uk])
                    P.op("dve", lambda h: h.tensor_scalar(out=f_t1, in0=ubuf[:, cch, 2:514], scalar1=pc[:, cw0 + 2:cw0 + 3],
                                                          scalar2=None, op0=ALU.mult), reads=[uk, "pc"], writes=["f_t1"])
                    P.op("dve", lambda h: h.scalar_tensor_tensor(out=f_t2, in0=ubuf[:, cch, 1:513], scalar=pc[:, cw0 + 1:cw0 + 2],
                                                                 in1=f_t1, op0=ALU.mult, op1=ALU.add), reads=[uk, "pc", "f_t1"], writes=["f_t2"])
                    P.op("dve", lambda h: h.scalar_tensor_tensor(out=f_t1, in0=ubuf[:, cch, 0:512], scalar=pc[:, cw0:cw0 + 1],
                                                                 in1=f_t2, op0=ALU.mult, op1=ALU.add), reads=[uk, "pc", "f_t2"], writes=["f_t1"])

                def ew3():
                    P.op("dve", lambda h: h.tensor_tensor(out=bufY[:, cch, T0:T0 + 512], in0=bks[0].t[:, :], in1=f_t1, op=ALU.mult),
                         reads=[bks[0].k(), "f_t1"], writes=[ykey(cch, j)])
                def seq(*fs):
                    def f():
                        for g_ in fs:
                            g_()
                    return f
                return [seq(mm(1), ew1), seq(mm(2), ew2), seq(mm(0), ew3)]

            def v_group(j, c8):
                T0 = j * 512
                rdA = [akey(4 * j + t) for t in range(4)]

                def f():
                    bk = Bank("f")
                    for c in range(8):
                        P.op("pe", lambda h, c=c: h.matmul(
                            bk.t[0:64, :], lhsT=bufA[:, c, T0 + c8 * 64:T0 + (c8 + 1) * 64], rhs=win[:, c, 5 * 512:6 * 512],
                            start=(c == 0), stop=(c == 7)), reads=rdA + ["win5"], writes=[bk.k()])
                    P.op("act", lambda h: h.activation(out=vtok[0:64, c8, :], in_=bk.t[0:64, :], func=AF.Copy),
                         reads=[bk.k()], writes=["vtok%d" % c8])
                return f

            def head_unit(j, hd, q):
                T0 = j * 512
                rdA = [akey(4 * j + t) for t in range(4)]
                t = TS[q]
                K = lambda n: "%s.%d" % (n, q)
                st_ = {}
                lbh = hp[:, hd:hd + 1]
                lnomlh = hp[:, 8 + hd:9 + hd]
                b3 = t["e"].rearrange("p (c n) -> p c n", c=8)
                sk = "S%d" % hd
                E = []

                def proj(name, s):
                    def f():
                        bk = Bank("f")
                        st_[name] = bk
                        for c in range(8):
                            P.op("pe", lambda h, c=c: h.matmul(
                                bk.t[:, :], lhsT=win[:, c, s * 512 + hd * 128:s * 512 + (hd + 1) * 128],
                                rhs=bufA[:, c, T0:T0 + 512], start=(c == 0), stop=(c == 7)),
                                reads=rdA + ["win%d" % s], writes=[bk.k()])
                    return f
                pz_ = proj("z", 4)

                def e1():
                    bz = st_["z"]
                    P.op("act", lambda h: h.activation(out=t["e"], in_=bz.t[:, :], func=AF.Exp, scale=-1.0), reads=[bz.k()], writes=[K("e")])
                    P.op("act", lambda h: h.activation(out=t["l2"], in_=t["e"], func=AF.Ln, bias=one_ap), reads=[K("e"), "one"], writes=[K("l2")])
                    P.op("act", lambda h: h.activation(out=t["l1"], in_=t["e"], func=AF.Ln, scale=lbh, bias=one_ap),
                         reads=[K("e"), "one", "hp_lb"], writes=[K("l1")])

                def e2():
                    bz = st_["z"]
                    P.op("pool", lambda h: h.tensor_tensor(out=t["l1"], in0=t["l1"], in1=t["l2"], op=ALU.subtract), reads=[K("l1"), K("l2")], writes=[K("l1")])
                    P.op("dve", lambda h: h.scalar_tensor_tensor(out=t["lnk"], in0=bz.t[:, :], scalar=-1.0, in1=t["l2"], op0=ALU.mult, op1=ALU.subtract),
                         reads=[bz.k(), K("l2")], writes=[K("lnk")])
                pq_ = proj("q", 3)

                def e3():
                    P.op("dve", lambda h: h.tensor_tensor_scan(out=t["e"], data0=rmask, data1=t["l1"], initial=0.0, op0=ALU.mult, op1=ALU.add),
                         reads=["cn", K("l1"), K("e")], writes=[K("e")])
                    P.op("pool", lambda h: h.tensor_tensor(out=t["d"].rearrange("p (c n) -> p c n", c=8), in0=b3,
                                                           in1=b3[:, :, 63:64].to_broadcast([128, 8, 64]), op=ALU.subtract),
                         reads=[K("e")], writes=[K("d")])

                def e4():
                    bq = st_["q"]
                    P.op("act", lambda h: h.activation(out=t["l2"], in_=t["d"], func=AF.Exp), reads=[K("d"), K("l2")], writes=[K("l2")])
                    P.op("dve", lambda h: h.tensor_tensor(out=t["qt"], in0=bq.t[:, :], in1=t["l2"], op=ALU.mult), reads=[bq.k(), K("l2")], writes=[K("qt")])
                pg_ = proj("g", 6)

                def e5():
                    P.op("pool", lambda h: h.tensor_tensor(out=t["lnk"], in0=t["lnk"], in1=t["d"], op=ALU.subtract), reads=[K("lnk"), K("d")], writes=[K("lnk")])
                    P.op("act", lambda h: h.activation(out=t["khT"], in_=t["lnk"], func=AF.Exp, bias=lnomlh),
                         reads=[K("lnk"), "hp_ln"], writes=[K("khT")])
                    P.op("act", lambda h: h.activation(out=t["dec"], in_=b3[:, :, 63], func=AF.Exp), reads=[K("e")], writes=[K("dec")])

                def e6():
                    bg = st_["g"]
                    P.op("act", lambda h: h.activation(out=t["l1"], in_=bg.t[:, :], func=AF.Exp, scale=-1.0), reads=[bg.k(), K("l1")], writes=[K("l1")])
                    P.op("act", lambda h: h.activation(out=t["l1"], in_=t["l1"], func=AF.Ln, bias=one_ap), reads=[K("l1"), "one"], writes=[K("l1")])
                    P.op("act", lambda h: h.activation(out=t["l1"], in_=t["l1"], func=AF.Exp, scale=-1.0), reads=[K("l1")], writes=[K("l1")])
                    P.op("dve", lambda h: h.tensor_tensor(out=t["l1"], in0=bg.t[:, :], in1=t["l1"], op=ALU.mult), reads=[bg.k(), K("l1")], writes=[K("l1")])

                def grp(*fs):
                    def f():
                        for g_ in fs:
                            g_()
                    return f
                E.append(grp(pz_, e1, e2))
                E.append(grp(pq_, e3, e4))
                E.append(grp(pg_, e5, e6))

                C = []

                def c0():
                    bkk = Bank("b")
                    for c8 in range(8):
                        P.op("pe", lambda h, c8=c8: h.transpose(out=bkk.t[0:64, c8 * 128:(c8 + 1) * 128], in_=t["khT"][:, c8 * 64:(c8 + 1) * 64],
                                                               identity=identb), reads=[K("khT"), "identb"], writes=[bkk.k()])
                    P.op("act", lambda h: h.activation(out=t["khtok"][0:64, :, :].rearrange("p c n -> p (c n)"), in_=bkk.t[0:64, :], func=AF.Copy),
                         reads=[bkk.k()], writes=[K("khtok")])
                    st_["o"] = Bank("f", hold=True)
                    st_["sc"] = Bank("f", hold=True)
                C.append(c0)

                def c1():
                    bsc = st_["sc"]
                    st_["ds0"] = Bank("f", hold=True)
                    st_["ds1"] = Bank("f", hold=True)
                    for c8 in range(8):
                        cs = slice(c8 * 64, (c8 + 1) * 64)
                        P.op("pe", lambda h, cs=cs: h.matmul(bsc.t[0:64, cs], lhsT=t["khT"][:, cs], rhs=t["qt"][:, cs], start=True, stop=True),
                             reads=[K("khT"), K("qt")], writes=[bsc.k(c8)])
                    for c8 in range(8):
                        bds = st_["ds%d" % (c8 // 4)]
                        ds_cols = slice((c8 % 4) * 128, (c8 % 4 + 1) * 128)
                        P.op("pe", lambda h, c8=c8, bds=bds, ds_cols=ds_cols: h.matmul(bds.t[:, ds_cols], lhsT=t["khtok"][0:64, c8, :],
                                                                                        rhs=vtok[0:64, c8, hd * 128:(hd + 1) * 128], start=True, stop=True),
                             reads=[K("khtok"), "vtok%d" % c8], writes=[bds.k(c8 % 4)])
                C.append(c1)

                def c2():
                    bsc = st_["sc"]
                    for c8 in range(8):
                        cs = slice(c8 * 64, (c8 + 1) * 64)
                        P.op("dve", lambda h, c8=c8, cs=cs: h.tensor_tensor(out=t["scm"][0:64, c8, :], in0=bsc.t[0:64, cs], in1=maskb[0:64, :], op=ALU.mult),
                             reads=[bsc.k(c) for c in range(8)] + ["maskb"], writes=[K("scm%d" % c8)])
                    for c8 in range(8):
                        bds = st_["ds%d" % (c8 // 4)]
                        ds_cols = slice((c8 % 4) * 128, (c8 % 4 + 1) * 128)
                        P.op("dve", lambda h, c8=c8: h.tensor_scalar(out=DSb[c8], in0=Sst[:, hd, :], scalar1=t["dec"][:, c8:c8 + 1], scalar2=None, op0=ALU.mult),
                             reads=[sk, K("dec")], writes=["DSb%d" % c8])
                        P.op("dve", lambda h, c8=c8, bds=bds, ds_cols=ds_cols: h.scalar_tensor_tensor(
                            out=Sst[:, hd, :], in0=Sst[:, hd, :], scalar=t["dec"][:, c8:c8 + 1], in1=bds.t[:, ds_cols], op0=ALU.mult, op1=ALU.add),
                            reads=[sk, K("dec")] + [bds.k(c) for c in range(4)], writes=[sk])
                    st_["sc"].done()
                    st_["ds0"].done()
                    st_["ds1"].done()
                C.append(c2)

                def c3():
                    bo = st_["o"]
                    for c8 in range(8):
                        cs = slice(c8 * 64, (c8 + 1) * 64)
                        P.op("pe", lambda h, c8=c8, cs=cs: h.matmul(bo.t[:, cs], lhsT=DSb[c8], rhs=t["qt"][:, cs], start=True, stop=False),
                             reads=["DSb%d" % c8, K("qt")], writes=[bo.k(c8)])
                        P.op("pe", lambda h, c8=c8, cs=cs: h.matmul(bo.t[:, cs], lhsT=vtok[0:64, c8, hd * 128:(hd + 1) * 128], rhs=t["scm"][0:64, c8, :],
                                                                    start=False, stop=True),
                             reads=["vtok%d" % c8, K("scm%d" % c8)], writes=[bo.k(c8)])
                C.append(c3)

                def c9():
                    bo = st_["o"]
                    okeys = [bo.k(c8) for c8 in range(8)]
                    P.op("act", lambda h: h.activation(out=t["khT"], in_=bo.t[:, :], func=AF.Square), reads=okeys + [K("khT")], writes=[K("khT")])
                    bss = Bank("f")
                    P.op("pe", lambda h: h.matmul(bss.t[:, :], lhsT=onesb, rhs=t["khT"], start=True, stop=True), reads=["onesb", K("khT")], writes=[bss.k()])
                    P.op("act", lambda h: h.activation(out=t["l2"], in_=bss.t[:, :], func=AF.Ln, scale=1.0 / 128, bias=eps_ap),
                         reads=[bss.k(), "eps", K("l2")], writes=[K("l2")])
                    P.op("act", lambda h: h.activation(out=t["l2"], in_=t["l2"], func=AF.Exp, scale=-0.5), reads=[K("l2")], writes=[K("l2")])
                    P.op("dve", lambda h: h.tensor_tensor(out=t["d"], in0=bo.t[:, :], in1=t["l2"], op=ALU.mult), reads=okeys + [K("l2"), K("d")], writes=[K("d")])
                    P.op("dve", lambda h: h.scalar_tensor_tensor(out=bufY[:, 4 + hd, T0:T0 + 512], in0=t["d"], scalar=pc[:, PC_HN + hd:PC_HN + hd + 1],
                                                                 in1=t["l1"], op0=ALU.mult, op1=ALU.mult),
                         reads=[K("d"), K("l1"), "pc"], writes=[ykey(4 + hd, j)])
                    bo.done()
                C.append(c9)
                return E, C

            def interleave(primary, fillers):
                n, m = len(primary), len(fillers)
                fi = 0
                for i, f in enumerate(primary):
                    f()
                    want = ((i + 1) * m) // max(n, 1)
                    while fi < want:
                        fillers[fi]()
                        fi += 1
                while fi < m:
                    fillers[fi]()
                    fi += 1

            for cch in range(4):
                for f in conv_chunk(0, cch):
                    f()
            for c8 in range(8):
                v_group(0, c8)()
            units = [(j, hd) for j in range(NJ) for hd in range(4)]
            E0, Cprev = head_unit(0, 0, 0)
            for f in E0:
                f()
            for u in range(len(units)):
                j, hd = units[u]
                fill = []
                if u + 1 < len(units):
                    jn, hn = units[u + 1]
                    En, Cn = head_unit(jn, hn, (u + 1) % NSET)
                    if jn == j:
                        fill += En
                else:
                    En, Cn = [], []
                if j + 1 < NJ:
                    fill += conv_chunk(j + 1, hd)
                for f in Cprev[0:3]:
                    f()
                for f in fill:
                    f()
                for f in Cprev[3:]:
                    f()
                if u + 1 < len(units) and units[u + 1][0] != j:
                    for c8 in range(8):
                        v_group(j + 1, c8)()
                    for f in En:
                        f()
                Cprev = Cn
            P.barrier()
            if stop_after == "A2":
                for c in (4, 5, 6, 7, 0, 1, 2, 3):
                    for jj in range(2):
                        P.op("pool", lambda h, c=c, jj=jj: h.dma_start(out=dbg_d[:, c * 1024 + jj * 512:c * 1024 + (jj + 1) * 512],
                                                                      in_=bufY[:, c, jj * 512:(jj + 1) * 512]), dma=True)
                return True

            top[0] = RT0
            wout = carve(4096, BF16, "p (c n) -> p c n", c=8)
            xsb = [carve(1024) for _ in range(2)]
            xn2 = [carve(512, BF16) for _ in range(2)]
            wq = carve(4096, BF16, "p (c n) -> p c n", c=8)
            wo = carve(4096, BF16, "p (c n) -> p c n", c=8)
            wq_off = RT0 + 4096 + 2048 + 1024
            P.op("pool", lambda h: h.dma_start(out=wq, in_=wview(w_q_d, 0, D)), writes=["wq"], dma=True)
            P.op("pool", lambda h: h.dma_start(out=wo, in_=wview(w_o_d, 0, D)), writes=["wo"], dma=True)
            for i in range(NT):
                xk = "xs%d" % (i % 2)
                P.op("sp", lambda h, i=i: h.dma_start(out=xsb[i % 2], in_=x_d[tok0 + i * 128:tok0 + (i + 1) * 128, :]), writes=[xk], dma=True)
                for n2 in range(2):
                    bk = Bank("f")
                    for c in range(8):
                        P.op("pe", lambda h, i=i, c=c, n2=n2, bk=bk: h.matmul(bk.t[:, :], lhsT=bufY[:, c, i * 128:(i + 1) * 128],
                                                                              rhs=wout[:, c, n2 * 512:(n2 + 1) * 512], start=(c == 0), stop=(c == 7)),
                             reads=["wout"], writes=[bk.k()])
                    P.op("dve", lambda h, i=i, n2=n2, bk=bk: h.tensor_tensor(out=xres[:, i, n2 * 512:(n2 + 1) * 512], in0=bk.t[:, :],
                                                                             in1=xsb[i % 2][:, n2 * 512:(n2 + 1) * 512], op=ALU.add),
                         reads=[bk.k(), xk], writes=[xkey(i)])
                norm_tile(xres[:, i, :], xkey(i), 1, bufA, akey(i), rstd3[:, i, :], "st.%d" % i, xn2[i % 2], "xn%d" % (i % 2), i)
            P.barrier()
            if stop_after == "B1":
                for i in range(8):
                    P.op("pool", lambda h, i=i: h.dma_start(out=dbg_d[:, i * 1024:(i + 1) * 1024], in_=xres[:, i, :]), dma=True)
                return True

            KT = carve_at(RT0, 1024, BF16, "p (c n) -> p c n", c=8)
            Vt = carve_at(RT0 + 1024, 1024, BF16, "p (c n) -> p c n", c=2)
            mstat = carve_at(RT0 + 2048, 4)
            wq = carve_at(wq_off, 4096, BF16, "p (c n) -> p c n", c=8)
            wo = carve_at(wq_off + 4096, 4096, BF16, "p (c n) -> p c n", c=8)
            top[0] = bufY_off
            wkvb = [carve(2048, BF16, "p (c n) -> p c n", c=8) for _ in range(2)]
            mT = carve(1024, BF16, "p (c n) -> p c n", c=8)
            mst = [carve(1024) for _ in range(2)]
            xn2 = [carve(512, BF16) for _ in range(2)]
            assert top[0] <= bufY_off + 8192
            for t in range(2):
                P.op("sp", lambda h, t=t: h.dma_start(out=mst[t], in_=mem_d[hf * 256 + t * 128:hf * 256 + (t + 1) * 128, :]),
                     writes=["mst%d" % t], dma=True)
            for blk in range(2):
                P.op("pool", lambda h, blk=blk: h.dma_start(out=wkvb[blk], in_=wview(w_kv_d, blk * 512, (blk + 1) * 512)), writes=["wkv%d" % blk], dma=True)
            for t in range(2):
                norm_tile(mst[t], "mst%d" % t, 2, mT, "mT", mstat[:, 2 * t:2 * t + 2], "mstat%d" % t, xn2[t], "xn%d" % t, t)
            for blk in range(4):
                wb = wkvb[blk % 2]
                wk = "wkv%d" % (blk % 2)
                if blk >= 2:
                    P.op("pool", lambda h, blk=blk, wb=wb: h.dma_start(out=wb, in_=wview(w_kv_d, blk * 512, (blk + 1) * 512)), writes=[wk], dma=True)
                if blk < 2:
                    for e4 in range(4):
                        ec = blk * 4 + e4
                        bk = Bank("f")
                        for c in range(8):
                            P.op("pe", lambda h, c=c, e4=e4, wb=wb, bk=bk: h.matmul(bk.t[:, 0:256], lhsT=wb[:, c, e4 * 128:(e4 + 1) * 128], rhs=mT[:, c, :],
                                                                                   start=(c == 0), stop=(c == 7)), reads=[wk, "mT"], writes=[bk.k()])
                        P.op("act", lambda h, ec=ec, bk=bk: h.activation(out=KT[:, ec, :], in_=bk.t[:, 0:256], func=AF.Copy), reads=[bk.k()], writes=["KT"])
                else:
                    n2 = blk - 2
                    for mc in range(2):
                        bk = Bank("f")
                        for c in range(8):
                            P.op("pe", lambda h, c=c, mc=mc, wb=wb, bk=bk: h.matmul(bk.t[:, :], lhsT=mT[:, c, mc * 128:(mc + 1) * 128], rhs=wb[:, c, :],
                                                                                   start=(c == 0), stop=(c == 7)), reads=[wk, "mT"], writes=[bk.k()])
                        P.op("act", lambda h, mc=mc, n2=n2, bk=bk: h.activation(out=Vt[:, mc, n2 * 512:(n2 + 1) * 512], in_=bk.t[:, :], func=AF.Copy),
                             reads=[bk.k()], writes=["Vt"])
            P.barrier()
            top[0] = bufY_off
            QT = carve(2048, BF16, "p (c n) -> p c n", c=8)
            OT = carve(2048, BF16, "p (c n) -> p c n", c=8)
            ET = [carve(512, BF16, "p (c n) -> p c n", c=2) for _ in range(2)]
            rden = [carve(512) for _ in range(2)]

            def q_proj(j):
                T0 = j * 512
                rdA = [akey(4 * j + t) for t in range(4)]
                for ec in range(8):
                    bk = Bank("f")
                    for c in range(8):
                        P.op("pe", lambda h, c=c, ec=ec, bk=bk: h.matmul(bk.t[:, :], lhsT=wq[:, c, ec * 128:(ec + 1) * 128], rhs=bufA[:, c, T0:T0 + 512],
                                                                        start=(c == 0), stop=(c == 7)), reads=rdA + ["wq"], writes=[bk.k()])
                    if ec % 2 == 0:
                        P.op("act", lambda h, ec=ec, bk=bk: h.activation(out=QT[:, ec, :], in_=bk.t[:, :], func=AF.Copy, scale=1.0 / 16.0),
                             reads=[bk.k()], writes=["QT%d" % ec])
                    else:
                        P.op("dve", lambda h, ec=ec, bk=bk: h.tensor_scalar(out=QT[:, ec, :], in0=bk.t[:, :], scalar1=1.0 / 16.0, scalar2=None, op0=ALU.mult),
                             reads=[bk.k()], writes=["QT%d" % ec])

            def s_exp(hd):
                et = ET[hd % 2]
                ek = "ET%d" % (hd % 2)
                for mc in range(2):
                    bk = Bank("f")
                    for k2 in range(2):
                        P.op("pe", lambda h, mc=mc, k2=k2, bk=bk: h.matmul(bk.t[:, :], lhsT=KT[:, 2 * hd + k2, mc * 128:(mc + 1) * 128],
                                                                          rhs=QT[:, 2 * hd + k2, :], start=(k2 == 0), stop=(k2 == 1)),
                             reads=["KT", "QT%d" % (2 * hd + k2)], writes=[bk.k()])
                    P.op("act", lambda h, mc=mc, bk=bk: h.activation(out=et[:, mc, :], in_=bk.t[:, :], func=AF.Exp), reads=[bk.k()], writes=[ek + ".%d" % mc])

            def pv(hd):
                et = ET[hd % 2]
                ek = "ET%d" % (hd % 2)
                bden = Bank("f")
                for mc in range(2):
                    P.op("pe", lambda h, mc=mc: h.matmul(bden.t[:, :], lhsT=onesb, rhs=et[:, mc, :], start=(mc == 0), stop=(mc == 1)),
                         reads=["onesb", ek + ".%d" % mc], writes=[bden.k()])
                rd = rden[hd % 2]
                rk = "rden%d" % (hd % 2)
                P.op("dve", lambda h: h.reciprocal(out=rd, in_=bden.t[:, :]), reads=[bden.k()], writes=[rk])
                for k2 in range(2):
                    bk = Bank("f")
                    for mc in range(2):
                        P.op("pe", lambda h, mc=mc, k2=k2, bk=bk: h.matmul(
                            bk.t[:, :], lhsT=Vt[:, mc, (2 * hd + k2) * 128:(2 * hd + k2 + 1) * 128], rhs=et[:, mc, :], start=(mc == 0), stop=(mc == 1)),
                            reads=["Vt", ek + ".%d" % mc], writes=[bk.k()])
                    P.op("dve", lambda h, k2=k2, bk=bk: h.tensor_tensor(out=OT[:, 2 * hd + k2, :], in0=bk.t[:, :], in1=rd, op=ALU.mult),
                         reads=[bk.k(), rk], writes=["OT%d" % (2 * hd + k2)])

            def w_o(j):
                for tt in range(4):
                    i = 4 * j + tt
                    for n2 in range(2):
                        bk = Bank("f")
                        for ec in range(8):
                            P.op("pe", lambda h, ec=ec, tt=tt, n2=n2, bk=bk: h.matmul(bk.t[:, :], lhsT=OT[:, ec, tt * 128:(tt + 1) * 128],
                                                                                     rhs=wo[:, ec, n2 * 512:(n2 + 1) * 512], start=(ec == 0), stop=(ec == 7)),
                                 reads=["OT%d" % ec, "wo"], writes=[bk.k()])
                        P.op("dve", lambda h, i=i, n2=n2, bk=bk: h.tensor_tensor(out=xres[:, i, n2 * 512:(n2 + 1) * 512], in0=bk.t[:, :],
                                                                                 in1=xres[:, i, n2 * 512:(n2 + 1) * 512], op=ALU.add),
                             reads=[bk.k(), xkey(i)], writes=[xkey(i)])

            q_proj(0)
            for j in range(NJ):
                s_exp(0)
                for hd in range(4):
                    if hd + 1 < 4:
                        s_exp(hd + 1)
                    pv(hd)
                if j + 1 < NJ:
                    q_proj(j + 1)
                w_o(j)
            P.barrier()
            if stop_after == "B2":
                for i in range(8):
                    P.op("pool", lambda h, i=i: h.dma_start(out=dbg_d[:, i * 1024:(i + 1) * 1024], in_=xres[:, i, :]), dma=True)
                return True

            top[0] = bufY_off
            wg = [carve(2048, BF16, "p (c n) -> p c n", c=8) for _ in range(2)]
            wu = [carve(2048, BF16, "p (c n) -> p c n", c=8) for _ in range(2)]
            top[0] = RT0
            wd = [carve(2048, BF16, "p (c n) -> p c n", c=4) for _ in range(2)]
            xn2 = [carve(512, BF16) for _ in range(2)]
            lg = carve(320, F32, "p (i n) -> p i n", i=16)
            comb = carve(256, F32, "p (i n) -> p i n", i=16)
            r_t = [carve(256) for _ in range(4)]
            r_s = [carve(64) for _ in range(6)]
            hid = [carve(1024, BF16, "p (c n) -> p c n", c=4) for _ in range(2)]
            sgb = [carve(256, BF16) for _ in range(2)]

            def wload(e):
                b = e % 2
                P.op("pool", lambda h: h.dma_start(out=wg[b], in_=w_gate_d[e].rearrange("(c p) n -> p c n", p=128)), writes=["wg%d" % b], dma=True)
                P.op("pool", lambda h: h.dma_start(out=wu[b], in_=w_up_d[e].rearrange("(c p) n -> p c n", p=128)), writes=["wu%d" % b], dma=True)
                P.op("pool", lambda h: h.dma_start(out=wd[b], in_=w_down_d[e].rearrange("(c p) n -> p c n", p=128)), writes=["wd%d" % b], dma=True)

            wload(0)
            wload(1)
            for i in range(NT):
                norm_tile(xres[:, i, :], xkey(i), 3, bufA, akey(i), rstd3[:, i, :], "st.%d" % i, xn2[i % 2], "xn%d" % (i % 2), i)
                bk = Bank("f")
                for c in range(8):
                    P.op("pe", lambda h, c=c, i=i, bk=bk: h.matmul(bk.t[:, 0:20], lhsT=bufA[:, c, i * 128:(i + 1) * 128], rhs=wrb[:, c, :],
                                                                  start=(c == 0), stop=(c == 7)), reads=[akey(i), "wrb"], writes=[bk.k()])
                P.op("dve", lambda h, i=i, bk=bk: h.tensor_tensor(out=lg[:, i, :], in0=bk.t[:, 0:20], in1=rows[:, 1024:1044], op=ALU.add),
                     reads=[bk.k(), "rows"], writes=["lg"])
            LG = lg[:, :, 0:4]
            LE = lg[:, :, 4:20].rearrange("p i (j k) -> p i j k", j=4)
            gmax, gsum, m1, m2, w1, w2 = [r[:, 0:16] for r in r_s]
            gsh = r_t[0][:, 0:64].rearrange("p (i j) -> p i j", i=16)
            gm = r_t[1][:, 0:64].rearrange("p (i j) -> p i j", i=16)
            tmp4 = r_t[2].rearrange("p (i j k) -> p i j k", i=16, j=4)
            esel = r_t[3][:, 0:64].rearrange("p (i k) -> p i k", i=16)
            mk1 = r_t[3][:, 64:128].rearrange("p (i k) -> p i k", i=16)
            e2 = r_t[3][:, 128:192].rearrange("p (i k) -> p i k", i=16)
            mk2 = r_t[3][:, 192:256].rearrange("p (i k) -> p i k", i=16)
            cig = r_t[0][:, 64:128].rearrange("p (i k) -> p i k", i=16)
            tq = r_t[0][:, 128:192].rearrange("p (i k) -> p i k", i=16)

            def bc3(a):
                return a.unsqueeze(2).to_broadcast([128, 16, 4])

            R = lambda fn, eng="dve": P.op(eng, fn, reads=["lg", "rt"], writes=["rt"])
            R(lambda h: h.tensor_reduce(out=gmax, in_=LG, axis=AX.X, op=ALU.max))
            R(lambda h: h.tensor_tensor(out=gsh, in0=LG, in1=bc3(gmax), op=ALU.subtract))
            R(lambda h: h.tensor_single_scalar(out=gm, in_=gsh, scalar=0.0, op=ALU.is_ge))
            R(lambda h: h.activation(out=gsh, in_=gsh, func=AF.Exp), "act")
            R(lambda h: h.tensor_reduce(out=gsum, in_=gsh, axis=AX.X, op=ALU.add))
            R(lambda h: h.reciprocal(out=gsum, in_=gsum))
            R(lambda h: h.tensor_tensor(out=tmp4, in0=LE, in1=gm.unsqueeze(3).to_broadcast([128, 16, 4, 4]), op=ALU.mult))
            R(lambda h: h.tensor_reduce(out=esel, in_=tmp4.rearrange("p i j k -> p i k j"), axis=AX.X, op=ALU.add))
            R(lambda h: h.tensor_reduce(out=m1, in_=esel, axis=AX.X, op=ALU.max))
            R(lambda h: h.tensor_tensor(out=mk1, in0=esel, in1=bc3(m1), op=ALU.is_ge))
            R(lambda h: h.scalar_tensor_tensor(out=e2, in0=mk1, scalar=-1e30, in1=esel, op0=ALU.mult, op1=ALU.add))
            R(lambda h: h.tensor_reduce(out=m2, in_=e2, axis=AX.X, op=ALU.max))
            R(lambda h: h.tensor_tensor(out=mk2, in0=e2, in1=bc3(m2), op=ALU.is_ge))
            R(lambda h: h.tensor_tensor(out=w2, in0=m2, in1=m1, op=ALU.subtract))
            R(lambda h: h.activation(out=w2, in_=w2, func=AF.Exp), "act")
            R(lambda h: h.tensor_scalar_add(out=w1, in0=w2, scalar1=1.0))
            R(lambda h: h.reciprocal(out=w1, in_=w1))
            R(lambda h: h.tensor_tensor(out=w2, in0=w2, in1=w1, op=ALU.mult))
            R(lambda h: h.tensor_tensor(out=w1, in0=w1, in1=gsum, op=ALU.mult))
            R(lambda h: h.tensor_tensor(out=w2, in0=w2, in1=gsum, op=ALU.mult))
            R(lambda h: h.tensor_tensor(out=cig, in0=mk1, in1=bc3(w1), op=ALU.mult))
            R(lambda h: h.tensor_tensor(out=tq, in0=mk2, in1=bc3(w2), op=ALU.mult))
            R(lambda h: h.tensor_tensor(out=cig, in0=cig, in1=tq, op=ALU.add))
            P.op("dve", lambda h: h.tensor_tensor(out=comb.rearrange("p i (j k) -> p i j k", j=4), in0=gm.unsqueeze(3).to_broadcast([128, 16, 4, 4]),
                                                  in1=cig.unsqueeze(2).to_broadcast([128, 16, 4, 4]), op=ALU.mult), reads=["rt"], writes=["comb"])
            if stop_after == "R":
                P.barrier()
                P.op("pool", lambda h: h.dma_start(out=dbg_d[:, 0:256], in_=comb.rearrange("p i n -> p (i n)")), dma=True)
                P.op("pool", lambda h: h.dma_start(out=dbg_d[:, 256:576], in_=lg.rearrange("p i n -> p (i n)")), dma=True)
                return True

            units = [(e, j) for e in range(NEXP) for j in range(NJ)]

            def gate_up(u):
                e, j = units[u]
                b = e % 2
                T0 = j * 512
                rdA = [akey(4 * j + t) for t in range(4)]
                hb = hid[u % 2]
                for f in range(4):
                    bg_, bu_ = Bank("f"), Bank("f")
                    for (bk, w, wk) in ((bg_, wg[b], "wg%d" % b), (bu_, wu[b], "wu%d" % b)):
                        for c in range(8):
                            P.op("pe", lambda h, c=c, f=f, bk=bk, w=w, T0=T0: h.matmul(bk.t[:, :], lhsT=w[:, c, f * 128:(f + 1) * 128], rhs=bufA[:, c, T0:T0 + 512],
                                                                                start=(c == 0), stop=(c == 7)), reads=rdA + [wk], writes=[bk.k()])
                    sg = sgb[f % 2]
                    P.op("act", lambda h, sg=sg, bg_=bg_: h.activation(out=sg, in_=bg_.t[:, :], func=AF.Silu), reads=[bg_.k()], writes=["sgb%d" % (f % 2)])
                    P.op("dve", lambda h, f=f, sg=sg, bu_=bu_, hb=hb: h.tensor_tensor(out=hb[:, f, :], in0=bu_.t[:, :], in1=sg, op=ALU.mult),
                         reads=[bu_.k(), "sgb%d" % (f % 2)], writes=["hid%d.%d" % (u % 2, f)])

            def down(u):
                e, j = units[u]
                b = e % 2
                hb = hid[u % 2]
                for tt in range(4):
                    i = 4 * j + tt
                    for n2 in range(2):
                        bk = Bank("f")
                        for f in range(4):
                            P.op("pe", lambda h, f=f, tt=tt, n2=n2, bk=bk: h.matmul(bk.t[:, :], lhsT=hb[:, f, tt * 128:(tt + 1) * 128],
                                                                                   rhs=wd[b][:, f, n2 * 512:(n2 + 1) * 512], start=(f == 0), stop=(f == 3)),
                                 reads=["hid%d.%d" % (u % 2, f), "wd%d" % b], writes=[bk.k()])
                        P.op("dve", lambda h, i=i, n2=n2, bk=bk: h.scalar_tensor_tensor(
                            out=xres[:, i, n2 * 512:(n2 + 1) * 512], in0=bk.t[:, :], scalar=comb[:, i, e:e + 1],
                            in1=xres[:, i, n2 * 512:(n2 + 1) * 512], op0=ALU.mult, op1=ALU.add),
                            reads=[bk.k(), "comb", xkey(i)], writes=[xkey(i)])

            for u in range(len(units) + 1):
                if u < len(units):
                    gate_up(u)
                if u >= 1:
                    down(u - 1)
                    e, j = units[u - 1]
                    if j == NJ - 1 and e + 2 < NEXP:
                        wload(e + 2)
            fin = rows[:, 0:1024]
            for i in range(NT):
                stt = rstd3[:, i, :]
                sk_ = "st.%d" % i
                P.op("act", lambda h, i=i, stt=stt: h.activation(out=xn2[i % 2], in_=xres[:, i, :], func=AF.Square, accum_out=stt[:, 0:1]),
                     reads=[xkey(i)], writes=["xn%d" % (i % 2), sk_])
                P.op("act", lambda h, stt=stt: h.activation(out=stt[:, 1:2], in_=stt[:, 0:1], func=AF.Ln, scale=1.0 / D, bias=eps_ap), reads=[sk_, "eps"], writes=[sk_])
                P.op("act", lambda h, stt=stt: h.activation(out=stt[:, 1:2], in_=stt[:, 1:2], func=AF.Exp, scale=-0.5), reads=[sk_], writes=[sk_])
                P.op("dve", lambda h, i=i, stt=stt: h.scalar_tensor_tensor(out=xres[:, i, :], in0=xres[:, i, :], scalar=stt[:, 1:2], in1=fin, op0=ALU.mult, op1=ALU.mult),
                     reads=[xkey(i), sk_, "rows"], writes=[xkey(i)])
                P.op("sp", lambda h, i=i: h.dma_start(out=out_d[tok0 + i * 128:tok0 + (i + 1) * 128, :], in_=xres[:, i, :]),
                     reads=[xkey(i)], writes=["out.%d.%d" % (hf, i)], dma=True)
            P.barrier()
            return False

        for hf_ in range(nhalves):
            if do_half(hf_):
                break
        P.emit()
    return nc


_CACHE = {}


def _host_consts():
    cn = np.zeros((128, CN_N), np.float32)
    cn[:, CN_ID:CN_ID + 128] = np.eye(128, dtype=np.float32)
    cn[:, CN_ONE:CN_ONE + 128] = 1.0
    s = np.arange(64)[:, None]
    t = np.arange(64)[None, :]
    cn[0:64, CN_MASK:CN_MASK + 64] = (s <= t).astype(np.float32)
    rm = np.ones(512, np.float32)
    rm[::64] = 0.0
    cn[:, CN_RM:CN_RM + 512] = rm[None, :]
    return cn


def _col(v, nchunk):
    return np.ascontiguousarray(np.asarray(v, np.float32).reshape(nchunk, 128).T)


def make_in_maps(inputs):
    f = lambda k: np.asarray(inputs[k], np.float32)
    pcols = np.zeros((128, PC_N), np.float32)
    for gi, k in enumerate(["mix_norm", "xattn_norm", "mem_norm", "ffn_norm"]):
        pcols[:, PC_G + gi * 8:PC_G + gi * 8 + 8] = _col(f(k)[0], 8)
    cw = f("conv_w")[0]
    for cch in range(4):
        for jj in range(3):
            pcols[:, PC_CONV + cch * 3 + jj] = cw[jj, cch * 128:(cch + 1) * 128]
    lbr = f("hgrn_lb")
    for r in range(2):
        pcols[:, PC_LB + r * 4:PC_LB + r * 4 + 4] = _col(lbr[r], 4)
    pcols[:, PC_HN:PC_HN + 4] = _col(f("hgrn_norm")[0], 4)
    wrc = np.concatenate([f("w_group")[0], f("w_expert")[0]], axis=1)
    wr = np.ascontiguousarray(wrc.reshape(8, 128, 20).transpose(1, 0, 2).reshape(128, 160))
    rows = np.concatenate([f("final_norm").reshape(-1), f("b_group")[0], f("b_expert")[0]])[None, :].astype(np.float32)
    consts = _host_consts()
    x = f("x")
    mem = f("mem")
    shared = dict(
        w_in=np.ascontiguousarray(f("w_in")[0]), w_out=np.ascontiguousarray(f("w_out")[0]),
        w_q=np.ascontiguousarray(f("w_q")[0]), w_kv=np.ascontiguousarray(f("w_kv")[0]), w_o=np.ascontiguousarray(f("w_o")[0]),
        w_gate=np.ascontiguousarray(f("w_gate")[0]), w_up=np.ascontiguousarray(f("w_up")[0]), w_down=np.ascontiguousarray(f("w_down")[0]),
        pcols=pcols, wr=wr, rows=np.ascontiguousarray(rows), consts=consts)
    maps = []
    for c in range(NCORES):
        m = dict(shared)
        m["x"] = np.ascontiguousarray(x[2 * c:2 * c + 2].reshape(2 * SEQ, D))
        m["mem"] = np.ascontiguousarray(mem[2 * c:2 * c + 2].reshape(512, D))
        maps.append(m)
    return maps


def kernel(**inputs):
    if "nc" not in _CACHE:
        _CACHE["nc"] = build_program()
    nc = _CACHE["nc"]
    maps = make_in_maps(inputs)
    res = run_bass_kernel_spmd(nc, maps, core_ids=list(range(NCORES)))
    outs = [np.asarray(r["out"], np.float32).reshape(2, SEQ, D) for r in res.results]
    return np.concatenate(outs, axis=0)
```

```python
import contextlib
import numpy as np
import concourse.bass as bass
import concourse.mybir as mybir
from concourse.bass_utils import run_bass_kernel_spmd

F32 = mybir.dt.float32
BF16 = mybir.dt.bfloat16
AF = mybir.ActivationFunctionType
ALU = mybir.AluOpType
AX = mybir.AxisListType

ENGS = ("pe", "act", "dve", "pool", "sp")
EPS = 1e-6
NCORES = 8
SEQ = 2048
D = 1024
NT = 16
NJ = 4
NEXP = 16


class Prog:
    NDMA_SEMS = 24

    def __init__(self, nc):
        self.nc = nc
        self.ops = []
        self.last_w = {}
        self.readers = {}
        self.dma_rr = [0, 0]
        self.dma_last = {}
        self.last_on = {}

    def alias(self, old_keys, new_key):
        s = set()
        for k in old_keys:
            w = self.last_w.get(k)
            if w is not None:
                s.add(w)
            s.update(self.readers.get(k, ()))
        self.last_w[new_key] = None
        self.readers[new_key] = list(s)

    def op(self, eng, fn, reads=(), writes=(), dma=False, extra=()):
        oid = len(self.ops)
        deps = set(extra)
        for k in reads:
            w = self.last_w.get(k)
            if w is not None:
                deps.add(w)
        for k in writes:
            w = self.last_w.get(k)
            if w is not None:
                deps.add(w)
            for r in self.readers.get(k, ()):
                deps.add(r)
        deps.discard(oid)
        rec = dict(id=oid, eng=eng, fn=fn, deps=deps, dma=dma, sig=False, dsem=None)
        if dma:
            half = self.NDMA_SEMS // 2
            q = 0 if eng == "pool" else 1
            s = q * half + (self.dma_rr[q] % half)
            self.dma_rr[q] += 1
            rec["dsem"] = s
            prev = self.dma_last.get(s)
            if prev is not None:
                deps.add(prev)
            self.dma_last[s] = oid
        self.ops.append(rec)
        if fn is not None:
            self.last_on[eng] = oid
        for k in writes:
            self.last_w[k] = oid
            self.readers[k] = []
        for k in reads:
            if k not in writes:
                self.readers.setdefault(k, []).append(oid)
        return oid

    def barrier(self):
        tails = set(self.last_on.values()) | set(self.dma_last.values())
        for e in ENGS:
            self.op(e, None, extra=set(tails))
        self.last_w = {}
        self.readers = {}

    def emit(self):
        nc = self.nc
        ops = self.ops
        for o in ops:
            if o["eng"] == "pe" and not o["dma"]:
                o["deps"] = {d for d in o["deps"] if ops[d]["dma"] or ops[d]["eng"] != "pe"}
        for o in ops:
            for d in o["deps"]:
                ops[d]["sig"] = True
        cnt = {e: 0 for e in ENGS}
        dcnt = {}
        for o in ops:
            if o["dma"]:
                s = o["dsem"]
                dcnt[s] = dcnt.get(s, 0) + 16
                o["ev"] = ("d%d" % s, dcnt[s])
            elif o["sig"]:
                cnt[o["eng"]] += 1
                o["ev"] = (o["eng"], cnt[o["eng"]])
        with contextlib.ExitStack() as st:
            sems = {}
            for e in ENGS:
                sems[e] = st.enter_context(nc.semaphore("sem_" + e))
            for s in range(self.NDMA_SEMS):
                sems["d%d" % s] = st.enter_context(nc.semaphore("sem_d%d" % s))
            block = st.enter_context(nc.Block())
            per = {e: [o for o in ops if o["eng"] == e] for e in ENGS}

            def run(e, handle):
                waited = {}
                for o in per[e]:
                    need = {}
                    for d in o["deps"]:
                        sname, val = ops[d]["ev"]
                        if need.get(sname, 0) < val:
                            need[sname] = val
                    for sname, val in need.items():
                        if waited.get(sname, 0) >= val:
                            continue
                        handle.wait_ge(sems[sname], val)
                        waited[sname] = val
                    if o["fn"] is None:
                        continue
                    ins = o["fn"](handle)
                    if o["dma"]:
                        ins.then_inc(sems[o["ev"][0]], 16)
                    elif o["sig"]:
                        ins.then_inc(sems[e], 1)

            @block.tensor
            def _(h):
                run("pe", h)

            @block.scalar
            def _(h):
                run("act", h)

            @block.vector
            def _(h):
                run("dve", h)

            @block.gpsimd
            def _(h):
                run("pool", h)

            @block.sync
            def _(h):
                run("sp", h)
                for s, v in dcnt.items():
                    h.wait_ge(sems["d%d" % s], v)


PC_G = 0
PC_CONV = 32
PC_LB = 44
PC_HN = 52
PC_N = 56
CN_ID = 0
CN_ONE = 128
CN_MASK = 256
CN_RM = 320
CN_N = 832


def build_program(stop_after=None, nhalves=2):
    nc = bass.Bass("TRN2", target_bir_lowering=False)

    def dram(name, shape, kind="ExternalInput"):
        return nc.dram_tensor(name, shape, F32, kind=kind).ap()

    x_d = dram("x", [2 * SEQ, D])
    mem_d = dram("mem", [512, D])
    w_in_d = dram("w_in", [D, 3584])
    w_out_d = dram("w_out", [D, D])
    w_q_d = dram("w_q", [D, D])
    w_kv_d = dram("w_kv", [D, 2 * D])
    w_o_d = dram("w_o", [D, D])
    w_gate_d = dram("w_gate", [NEXP, D, 512])
    w_up_d = dram("w_up", [NEXP, D, 512])
    w_down_d = dram("w_down", [NEXP, 512, D])
    pcols_d = dram("pcols", [128, PC_N])
    wr_d = dram("wr", [128, 160])
    rows_d = dram("rows", [1, 1044])
    consts_d = dram("consts", [128, CN_N])
    out_d = dram("out", [2 * SEQ, D], kind="ExternalOutput")
    dbg_d = dram("dbg", [128, 8192], kind="ExternalOutput") if stop_after else None

    st = contextlib.ExitStack()
    with st:
        NW = 51 * 1024
        big = st.enter_context(nc.sbuf_tensor("big", [128, NW], F32))
        psf = [st.enter_context(nc.psum_tensor("psf%d" % i, [128, 512], F32)) for i in range(6)]
        psb = [st.enter_context(nc.psum_tensor("psb%d" % i, [128, 1024], BF16)) for i in range(2)]
        P = Prog(nc)

        top = [0]

        def carve(words, dtype=F32, pattern=None, **kw):
            off = top[0]
            top[0] += words
            assert top[0] <= NW, "SBUF arena overflow %d" % top[0]
            ap = big[:, off:off + words]
            if dtype == BF16:
                ap = ap.bitcast(BF16)
            if pattern:
                ap = ap.rearrange(pattern, **kw)
            return ap

        bank_state = {}
        rr = {"f": 0, "b": 0}

        held = set()

        class Bank:
            def __init__(self, kind, hold=False):
                n = 6 if kind == "f" else 2
                for _ in range(n):
                    self.idx = rr[kind] % n
                    rr[kind] += 1
                    if (kind, self.idx) not in held:
                        break
                else:
                    raise RuntimeError("all PSUM banks held")
                self.kind = kind
                self.gen = rr[kind]
                self.t = psf[self.idx] if kind == "f" else psb[self.idx]
                self.name = "%s%d" % (kind, self.idx)
                self.old = bank_state.get(self.name, [])
                self.keys = {}
                bank_state[self.name] = []
                if hold:
                    held.add((kind, self.idx))

            def done(self):
                held.discard((self.kind, self.idx))

            def k(self, sub=0):
                if sub not in self.keys:
                    key = "ps.%s.%d.%s" % (self.name, self.gen, sub)
                    P.alias(self.old, key)
                    self.keys[sub] = key
                    bank_state[self.name].append(key)
                return self.keys[sub]

        pc = carve(PC_N)
        cn = carve(CN_N)
        identb = carve(64, BF16)
        onesb = carve(64, BF16)
        maskb = carve(32, BF16)
        wr = carve(160, F32, "p (c n) -> p c n", c=8)
        wrg = carve(160, F32, "p (c n) -> p c n", c=8)
        rows = carve(1044)
        hp = carve(16)
        ident = cn[:, CN_ID:CN_ID + 128]
        rmask = cn[:, CN_RM:CN_RM + 512]
        base_top = top[0]

        P.op("sp", lambda h: h.dma_start(out=pc, in_=pcols_d), writes=["pc"], dma=True)
        P.op("sp", lambda h: h.dma_start(out=cn, in_=consts_d), writes=["cn"], dma=True)
        P.op("sp", lambda h: h.dma_start(out=wr.rearrange("p c n -> p (c n)"), in_=wr_d), writes=["wr"], dma=True)
        P.op("sp", lambda h: h.dma_start(out=rows, in_=rows_d.partition_broadcast(128)), writes=["rows"], dma=True)
        P.op("dve", lambda h: h.tensor_copy(out=identb, in_=cn[:, CN_ID:CN_ID + 128]), reads=["cn"], writes=["identb"])
        P.op("dve", lambda h: h.tensor_copy(out=onesb, in_=cn[:, CN_ONE:CN_ONE + 128]), reads=["cn"], writes=["onesb"])
        P.op("dve", lambda h: h.tensor_copy(out=maskb, in_=cn[:, CN_MASK:CN_MASK + 64]), reads=["cn"], writes=["maskb"])
        P.op("dve", lambda h: h.tensor_tensor(out=hp[:, 12:16], in0=pc[:, PC_LB + 4:PC_LB + 8], in1=pc[:, PC_LB:PC_LB + 4],
                                              op=ALU.subtract), reads=["pc"], writes=["hp_t"])
        P.op("act", lambda h: h.activation(out=hp[:, 12:16], in_=hp[:, 12:16], func=AF.Exp), reads=["hp_t"], writes=["hp_t"])
        P.op("dve", lambda h: h.tensor_scalar_add(out=hp[:, 12:16], in0=hp[:, 12:16], scalar1=1.0), reads=["hp_t"], writes=["hp_t"])
        P.op("dve", lambda h: h.reciprocal(out=hp[:, 0:4], in_=hp[:, 12:16]), reads=["hp_t"], writes=["hp_lb"])
        P.op("dve", lambda h: h.tensor_scalar(out=hp[:, 4:8], in0=hp[:, 0:4], scalar1=-1.0, scalar2=1.0, op0=ALU.mult, op1=ALU.add),
             reads=["hp_lb"], writes=["hp_oml"])
        P.op("act", lambda h: h.activation(out=hp[:, 8:12], in_=hp[:, 4:8], func=AF.Ln), reads=["hp_oml"], writes=["hp_ln"])
        for c in range(8):
            P.op("dve", lambda h, c=c: h.tensor_scalar(out=wrg[:, c, :], in0=wr[:, c, :], scalar1=pc[:, PC_G + 24 + c:PC_G + 25 + c],
                                                       scalar2=None, op0=ALU.mult), reads=["wr", "pc"], writes=["wrg"])

        def wview(w2d, c0, c1):
            return w2d.rearrange("(c p) n -> p c n", p=128)[:, :, c0:c1]

        def gbc(gi):
            return pc[:, PC_G + gi * 8:PC_G + gi * 8 + 8].unsqueeze(2).to_broadcast([128, 8, 128])

        def norm_a(src, src_key, stat, stat_key, xn, xn_key):
            ss = stat[:, 0:1]
            rs = stat[:, 1:2]
            P.op("act", lambda h: h.activation(out=xn, in_=src, func=AF.Square, accum_out=ss),
                 reads=[src_key], writes=[xn_key, stat_key])
            P.op("act", lambda h: h.activation(out=rs, in_=ss, func=AF.Ln, scale=1.0 / D, bias=eps_ap), reads=[stat_key, "eps"], writes=[stat_key])
            P.op("act", lambda h: h.activation(out=rs, in_=rs, func=AF.Exp, scale=-0.5), reads=[stat_key], writes=[stat_key])
            P.op("dve", lambda h: h.tensor_scalar(out=xn, in0=src, scalar1=rs, scalar2=None, op0=ALU.mult),
                 reads=[src_key, stat_key], writes=[xn_key])

        def norm_b(xn, xn_key, gi, dst3, dst_key, ti):
            bk = Bank("b")
            for c in range(8):
                P.op("pe", lambda h, c=c: h.transpose(out=bk.t[:, c * 128:(c + 1) * 128], in_=xn[:, c * 128:(c + 1) * 128], identity=identb),
                     reads=[xn_key, "identb"], writes=[bk.k()])
            P.op("dve", lambda h: h.tensor_tensor(out=dst3[:, :, ti * 128:(ti + 1) * 128],
                                                  in0=bk.t[:, :].rearrange("p (c n) -> p c n", c=8), in1=gbc(gi), op=ALU.mult),
                 reads=[bk.k(), "pc"], writes=[dst_key])

        def norm_tile(src, src_key, gi, dst3, dst_key, stat, stat_key, xn, xn_key, ti):
            norm_a(src, src_key, stat, stat_key, xn, xn_key)
            norm_b(xn, xn_key, gi, dst3, dst_key, ti)

        eps_ap = carve(1)
        P.op("dve", lambda h: h.memset(eps_ap, EPS), writes=["eps"])
        one_ap = carve(1)
        P.op("dve", lambda h: h.memset(one_ap, 1.0), writes=["one"])
        base_top = top[0]

        if stop_after == "S":
            P.barrier()
            P.op("sp", lambda h: h.dma_start(out=dbg_d[:, 0:16], in_=hp), dma=True)
            P.op("sp", lambda h: h.dma_start(out=dbg_d[:, 16:1060], in_=rows), dma=True)
            P.op("sp", lambda h: h.dma_start(out=dbg_d[:, 1060:1220], in_=wrg.rearrange("p c n -> p (c n)")), dma=True)
            nhalves = 0

        wrb = carve(80, BF16, "p (c n) -> p c n", c=8)
        P.op("dve", lambda h: h.tensor_copy(out=wrb, in_=wr), reads=["wr"], writes=["wrb"])
        base_top = top[0]

        def carve_at(off, words, dtype=F32, pattern=None, **kw):
            sv = top[0]
            top[0] = off
            ap = carve(words, dtype, pattern, **kw)
            top[0] = sv
            return ap

        def do_half(hf):
            top[0] = base_top
            bufA = carve(8192, BF16, "p (c n) -> p c n", c=8)
            bufY_off = top[0]
            bufY = carve(8192, BF16, "p (c n) -> p c n", c=8)
            xres_off = top[0]
            xres = carve(16384, F32, "p (i n) -> p i n", i=16)
            rstd3 = carve(32, F32, "p (i n) -> p i n", i=16)
            RT0 = top[0]
            tok0 = hf * SEQ

            def akey(i):
                return "A.%d" % i

            def ykey(c, j):
                return "Y.%d.%d" % (c, j)

            def xkey(i):
                return "X.%d" % i

            win = carve_at(xres_off, 14336, BF16, "p (c n) -> p c n", c=8)
            vtok = carve_at(xres_off + 14336, 2048, BF16, "p (c n) -> p c n", c=8)
            for s in (0, 1, 2, 5, 4, 3, 6):
                P.op("pool", lambda h, s=s: h.dma_start(out=win[:, :, s * 512:(s + 1) * 512], in_=wview(w_in_d, s * 512, (s + 1) * 512)),
                     writes=["win%d" % s], dma=True)

            top[0] = RT0
            xs = [carve(1024) for _ in range(3)]
            xn3 = [carve(512, BF16) for _ in range(3)]
            for i in range(NT + 1):
                if i < NT:
                    xk = "xs%d" % (i % 3)
                    P.op("sp", lambda h, i=i: h.dma_start(out=xs[i % 3], in_=x_d[tok0 + i * 128:tok0 + (i + 1) * 128, :]),
                         writes=[xk], dma=True)
                    norm_a(xs[i % 3], xk, rstd3[:, i, :], "st.%d" % i, xn3[i % 3], "xn%d" % (i % 3))
                if i >= 1:
                    norm_b(xn3[(i - 1) % 3], "xn%d" % ((i - 1) % 3), 0, bufA, akey(i - 1), i - 1)
            P.barrier()
            if stop_after == "A1":
                for c in range(8):
                    P.op("pool", lambda h, c=c: h.dma_start(out=dbg_d[:, c * 1024:(c + 1) * 1024], in_=bufA[:, c, 0:1024]), dma=True)
                return True

            top[0] = RT0
            wout = carve(4096, BF16, "p (c n) -> p c n", c=8)
            P.op("pool", lambda h: h.dma_start(out=wout, in_=wview(w_out_d, 0, D)), writes=["wout"], dma=True)
            Sst = carve(512, F32, "p (h n) -> p h n", h=4)
            ubuf = carve(4 * 514, F32, "p (c n) -> p c n", c=4)
            P.op("pool", lambda h: h.memset(Sst.rearrange("p h n -> p (h n)"), 0.0), writes=["S0", "S1", "S2", "S3"])
            P.op("pool", lambda h: h.memset(ubuf.rearrange("p c n -> p (c n)"), 0.0), writes=["u0", "u1", "u2", "u3"])
            f_t1 = carve(512)
            f_t2 = carve(512)
            DSb = [carve(64, BF16) for _ in range(8)]
            NSET = 2
            TS = []
            for q in range(NSET):
                TS.append(dict(
                    e=carve(512), l2=carve(512), l1=carve(512), lnk=carve(512), d=carve(512), dec=carve(8),
                    qt=carve(256, BF16), khT=carve(256, BF16),
                    khtok=carve(512, BF16, "p (c n) -> p c n", c=8), scm=carve(256, BF16, "p (c n) -> p c n", c=8)))

            def conv_chunk(j, cch):
                T0 = j * 512
                rdA = [akey(4 * j + t) for t in range(4)]
                th = []
                bks = [None, None, None]

                def mm(s):
                    def f():
                        bks[s] = Bank("f")
                        bk = bks[s]
                        for c in range(8):
                            P.op("pe", lambda h, c=c: h.matmul(
                                bk.t[:, :], lhsT=win[:, c, s * 512 + cch * 128:s * 512 + (cch + 1) * 128],
                                rhs=bufA[:, c, T0:T0 + 512], start=(c == 0), stop=(c == 7)),
                                reads=rdA + ["win%d" % s], writes=[bk.k()])
                    return f
                uk = "u%d" % cch
                cw0 = PC_CONV + cch * 3

                def ew1():
                    if j > 0:
                        P.op("pool", lambda h: h.tensor_copy(out=ubuf[:, cch, 0:2], in_=ubuf[:, cch, 512:514]), reads=[uk], writes=[uk])
                    P.op("act", lambda h: h.activation(out=ubuf[:, cch, 2:514], in_=bks[1].t[:, :], func=AF.Copy), reads=[bks[1].k()], writes=[uk])

                def ew2():
                    P.op("dve", lambda h: h.tensor_tensor(out=ubuf[:, cch, 2:514], in0=bks[2].t[:, :], in1=ubuf[:, cch, 2:514], op=ALU.mult),
                         reads=[bks[2].k(), uk], writes=[uk])
                    P.op("dve", lambda h: h.tensor_scalar(out=f_t1, in0=ubuf[:, cch, 2:514], scalar1=pc[:, cw0 + 2:cw0 + 3],
                                                          scalar2=None, op0=ALU.mult), reads=[uk, "pc"], writes=["f_t1"])
                    P.op("dve", lambda h: h.scalar_tensor_tensor(out=f_t2, in0=ubuf[:, cch, 1:513], scalar=pc[:, cw0 + 1:cw0 + 2],
                                                                 in1=f_t1, op0=ALU.mult, op1=ALU.add), reads=[uk, "pc", "f_t1"], writes=["f_t2"])
                    P.op("dve", lambda h: h.scalar_tensor_tensor(out=f_t1, in0=ubuf[:, cch, 0:512], scalar=pc[:, cw0:cw0 + 1],
                                                                 in1=f_t2, op0=ALU.mult, op1=ALU.add), reads=[uk, "pc", "f_t2"], writes=["f_t1"])

                def ew3():
                    P.op("dve", lambda h: h.tensor_tensor(out=bufY[:, cch, T0:T0 + 512], in0=bks[0].t[:, :], in1=f_t1, op=ALU.mult),
                         reads=[bks[0].k(), "f_t1"], writes=[ykey(cch, j)])
                def seq(*fs):
                    def f():
                        for g_ in fs:
                            g_()
                    return f
                return [seq(mm(1), ew1), seq(mm(2), ew2), seq(mm(0), ew3)]

            def v_group(j, c8):
                T0 = j * 512
                rdA = [akey(4 * j + t) for t in range(4)]

                def f():
                    bk = Bank("f")
                    for c in range(8):
                        P.op("pe", lambda h, c=c: h.matmul(
                            bk.t[0:64, :], lhsT=bufA[:, c, T0 + c8 * 64:T0 + (c8 + 1) * 64], rhs=win[:, c, 5 * 512:6 * 512],
                            start=(c == 0), stop=(c == 7)), reads=rdA + ["win5"], writes=[bk.k()])
                    P.op("act", lambda h: h.activation(out=vtok[0:64, c8, :], in_=bk.t[0:64, :], func=AF.Copy),
                         reads=[bk.k()], writes=["vtok%d" % c8])
                return f

            def head_unit(j, hd, q):
                T0 = j * 512
                rdA = [akey(4 * j + t) for t in range(4)]
                t = TS[q]
                K = lambda n: "%s.%d" % (n, q)
                st_ = {}
                lbh = hp[:, hd:hd + 1]
                lnomlh = hp[:, 8 + hd:9 + hd]
                b3 = t["e"].rearrange("p (c n) -> p c n", c=8)
                sk = "S%d" % hd
                E = []

                def proj(name, s):
                    def f():
                        bk = Bank("f")
                        st_[name] = bk
                        for c in range(8):
                            P.op("pe", lambda h, c=c: h.matmul(
                                bk.t[:, :], lhsT=win[:, c, s * 512 + hd * 128:s * 512 + (hd + 1) * 128],
                                rhs=bufA[:, c, T0:T0 + 512], start=(c == 0), stop=(c == 7)),
                                reads=rdA + ["win%d" % s], writes=[bk.k()])
                    return f
                pz_ = proj("z", 4)

                def e1():
                    bz = st_["z"]
                    P.op("act", lambda h: h.activation(out=t["e"], in_=bz.t[:, :], func=AF.Exp, scale=-1.0), reads=[bz.k()], writes=[K("e")])
                    P.op("act", lambda h: h.activation(out=t["l2"], in_=t["e"], func=AF.Ln, bias=one_ap), reads=[K("e"), "one"], writes=[K("l2")])
                    P.op("act", lambda h: h.activation(out=t["l1"], in_=t["e"], func=AF.Ln, scale=lbh, bias=one_ap),
                         reads=[K("e"), "one", "hp_lb"], writes=[K("l1")])

                def e2():
                    bz = st_["z"]
                    P.op("pool", lambda h: h.tensor_tensor(out=t["l1"], in0=t["l1"], in1=t["l2"], op=ALU.subtract), reads=[K("l1"), K("l2")], writes=[K("l1")])
                    P.op("dve", lambda h: h.scalar_tensor_tensor(out=t["lnk"], in0=bz.t[:, :], scalar=-1.0, in1=t["l2"], op0=ALU.mult, op1=ALU.subtract),
                         reads=[bz.k(), K("l2")], writes=[K("lnk")])
                pq_ = proj("q", 3)

                def e3():
                    P.op("dve", lambda h: h.tensor_tensor_scan(out=t["e"], data0=rmask, data1=t["l1"], initial=0.0, op0=ALU.mult, op1=ALU.add),
                         reads=["cn", K("l1"), K("e")], writes=[K("e")])
                    P.op("pool", lambda h: h.tensor_tensor(out=t["d"].rearrange("p (c n) -> p c n", c=8), in0=b3,
                                                           in1=b3[:, :, 63:64].to_broadcast([128, 8, 64]), op=ALU.subtract),
                         reads=[K("e")], writes=[K("d")])

                def e4():
                    bq = st_["q"]
                    P.op("act", lambda h: h.activation(out=t["l2"], in_=t["d"], func=AF.Exp), reads=[K("d"), K("l2")], writes=[K("l2")])
                    P.op("dve", lambda h: h.tensor_tensor(out=t["qt"], in0=bq.t[:, :], in1=t["l2"], op=ALU.mult), reads=[bq.k(), K("l2")], writes=[K("qt")])
                pg_ = proj("g", 6)

                def e5():
                    P.op("pool", lambda h: h.tensor_tensor(out=t["lnk"], in0=t["lnk"], in1=t["d"], op=ALU.subtract), reads=[K("lnk"), K("d")], writes=[K("lnk")])
                    P.op("act", lambda h: h.activation(out=t["khT"], in_=t["lnk"], func=AF.Exp, bias=lnomlh),
                         reads=[K("lnk"), "hp_ln"], writes=[K("khT")])
                    P.op("act", lambda h: h.activation(out=t["dec"], in_=b3[:, :, 63], func=AF.Exp), reads=[K("e")], writes=[K("dec")])

                def e6():
                    bg = st_["g"]
                    P.op("act", lambda h: h.activation(out=t["l1"], in_=bg.t[:, :], func=AF.Exp, scale=-1.0), reads=[bg.k(), K("l1")], writes=[K("l1")])
                    P.op("act", lambda h: h.activation(out=t["l1"], in_=t["l1"], func=AF.Ln, bias=one_ap), reads=[K("l1"), "one"], writes=[K("l1")])
                    P.op("act", lambda h: h.activation(out=t["l1"], in_=t["l1"], func=AF.Exp, scale=-1.0), reads=[K("l1")], writes=[K("l1")])
                    P.op("dve", lambda h: h.tensor_tensor(out=t["l1"], in0=bg.t[:, :], in1=t["l1"], op=ALU.mult), reads=[bg.k(), K("l1")], writes=[K("l1")])

                def grp(*fs):
                    def f():
                        for g_ in fs:
                            g_()
                    return f
                E.append(grp(pz_, e1, e2))
                E.append(grp(pq_, e3, e4))
                E.append(grp(pg_, e5, e6))

                C = []

                def c0():
                    bkk = Bank("b")
                    for c8 in range(8):
                        P.op("pe", lambda h, c8=c8: h.transpose(out=bkk.t[0:64, c8 * 128:(c8 + 1) * 128], in_=t["khT"][:, c8 * 64:(c8 + 1) * 64],
                                                               identity=identb), reads=[K("khT"), "identb"], writes=[bkk.k()])
                    P.op("act", lambda h: h.activation(out=t["khtok"][0:64, :, :].rearrange("p c n -> p (c n)"), in_=bkk.t[0:64, :], func=AF.Copy),
                         reads=[bkk.k()], writes=[K("khtok")])
                    st_["o"] = Bank("f", hold=True)
                    st_["sc"] = Bank("f", hold=True)
                C.append(c0)

                def c1():
                    bsc = st_["sc"]
                    st_["ds0"] = Bank("f", hold=True)
                    st_["ds1"] = Bank("f", hold=True)
                    for c8 in range(8):
                        cs = slice(c8 * 64, (c8 + 1) * 64)
                        P.op("pe", lambda h, cs=cs: h.matmul(bsc.t[0:64, cs], lhsT=t["khT"][:, cs], rhs=t["qt"][:, cs], start=True, stop=True),
                             reads=[K("khT"), K("qt")], writes=[bsc.k(c8)])
                    for c8 in range(8):
                        bds = st_["ds%d" % (c8 // 4)]
                        ds_cols = slice((c8 % 4) * 128, (c8 % 4 + 1) * 128)
                        P.op("pe", lambda h, c8=c8, bds=bds, ds_cols=ds_cols: h.matmul(bds.t[:, ds_cols], lhsT=t["khtok"][0:64, c8, :],
                                                                                        rhs=vtok[0:64, c8, hd * 128:(hd + 1) * 128], start=True, stop=True),
                             reads=[K("khtok"), "vtok%d" % c8], writes=[bds.k(c8 % 4)])
                C.append(c1)

                def c2():
                    bsc = st_["sc"]
                    for c8 in range(8):
                        cs = slice(c8 * 64, (c8 + 1) * 64)
                        P.op("dve", lambda h, c8=c8, cs=cs: h.tensor_tensor(out=t["scm"][0:64, c8, :], in0=bsc.t[0:64, cs], in1=maskb[0:64, :], op=ALU.mult),
                             reads=[bsc.k(c) for c in range(8)] + ["maskb"], writes=[K("scm%d" % c8)])
                    for c8 in range(8):
                        bds = st_["ds%d" % (c8 // 4)]
                        ds_cols = slice((c8 % 4) * 128, (c8 % 4 + 1) * 128)
                        P.op("dve", lambda h, c8=c8: h.tensor_scalar(out=DSb[c8], in0=Sst[:, hd, :], scalar1=t["dec"][:, c8:c8 + 1], scalar2=None, op0=ALU.mult),
                             reads=[sk, K("dec")], writes=["DSb%d" % c8])
                        P.op("dve", lambda h, c8=c8, bds=bds, ds_cols=ds_cols: h.scalar_tensor_tensor(
                            out=Sst[:, hd, :], in0=Sst[:, hd, :], scalar=t["dec"][:, c8:c8 + 1], in1=bds.t[:, ds_cols], op0=ALU.mult, op1=ALU.add),
                            reads=[sk, K("dec")] + [bds.k(c) for c in range(4)], writes=[sk])
                    st_["sc"].done()
                    st_["ds0"].done()
                    st_["ds1"].done()
                C.append(c2)

                def c3():
                    bo = st_["o"]
                    for c8 in range(8):
                        cs = slice(c8 * 64, (c8 + 1) * 64)
                        P.op("pe", lambda h, c8=c8, cs=cs: h.matmul(bo.t[:, cs], lhsT=DSb[c8], rhs=t["qt"][:, cs], start=True, stop=False),
                             reads=["DSb%d" % c8, K("qt")], writes=[bo.k(c8)])
                        P.op("pe", lambda h, c8=c8, cs=cs: h.matmul(bo.t[:, cs], lhsT=vtok[0:64, c8, hd * 128:(hd + 1) * 128], rhs=t["scm"][0:64, c8, :],
                                                                    start=False, stop=True),
                             reads=["vtok%d" % c8, K("scm%d" % c8)], writes=[bo.k(c8)])
                C.append(c3)

                def c9():
                    bo = st_["o"]
                    okeys = [bo.k(c8) for c8 in range(8)]
                    P.op("act", lambda h: h.activation(out=t["khT"], in_=bo.t[:, :], func=AF.Square), reads=okeys + [K("khT")], writes=[K("khT")])
                    bss = Bank("f")
                    P.op("pe", lambda h: h.matmul(bss.t[:, :], lhsT=onesb, rhs=t["khT"], start=True, stop=True), reads=["onesb", K("khT")], writes=[bss.k()])
                    P.op("act", lambda h: h.activation(out=t["l2"], in_=bss.t[:, :], func=AF.Ln, scale=1.0 / 128, bias=eps_ap),
                         reads=[bss.k(), "eps", K("l2")], writes=[K("l2")])
                    P.op("act", lambda h: h.activation(out=t["l2"], in_=t["l2"], func=AF.Exp, scale=-0.5), reads=[K("l2")], writes=[K("l2")])
                    P.op("dve", lambda h: h.tensor_tensor(out=t["d"], in0=bo.t[:, :], in1=t["l2"], op=ALU.mult), reads=okeys + [K("l2"), K("d")], writes=[K("d")])
                    P.op("dve", lambda h: h.scalar_tensor_tensor(out=bufY[:, 4 + hd, T0:T0 + 512], in0=t["d"], scalar=pc[:, PC_HN + hd:PC_HN + hd + 1],
                                                                 in1=t["l1"], op0=ALU.mult, op1=ALU.mult),
                         reads=[K("d"), K("l1"), "pc"], writes=[ykey(4 + hd, j)])
                    bo.done()
                C.append(c9)
                return E, C

            def interleave(primary, fillers):
                n, m = len(primary), len(fillers)
                fi = 0
                for i, f in enumerate(primary):
                    f()
                    want = ((i + 1) * m) // max(n, 1)
                    while fi < want:
                        fillers[fi]()
                        fi += 1
                while fi < m:
                    fillers[fi]()
                    fi += 1

            for cch in range(4):
                for f in conv_chunk(0, cch):
                    f()
            for c8 in range(8):
                v_group(0, c8)()
            units = [(j, hd) for j in range(NJ) for hd in range(4)]
            E0, Cprev = head_unit(0, 0, 0)
            for f in E0:
                f()
            for u in range(len(units)):
                j, hd = units[u]
                fill = []
                if u + 1 < len(units):
                    jn, hn = units[u + 1]
                    En, Cn = head_unit(jn, hn, (u + 1) % NSET)
                    if jn == j:
                        fill += En
                else:
                    En, Cn = [], []
                if j + 1 < NJ:
                    fill += conv_chunk(j + 1, hd)
                for f in Cprev[0:3]:
                    f()
                for f in fill:
                    f()
                for f in Cprev[3:]:
                    f()
                if u + 1 < len(units) and units[u + 1][0] != j:
                    for c8 in range(8):
                        v_group(j + 1, c8)()
                    for f in En:
                        f()
                Cprev = Cn
            P.barrier()
            if stop_after == "A2":
                for c in (4, 5, 6, 7, 0, 1, 2, 3):
                    for jj in range(2):
                        P.op("pool", lambda h, c=c, jj=jj: h.dma_start(out=dbg_d[:, c * 1024 + jj * 512:c * 1024 + (jj + 1) * 512],
                                                                      in_=bufY[:, c, jj * 512:(jj + 1) * 512]), dma=True)
                return True

            top[0] = RT0
            wout = carve(4096, BF16, "p (c n) -> p c n", c=8)
            xsb = [carve(1024) for _ in range(3)]
            xnb = [carve(512, BF16) for _ in range(3)]
            wq_off = top[0]
            wq = carve(4096, BF16, "p (c n) -> p c n", c=8)
            wo = carve(4096, BF16, "p (c n) -> p c n", c=8)
            P.op("pool", lambda h: h.dma_start(out=wq, in_=wview(w_q_d, 0, D)), writes=["wq"], dma=True)
            P.op("pool", lambda h: h.dma_start(out=wo, in_=wview(w_o_d, 0, D)), writes=["wo"], dma=True)

            def b1_stage1(i):
                xk = "xsb%d" % (i % 3)
                P.op("sp", lambda h: h.dma_start(out=xsb[i % 3], in_=x_d[tok0 + i * 128:tok0 + (i + 1) * 128, :]), writes=[xk], dma=True)
                for n2 in range(2):
                    bk = Bank("f")
                    for c in range(8):
                        P.op("pe", lambda h, c=c, n2=n2, bk=bk: h.matmul(bk.t[:, :], lhsT=bufY[:, c, i * 128:(i + 1) * 128],
                                                                         rhs=wout[:, c, n2 * 512:(n2 + 1) * 512], start=(c == 0), stop=(c == 7)),
                             reads=["wout"], writes=[bk.k()])
                    P.op("dve", lambda h, n2=n2, bk=bk: h.tensor_tensor(out=xres[:, i, n2 * 512:(n2 + 1) * 512], in0=bk.t[:, :],
                                                                        in1=xsb[i % 3][:, n2 * 512:(n2 + 1) * 512], op=ALU.add),
                         reads=[bk.k(), xk], writes=[xkey(i)])

            b1_stage1(0)
            b1_stage1(1)
            for i in range(NT + 1):
                if i + 2 < NT:
                    b1_stage1(i + 2)
                if i < NT:
                    norm_a(xres[:, i, :], xkey(i), rstd3[:, i, :], "st.%d" % i, xnb[i % 3], "xnb%d" % (i % 3))
                if i >= 1:
                    norm_b(xnb[(i - 1) % 3], "xnb%d" % ((i - 1) % 3), 1, bufA, akey(i - 1), i - 1)
            P.barrier()
            if stop_after == "B1":
                for i in range(8):
                    P.op("pool", lambda h, i=i: h.dma_start(out=dbg_d[:, i * 1024:(i + 1) * 1024], in_=xres[:, i, :]), dma=True)
                return True

            KT = carve_at(RT0, 1024, BF16, "p (c n) -> p c n", c=8)
            Vt = carve_at(RT0 + 1024, 1024, BF16, "p (c n) -> p c n", c=2)
            mstat = carve_at(RT0 + 2048, 4)
            wq = carve_at(wq_off, 4096, BF16, "p (c n) -> p c n", c=8)
            wo = carve_at(wq_off + 4096, 4096, BF16, "p (c n) -> p c n", c=8)
            top[0] = bufY_off
            wkvb = [carve(2048, BF16, "p (c n) -> p c n", c=8) for _ in range(2)]
            mT = carve(1024, BF16, "p (c n) -> p c n", c=8)
            mst = [carve(1024) for _ in range(2)]
            xn2 = [carve(512, BF16) for _ in range(2)]
            assert top[0] <= bufY_off + 8192
            for t in range(2):
                P.op("sp", lambda h, t=t: h.dma_start(out=mst[t], in_=mem_d[hf * 256 + t * 128:hf * 256 + (t + 1) * 128, :]),
                     writes=["mst%d" % t], dma=True)
            for blk in range(2):
                P.op("pool", lambda h, blk=blk: h.dma_start(out=wkvb[blk], in_=wview(w_kv_d, blk * 512, (blk + 1) * 512)), writes=["wkv%d" % blk], dma=True)
            for t in range(2):
                norm_tile(mst[t], "mst%d" % t, 2, mT, "mT", mstat[:, 2 * t:2 * t + 2], "mstat%d" % t, xn2[t], "xn%d" % t, t)
            for blk in range(4):
                wb = wkvb[blk % 2]
                wk = "wkv%d" % (blk % 2)
                if blk >= 2:
                    P.op("pool", lambda h, blk=blk, wb=wb: h.dma_start(out=wb, in_=wview(w_kv_d, blk * 512, (blk + 1) * 512)), writes=[wk], dma=True)
                if blk < 2:
                    for e4 in range(4):
                        ec = blk * 4 + e4
                        bk = Bank("f")
                        for c in range(8):
                            P.op("pe", lambda h, c=c, e4=e4, wb=wb, bk=bk: h.matmul(bk.t[:, 0:256], lhsT=wb[:, c, e4 * 128:(e4 + 1) * 128], rhs=mT[:, c, :],
                                                                                   start=(c == 0), stop=(c == 7)), reads=[wk, "mT"], writes=[bk.k()])
                        P.op("act", lambda h, ec=ec, bk=bk: h.activation(out=KT[:, ec, :], in_=bk.t[:, 0:256], func=AF.Copy), reads=[bk.k()], writes=["KT"])
                else:
                    n2 = blk - 2
                    for mc in range(2):
                        bk = Bank("f")
                        for c in range(8):
                            P.op("pe", lambda h, c=c, mc=mc, wb=wb, bk=bk: h.matmul(bk.t[:, :], lhsT=mT[:, c, mc * 128:(mc + 1) * 128], rhs=wb[:, c, :],
                                                                                   start=(c == 0), stop=(c == 7)), reads=[wk, "mT"], writes=[bk.k()])
                        P.op("act", lambda h, mc=mc, n2=n2, bk=bk: h.activation(out=Vt[:, mc, n2 * 512:(n2 + 1) * 512], in_=bk.t[:, :], func=AF.Copy),
                             reads=[bk.k()], writes=["Vt"])
            P.barrier()
            top[0] = bufY_off
            QT = carve(2048, BF16, "p (c n) -> p c n", c=8)
            OT = carve(2048, BF16, "p (c n) -> p c n", c=8)
            ET = [carve(512, BF16, "p (c n) -> p c n", c=2) for _ in range(2)]
            rden = [carve(512) for _ in range(2)]

            def q_proj(j):
                T0 = j * 512
                rdA = [akey(4 * j + t) for t in range(4)]
                for ec in range(8):
                    bk = Bank("f")
                    for c in range(8):
                        P.op("pe", lambda h, c=c, ec=ec, bk=bk: h.matmul(bk.t[:, :], lhsT=wq[:, c, ec * 128:(ec + 1) * 128], rhs=bufA[:, c, T0:T0 + 512],
                                                                        start=(c == 0), stop=(c == 7)), reads=rdA + ["wq"], writes=[bk.k()])
                    if ec % 2 == 0:
                        P.op("act", lambda h, ec=ec, bk=bk: h.activation(out=QT[:, ec, :], in_=bk.t[:, :], func=AF.Copy, scale=1.0 / 16.0),
                             reads=[bk.k()], writes=["QT%d" % ec])
                    else:
                        P.op("dve", lambda h, ec=ec, bk=bk: h.tensor_scalar(out=QT[:, ec, :], in0=bk.t[:, :], scalar1=1.0 / 16.0, scalar2=None, op0=ALU.mult),
                             reads=[bk.k()], writes=["QT%d" % ec])

            def s_exp(hd):
                et = ET[hd % 2]
                ek = "ET%d" % (hd % 2)
                for mc in range(2):
                    bk = Bank("f")
                    for k2 in range(2):
                        P.op("pe", lambda h, mc=mc, k2=k2, bk=bk: h.matmul(bk.t[:, :], lhsT=KT[:, 2 * hd + k2, mc * 128:(mc + 1) * 128],
                                                                          rhs=QT[:, 2 * hd + k2, :], start=(k2 == 0), stop=(k2 == 1)),
                             reads=["KT", "QT%d" % (2 * hd + k2)], writes=[bk.k()])
                    P.op("act", lambda h, mc=mc, bk=bk: h.activation(out=et[:, mc, :], in_=bk.t[:, :], func=AF.Exp), reads=[bk.k()], writes=[ek + ".%d" % mc])

            def pv(hd):
                et = ET[hd % 2]
                ek = "ET%d" % (hd % 2)
                bden = Bank("f")
                for mc in range(2):
                    P.op("pe", lambda h, mc=mc: h.matmul(bden.t[:, :], lhsT=onesb, rhs=et[:, mc, :], start=(mc == 0), stop=(mc == 1)),
                         reads=["onesb", ek + ".%d" % mc], writes=[bden.k()])
                rd = rden[hd % 2]
                rk = "rden%d" % (hd % 2)
                P.op("dve", lambda h: h.reciprocal(out=rd, in_=bden.t[:, :]), reads=[bden.k()], writes=[rk])
                for k2 in range(2):
                    bk = Bank("f")
                    for mc in range(2):
                        P.op("pe", lambda h, mc=mc, k2=k2, bk=bk: h.matmul(
                            bk.t[:, :], lhsT=Vt[:, mc, (2 * hd + k2) * 128:(2 * hd + k2 + 1) * 128], rhs=et[:, mc, :], start=(mc == 0), stop=(mc == 1)),
                            reads=["Vt", ek + ".%d" % mc], writes=[bk.k()])
                    P.op("dve", lambda h, k2=k2, bk=bk: h.tensor_tensor(out=OT[:, 2 * hd + k2, :], in0=bk.t[:, :], in1=rd, op=ALU.mult),
                         reads=[bk.k(), rk], writes=["OT%d" % (2 * hd + k2)])

            def w_o(j):
                for tt in range(4):
                    i = 4 * j + tt
                    for n2 in range(2):
                        bk = Bank("f")
                        for ec in range(8):
                            P.op("pe", lambda h, ec=ec, tt=tt, n2=n2, bk=bk: h.matmul(bk.t[:, :], lhsT=OT[:, ec, tt * 128:(tt + 1) * 128],
                                                                                     rhs=wo[:, ec, n2 * 512:(n2 + 1) * 512], start=(ec == 0), stop=(ec == 7)),
                                 reads=["OT%d" % ec, "wo"], writes=[bk.k()])
                        P.op("dve", lambda h, i=i, n2=n2, bk=bk: h.tensor_tensor(out=xres[:, i, n2 * 512:(n2 + 1) * 512], in0=bk.t[:, :],
                                                                                 in1=xres[:, i, n2 * 512:(n2 + 1) * 512], op=ALU.add),
                             reads=[bk.k(), xkey(i)], writes=[xkey(i)])

            q_proj(0)
            for j in range(NJ):
                s_exp(0)
                for hd in range(4):
                    if hd + 1 < 4:
                        s_exp(hd + 1)
                    pv(hd)
                if j + 1 < NJ:
                    q_proj(j + 1)
                w_o(j)
            P.barrier()
            if stop_after == "B2":
                for i in range(8):
                    P.op("pool", lambda h, i=i: h.dma_start(out=dbg_d[:, i * 1024:(i + 1) * 1024], in_=xres[:, i, :]), dma=True)
                return True

            top[0] = bufY_off
            wg = [carve(2048, BF16, "p (c n) -> p c n", c=8) for _ in range(2)]
            wu = [carve(2048, BF16, "p (c n) -> p c n", c=8) for _ in range(2)]
            top[0] = RT0
            wd = [carve(2048, BF16, "p (c n) -> p c n", c=4) for _ in range(2)]
            xn2 = [carve(512, BF16) for _ in range(2)]
            lg = carve(320, F32, "p (i n) -> p i n", i=16)
            comb = carve(256, F32, "p (i n) -> p i n", i=16)
            r_t = [carve(256) for _ in range(4)]
            r_s = [carve(64) for _ in range(6)]
            hid = [carve(1024, BF16, "p (c n) -> p c n", c=4) for _ in range(2)]
            sgb = [carve(256, BF16) for _ in range(2)]

            def wload(e):
                b = e % 2
                P.op("pool", lambda h: h.dma_start(out=wg[b], in_=w_gate_d[e].rearrange("(c p) n -> p c n", p=128)), writes=["wg%d" % b], dma=True)
                P.op("pool", lambda h: h.dma_start(out=wu[b], in_=w_up_d[e].rearrange("(c p) n -> p c n", p=128)), writes=["wu%d" % b], dma=True)
                P.op("pool", lambda h: h.dma_start(out=wd[b], in_=w_down_d[e].rearrange("(c p) n -> p c n", p=128)), writes=["wd%d" % b], dma=True)

            wload(0)
            wload(1)
            xnc = xn2 + [carve(512, BF16)]

            def router(i):
                bk = Bank("f")
                for c in range(8):
                    P.op("pe", lambda h, c=c: h.matmul(bk.t[:, 0:20], lhsT=bufA[:, c, i * 128:(i + 1) * 128], rhs=wrb[:, c, :],
                                                       start=(c == 0), stop=(c == 7)), reads=[akey(i), "wrb"], writes=[bk.k()])
                P.op("dve", lambda h: h.tensor_tensor(out=lg[:, i, :], in0=bk.t[:, 0:20], in1=rows[:, 1024:1044], op=ALU.add),
                     reads=[bk.k(), "rows"], writes=["lg"])

            for i in range(NT + 1):
                if i < NT:
                    norm_a(xres[:, i, :], xkey(i), rstd3[:, i, :], "st.%d" % i, xnc[i % 3], "xnc%d" % (i % 3))
                if i >= 1:
                    norm_b(xnc[(i - 1) % 3], "xnc%d" % ((i - 1) % 3), 3, bufA, akey(i - 1), i - 1)
                    router(i - 1)
            LG = lg[:, :, 0:4]
            LE = lg[:, :, 4:20].rearrange("p i (j k) -> p i j k", j=4)
            gmax, gsum, m1, m2, w1, w2 = [r[:, 0:16] for r in r_s]
            gsh = r_t[0][:, 0:64].rearrange("p (i j) -> p i j", i=16)
            gm = r_t[1][:, 0:64].rearrange("p (i j) -> p i j", i=16)
            tmp4 = r_t[2].rearrange("p (i j k) -> p i j k", i=16, j=4)
            esel = r_t[3][:, 0:64].rearrange("p (i k) -> p i k", i=16)
            mk1 = r_t[3][:, 64:128].rearrange("p (i k) -> p i k", i=16)
            e2 = r_t[3][:, 128:192].rearrange("p (i k) -> p i k", i=16)
            mk2 = r_t[3][:, 192:256].rearrange("p (i k) -> p i k", i=16)
            cig = r_t[0][:, 64:128].rearrange("p (i k) -> p i k", i=16)
            tq = r_t[0][:, 128:192].rearrange("p (i k) -> p i k", i=16)

            def bc3(a):
                return a.unsqueeze(2).to_broadcast([128, 16, 4])

            R = lambda fn, eng="dve": P.op(eng, fn, reads=["lg", "rt"], writes=["rt"])
            R(lambda h: h.tensor_reduce(out=gmax, in_=LG, axis=AX.X, op=ALU.max))
            R(lambda h: h.tensor_tensor(out=gsh, in0=LG, in1=bc3(gmax), op=ALU.subtract))
            R(lambda h: h.tensor_single_scalar(out=gm, in_=gsh, scalar=0.0, op=ALU.is_ge))
            R(lambda h: h.activation(out=gsh, in_=gsh, func=AF.Exp), "act")
            R(lambda h: h.tensor_reduce(out=gsum, in_=gsh, axis=AX.X, op=ALU.add))
            R(lambda h: h.reciprocal(out=gsum, in_=gsum))
            R(lambda h: h.tensor_tensor(out=tmp4, in0=LE, in1=gm.unsqueeze(3).to_broadcast([128, 16, 4, 4]), op=ALU.mult))
            R(lambda h: h.tensor_reduce(out=esel, in_=tmp4.rearrange("p i j k -> p i k j"), axis=AX.X, op=ALU.add))
            R(lambda h: h.tensor_reduce(out=m1, in_=esel, axis=AX.X, op=ALU.max))
            R(lambda h: h.tensor_tensor(out=mk1, in0=esel, in1=bc3(m1), op=ALU.is_ge))
            R(lambda h: h.scalar_tensor_tensor(out=e2, in0=mk1, scalar=-1e30, in1=esel, op0=ALU.mult, op1=ALU.add))
            R(lambda h: h.tensor_reduce(out=m2, in_=e2, axis=AX.X, op=ALU.max))
            R(lambda h: h.tensor_tensor(out=mk2, in0=e2, in1=bc3(m2), op=ALU.is_ge))
            R(lambda h: h.tensor_tensor(out=w2, in0=m2, in1=m1, op=ALU.subtract))
            R(lambda h: h.activation(out=w2, in_=w2, func=AF.Exp), "act")
            R(lambda h: h.tensor_scalar_add(out=w1, in0=w2, scalar1=1.0))
            R(lambda h: h.reciprocal(out=w1, in_=w1))
            R(lambda h: h.tensor_tensor(out=w2, in0=w2, in1=w1, op=ALU.mult))
            R(lambda h: h.tensor_tensor(out=w1, in0=w1, in1=gsum, op=ALU.mult))
            R(lambda h: h.tensor_tensor(out=w2, in0=w2, in1=gsum, op=ALU.mult))
            R(lambda h: h.tensor_tensor(out=cig, in0=mk1, in1=bc3(w1), op=ALU.mult))
            R(lambda h: h.tensor_tensor(out=tq, in0=mk2, in1=bc3(w2), op=ALU.mult))
            R(lambda h: h.tensor_tensor(out=cig, in0=cig, in1=tq, op=ALU.add))
            P.op("dve", lambda h: h.tensor_tensor(out=comb.rearrange("p i (j k) -> p i j k", j=4), in0=gm.unsqueeze(3).to_broadcast([128, 16, 4, 4]),
                                                  in1=cig.unsqueeze(2).to_broadcast([128, 16, 4, 4]), op=ALU.mult), reads=["rt"], writes=["comb"])
            if stop_after == "R":
                P.barrier()
                P.op("pool", lambda h: h.dma_start(out=dbg_d[:, 0:256], in_=comb.rearrange("p i n -> p (i n)")), dma=True)
                P.op("pool", lambda h: h.dma_start(out=dbg_d[:, 256:576], in_=lg.rearrange("p i n -> p (i n)")), dma=True)
                return True

            units = [(e, j) for e in range(NEXP) for j in range(NJ)]

            def gate_up(u):
                e, j = units[u]
                b = e % 2
                T0 = j * 512
                rdA = [akey(4 * j + t) for t in range(4)]
                hb = hid[u % 2]
                for f in range(4):
                    bg_, bu_ = Bank("f"), Bank("f")
                    for (bk, w, wk) in ((bg_, wg[b], "wg%d" % b), (bu_, wu[b], "wu%d" % b)):
                        for c in range(8):
                            P.op("pe", lambda h, c=c, f=f, bk=bk, w=w, T0=T0: h.matmul(bk.t[:, :], lhsT=w[:, c, f * 128:(f + 1) * 128], rhs=bufA[:, c, T0:T0 + 512],
                                                                                start=(c == 0), stop=(c == 7)), reads=rdA + [wk], writes=[bk.k()])
                    sg = sgb[f % 2]
                    P.op("act", lambda h, sg=sg, bg_=bg_: h.activation(out=sg, in_=bg_.t[:, :], func=AF.Silu), reads=[bg_.k()], writes=["sgb%d" % (f % 2)])
                    P.op("dve", lambda h, f=f, sg=sg, bu_=bu_, hb=hb: h.tensor_tensor(out=hb[:, f, :], in0=bu_.t[:, :], in1=sg, op=ALU.mult),
                         reads=[bu_.k(), "sgb%d" % (f % 2)], writes=["hid%d.%d" % (u % 2, f)])

            def down(u):
                e, j = units[u]
                b = e % 2
                hb = hid[u % 2]
                for tt in range(4):
                    i = 4 * j + tt
                    for n2 in range(2):
                        bk = Bank("f")
                        for f in range(4):
                            P.op("pe", lambda h, f=f, tt=tt, n2=n2, bk=bk: h.matmul(bk.t[:, :], lhsT=hb[:, f, tt * 128:(tt + 1) * 128],
                                                                                   rhs=wd[b][:, f, n2 * 512:(n2 + 1) * 512], start=(f == 0), stop=(f == 3)),
                                 reads=["hid%d.%d" % (u % 2, f), "wd%d" % b], writes=[bk.k()])
                        P.op("dve", lambda h, i=i, n2=n2, bk=bk: h.scalar_tensor_tensor(
                            out=xres[:, i, n2 * 512:(n2 + 1) * 512], in0=bk.t[:, :], scalar=comb[:, i, e:e + 1],
                            in1=xres[:, i, n2 * 512:(n2 + 1) * 512], op0=ALU.mult, op1=ALU.add),
                            reads=[bk.k(), "comb", xkey(i)], writes=[xkey(i)])

            for u in range(len(units) + 1):
                if u < len(units):
                    gate_up(u)
                if u >= 1:
                    down(u - 1)
                    e, j = units[u - 1]
                    if j == NJ - 1 and e + 2 < NEXP:
                        wload(e + 2)
            fin = rows[:, 0:1024]
            for i in range(NT):
                stt = rstd3[:, i, :]
                sk_ = "st.%d" % i
                P.op("act", lambda h, i=i, stt=stt: h.activation(out=xn2[i % 2], in_=xres[:, i, :], func=AF.Square, accum_out=stt[:, 0:1]),
                     reads=[xkey(i)], writes=["xn%d" % (i % 2), sk_])
                P.op("act", lambda h, stt=stt: h.activation(out=stt[:, 1:2], in_=stt[:, 0:1], func=AF.Ln, scale=1.0 / D, bias=eps_ap), reads=[sk_, "eps"], writes=[sk_])
                P.op("act", lambda h, stt=stt: h.activation(out=stt[:, 1:2], in_=stt[:, 1:2], func=AF.Exp, scale=-0.5), reads=[sk_], writes=[sk_])
                P.op("dve", lambda h, i=i, stt=stt: h.scalar_tensor_tensor(out=xres[:, i, :], in0=xres[:, i, :], scalar=stt[:, 1:2], in1=fin, op0=ALU.mult, op1=ALU.mult),
                     reads=[xkey(i), sk_, "rows"], writes=[xkey(i)])
                P.op("sp", lambda h, i=i: h.dma_start(out=out_d[tok0 + i * 128:tok0 + (i + 1) * 128, :], in_=xres[:, i, :]),
                     reads=[xkey(i)], writes=["out.%d.%d" % (hf, i)], dma=True)
            P.barrier()
            return False

        for hf_ in range(nhalves):
            if do_half(hf_):
                break
        P.emit()
    return nc


_CACHE = {}


def _host_consts():
    cn = np.zeros((128, CN_N), np.float32)
    cn[:, CN_ID:CN_ID + 128] = np.eye(128, dtype=np.float32)
    cn[:, CN_ONE:CN_ONE + 128] = 1.0
    s = np.arange(64)[:, None]
    t = np.arange(64)[None, :]
    cn[0:64, CN_MASK:CN_MASK + 64] = (s <= t).astype(np.float32)
    rm = np.ones(512, np.float32)
    rm[::64] = 0.0
    cn[:, CN_RM:CN_RM + 512] = rm[None, :]
    return cn


def _col(v, nchunk):
    return np.ascontiguousarray(np.asarray(v, np.float32).reshape(nchunk, 128).T)


def make_in_maps(inputs):
    f = lambda k: np.asarray(inputs[k], np.float32)
    pcols = np.zeros((128, PC_N), np.float32)
    for gi, k in enumerate(["mix_norm", "xattn_norm", "mem_norm", "ffn_norm"]):
        pcols[:, PC_G + gi * 8:PC_G + gi * 8 + 8] = _col(f(k)[0], 8)
    cw = f("conv_w")[0]
    for cch in range(4):
        for jj in range(3):
            pcols[:, PC_CONV + cch * 3 + jj] = cw[jj, cch * 128:(cch + 1) * 128]
    lbr = f("hgrn_lb")
    for r in range(2):
        pcols[:, PC_LB + r * 4:PC_LB + r * 4 + 4] = _col(lbr[r], 4)
    pcols[:, PC_HN:PC_HN + 4] = _col(f("hgrn_norm")[0], 4)
    wrc = np.concatenate([f("w_group")[0], f("w_expert")[0]], axis=1)
    wr = np.ascontiguousarray(wrc.reshape(8, 128, 20).transpose(1, 0, 2).reshape(128, 160))
    rows = np.concatenate([f("final_norm").reshape(-1), f("b_group")[0], f("b_expert")[0]])[None, :].astype(np.float32)
    consts = _host_consts()
    x = f("x")
    mem = f("mem")
    shared = dict(
        w_in=np.ascontiguousarray(f("w_in")[0]), w_out=np.ascontiguousarray(f("w_out")[0]),
        w_q=np.ascontiguousarray(f("w_q")[0]), w_kv=np.ascontiguousarray(f("w_kv")[0]), w_o=np.ascontiguousarray(f("w_o")[0]),
        w_gate=np.ascontiguousarray(f("w_gate")[0]), w_up=np.ascontiguousarray(f("w_up")[0]), w_down=np.ascontiguousarray(f("w_down")[0]),
        pcols=pcols, wr=wr, rows=np.ascontiguousarray(rows), consts=consts)
    maps = []
    for c in range(NCORES):
        m = dict(shared)
        m["x"] = np.ascontiguousarray(x[2 * c:2 * c + 2].reshape(2 * SEQ, D))
        m["mem"] = np.ascontiguousarray(mem[2 * c:2 * c + 2].reshape(512, D))
        maps.append(m)
    return maps


def kernel(**inputs):
    if "nc" not in _CACHE:
        _CACHE["nc"] = build_program()
    nc = _CACHE["nc"]
    maps = make_in_maps(inputs)
    res = run_bass_kernel_spmd(nc, maps, core_ids=list(range(NCORES)))
    outs = [np.asarray(r["out"], np.float32).reshape(2, SEQ, D) for r in res.results]
    return np.concatenate(outs, axis=0)
```

```python
import contextlib
import numpy as np
import concourse.bass as bass
import concourse.mybir as mybir
from concourse.bass_utils import run_bass_kernel_spmd

F32 = mybir.dt.float32
BF16 = mybir.dt.bfloat16
I32 = mybir.dt.int32
U32 = mybir.dt.uint32
AF = mybir.ActivationFunctionType
ALU = mybir.AluOpType
AX = mybir.AxisListType

ENGS = ("pe", "act", "dve", "pool", "sp")
EPS = 1e-6
NCORES = 8
SEQ = 2048
D = 1024
NT = 16
NJ = 4
NEXP = 16


class Prog:
    NDMA_SEMS = 24

    def __init__(self, nc):
        self.nc = nc
        self.ops = []
        self.last_w = {}
        self.readers = {}
        self.dma_rr = [0, 0]
        self.dma_last = {}
        self.last_on = {}

    def alias(self, old_keys, new_key):
        s = set()
        for k in old_keys:
            w = self.last_w.get(k)
            if w is not None:
                s.add(w)
            s.update(self.readers.get(k, ()))
        self.last_w[new_key] = None
        self.readers[new_key] = list(s)

    def op(self, eng, fn, reads=(), writes=(), dma=False, extra=()):
        oid = len(self.ops)
        deps = set(extra)
        for k in reads:
            w = self.last_w.get(k)
            if w is not None:
                deps.add(w)
        for k in writes:
            w = self.last_w.get(k)
            if w is not None:
                deps.add(w)
            for r in self.readers.get(k, ()):
                deps.add(r)
        deps.discard(oid)
        rec = dict(id=oid, eng=eng, fn=fn, deps=deps, dma=dma, sig=False, dsem=None)
        if dma:
            half = self.NDMA_SEMS // 2
            q = 0 if eng == "pool" else 1
            s = q * half + (self.dma_rr[q] % half)
            self.dma_rr[q] += 1
            rec["dsem"] = s
            prev = self.dma_last.get(s)
            if prev is not None:
                deps.add(prev)
            self.dma_last[s] = oid
        self.ops.append(rec)
        if fn is not None:
            self.last_on[eng] = oid
        for k in writes:
            self.last_w[k] = oid
            self.readers[k] = []
        for k in reads:
            if k not in writes:
                self.readers.setdefault(k, []).append(oid)
        return oid

    def barrier(self):
        tails = set(self.last_on.values()) | set(self.dma_last.values())
        for e in ENGS:
            self.op(e, None, extra=set(tails))
        self.last_w = {}
        self.readers = {}

    def emit(self):
        nc = self.nc
        ops = self.ops
        for o in ops:
            if o["eng"] == "pe" and not o["dma"]:
                o["deps"] = {d for d in o["deps"] if ops[d]["dma"] or ops[d]["eng"] != "pe"}
        for o in ops:
            for d in o["deps"]:
                ops[d]["sig"] = True
        cnt = {e: 0 for e in ENGS}
        dcnt = {}
        for o in ops:
            if o["dma"]:
                s = o["dsem"]
                dcnt[s] = dcnt.get(s, 0) + 16
                o["ev"] = ("d%d" % s, dcnt[s])
            elif o["sig"]:
                cnt[o["eng"]] += 1
                o["ev"] = (o["eng"], cnt[o["eng"]])
        with contextlib.ExitStack() as st:
            sems = {}
            for e in ENGS:
                sems[e] = st.enter_context(nc.semaphore("sem_" + e))
            for s in range(self.NDMA_SEMS):
                sems["d%d" % s] = st.enter_context(nc.semaphore("sem_d%d" % s))
            block = st.enter_context(nc.Block())
            per = {e: [o for o in ops if o["eng"] == e] for e in ENGS}

            def run(e, handle):
                waited = {}
                for o in per[e]:
                    need = {}
                    for d in o["deps"]:
                        sname, val = ops[d]["ev"]
                        if need.get(sname, 0) < val:
                            need[sname] = val
                    for sname, val in need.items():
                        if waited.get(sname, 0) >= val:
                            continue
                        handle.wait_ge(sems[sname], val)
                        waited[sname] = val
                    if o["fn"] is None:
                        continue
                    ins = o["fn"](handle)
                    if o["dma"]:
                        ins.then_inc(sems[o["ev"][0]], 16)
                    elif o["sig"]:
                        ins.then_inc(sems[e], 1)

            @block.tensor
            def _(h):
                run("pe", h)

            @block.scalar
            def _(h):
                run("act", h)

            @block.vector
            def _(h):
                run("dve", h)

            @block.gpsimd
            def _(h):
                run("pool", h)

            @block.sync
            def _(h):
                run("sp", h)
                for s, v in dcnt.items():
                    h.wait_ge(sems["d%d" % s], v)


PC_G = 0
PC_CONV = 32
PC_LB = 44
PC_HN = 52
PC_N = 56
CN_ID = 0
CN_ONE = 128
CN_MASK = 256
CN_RM = 320
CN_U = 832
CN_IOTA = 960
CN_PIO = 992
CN_N = 1024


def build_program(stop_after=None, nhalves=2):
    nc = bass.Bass("TRN2", target_bir_lowering=False)

    def dram(name, shape, kind="ExternalInput"):
        return nc.dram_tensor(name, shape, F32, kind=kind).ap()

    x_d = dram("x", [2 * SEQ, D])
    mem_d = dram("mem", [512, D])
    w_in_d = dram("w_in", [D, 3584])
    w_out_d = dram("w_out", [D, D])
    w_q_d = dram("w_q", [D, D])
    w_kv_d = dram("w_kv", [D, 2 * D])
    w_o_d = dram("w_o", [D, D])
    wgl_d = [dram("wgl%d" % q, [NEXP * 128, 2048]) for q in range(2)]
    wul_d = [dram("wul%d" % q, [NEXP * 128, 2048]) for q in range(2)]
    wdl_d = [dram("wdl%d" % q, [NEXP * 128, 2048]) for q in range(2)]
    h3_d = nc.dram_tensor("h3s", [SEQ, D], BF16, kind="Internal").ap()
    tokslot_d = nc.dram_tensor("tokslot", [23 * 512, 1], I32, kind="Internal").ap()
    y_d = nc.dram_tensor("ysc", [23 * 512, D], F32, kind="Internal").ap()
    pcols_d = dram("pcols", [128, PC_N])
    wr_d = dram("wr", [128, 160])
    rows_d = dram("rows", [1, 1044])
    consts_d = dram("consts", [128, CN_N])
    out_d = dram("out", [2 * SEQ, D], kind="ExternalOutput")
    dbg_d = dram("dbg", [128, 8192], kind="ExternalOutput") if stop_after else None

    st = contextlib.ExitStack()
    with st:
        NW = 52800
        big = st.enter_context(nc.sbuf_tensor("big", [128, NW], F32))
        psf = [st.enter_context(nc.psum_tensor("psf%d" % i, [128, 512], F32)) for i in range(6)]
        psb = [st.enter_context(nc.psum_tensor("psb%d" % i, [128, 1024], BF16)) for i in range(2)]
        P = Prog(nc)

        top = [0]

        def carve(words, dtype=F32, pattern=None, **kw):
            off = top[0]
            top[0] += words
            assert top[0] <= NW, "SBUF arena overflow %d" % top[0]
            ap = big[:, off:off + words]
            if dtype == BF16:
                ap = ap.bitcast(BF16)
            if pattern:
                ap = ap.rearrange(pattern, **kw)
            return ap

        bank_state = {}
        rr = {"f": 0, "b": 0}

        held = set()

        class Bank:
            def __init__(self, kind, hold=False):
                n = 6 if kind == "f" else 2
                for _ in range(n):
                    self.idx = rr[kind] % n
                    rr[kind] += 1
                    if (kind, self.idx) not in held:
                        break
                else:
                    raise RuntimeError("all PSUM banks held")
                self.kind = kind
                self.gen = rr[kind]
                self.t = psf[self.idx] if kind == "f" else psb[self.idx]
                self.name = "%s%d" % (kind, self.idx)
                self.old = bank_state.get(self.name, [])
                self.keys = {}
                bank_state[self.name] = []
                if hold:
                    held.add((kind, self.idx))

            def done(self):
                held.discard((self.kind, self.idx))

            def k(self, sub=0):
                if sub not in self.keys:
                    key = "ps.%s.%d.%s" % (self.name, self.gen, sub)
                    P.alias(self.old, key)
                    self.keys[sub] = key
                    bank_state[self.name].append(key)
                return self.keys[sub]

        pc = carve(PC_N)
        cn = carve(CN_N)
        identb = carve(64, BF16)
        onesb = carve(64, BF16)
        maskb = carve(32, BF16)
        wr = carve(160, F32, "p (c n) -> p c n", c=8)
        wrg = carve(160, F32, "p (c n) -> p c n", c=8)
        rows = carve(1044)
        hp = carve(16)
        ident = cn[:, CN_ID:CN_ID + 128]
        rmask = cn[:, CN_RM:CN_RM + 512]
        base_top = top[0]

        P.op("sp", lambda h: h.dma_start(out=pc, in_=pcols_d), writes=["pc"], dma=True)
        P.op("sp", lambda h: h.dma_start(out=cn, in_=consts_d), writes=["cn"], dma=True)
        P.op("sp", lambda h: h.dma_start(out=wr.rearrange("p c n -> p (c n)"), in_=wr_d), writes=["wr"], dma=True)
        P.op("sp", lambda h: h.dma_start(out=rows, in_=rows_d.partition_broadcast(128)), writes=["rows"], dma=True)
        P.op("dve", lambda h: h.tensor_copy(out=identb, in_=cn[:, CN_ID:CN_ID + 128]), reads=["cn"], writes=["identb"])
        P.op("dve", lambda h: h.tensor_copy(out=onesb, in_=cn[:, CN_ONE:CN_ONE + 128]), reads=["cn"], writes=["onesb"])
        P.op("dve", lambda h: h.tensor_copy(out=maskb, in_=cn[:, CN_MASK:CN_MASK + 64]), reads=["cn"], writes=["maskb"])
        P.op("dve", lambda h: h.tensor_tensor(out=hp[:, 12:16], in0=pc[:, PC_LB + 4:PC_LB + 8], in1=pc[:, PC_LB:PC_LB + 4],
                                              op=ALU.subtract), reads=["pc"], writes=["hp_t"])
        P.op("act", lambda h: h.activation(out=hp[:, 12:16], in_=hp[:, 12:16], func=AF.Exp), reads=["hp_t"], writes=["hp_t"])
        P.op("dve", lambda h: h.tensor_scalar_add(out=hp[:, 12:16], in0=hp[:, 12:16], scalar1=1.0), reads=["hp_t"], writes=["hp_t"])
        P.op("dve", lambda h: h.reciprocal(out=hp[:, 0:4], in_=hp[:, 12:16]), reads=["hp_t"], writes=["hp_lb"])
        P.op("dve", lambda h: h.tensor_scalar(out=hp[:, 4:8], in0=hp[:, 0:4], scalar1=-1.0, scalar2=1.0, op0=ALU.mult, op1=ALU.add),
             reads=["hp_lb"], writes=["hp_oml"])
        P.op("act", lambda h: h.activation(out=hp[:, 8:12], in_=hp[:, 4:8], func=AF.Ln), reads=["hp_oml"], writes=["hp_ln"])
        for c in range(8):
            P.op("dve", lambda h, c=c: h.tensor_scalar(out=wrg[:, c, :], in0=wr[:, c, :], scalar1=pc[:, PC_G + 24 + c:PC_G + 25 + c],
                                                       scalar2=None, op0=ALU.mult), reads=["wr", "pc"], writes=["wrg"])

        def wview(w2d, c0, c1):
            return w2d.rearrange("(c p) n -> p c n", p=128)[:, :, c0:c1]

        def gbc(gi):
            return pc[:, PC_G + gi * 8:PC_G + gi * 8 + 8].unsqueeze(2).to_broadcast([128, 8, 128])

        def norm_a(src, src_key, stat, stat_key, xn, xn_key):
            ss = stat[:, 0:1]
            rs = stat[:, 1:2]
            P.op("act", lambda h: h.activation(out=xn, in_=src, func=AF.Square, accum_out=ss),
                 reads=[src_key], writes=[xn_key, stat_key])
            P.op("act", lambda h: h.activation(out=rs, in_=ss, func=AF.Ln, scale=1.0 / D, bias=eps_ap), reads=[stat_key, "eps"], writes=[stat_key])
            P.op("act", lambda h: h.activation(out=rs, in_=rs, func=AF.Exp, scale=-0.5), reads=[stat_key], writes=[stat_key])
            P.op("dve", lambda h: h.tensor_scalar(out=xn, in0=src, scalar1=rs, scalar2=None, op0=ALU.mult),
                 reads=[src_key, stat_key], writes=[xn_key])

        def norm_b(xn, xn_key, gi, dst3, dst_key, ti):
            bk = Bank("b")
            for c in range(8):
                P.op("pe", lambda h, c=c: h.transpose(out=bk.t[:, c * 128:(c + 1) * 128], in_=xn[:, c * 128:(c + 1) * 128], identity=identb),
                     reads=[xn_key, "identb"], writes=[bk.k()])
            P.op("dve", lambda h: h.tensor_tensor(out=dst3[:, :, ti * 128:(ti + 1) * 128],
                                                  in0=bk.t[:, :].rearrange("p (c n) -> p c n", c=8), in1=gbc(gi), op=ALU.mult),
                 reads=[bk.k(), "pc"], writes=[dst_key])

        def norm_tile(src, src_key, gi, dst3, dst_key, stat, stat_key, xn, xn_key, ti):
            norm_a(src, src_key, stat, stat_key, xn, xn_key)
            norm_b(xn, xn_key, gi, dst3, dst_key, ti)

        eps_ap = carve(1)
        P.op("dve", lambda h: h.memset(eps_ap, EPS), writes=["eps"])
        one_ap = carve(1)
        P.op("dve", lambda h: h.memset(one_ap, 1.0), writes=["one"])
        base_top = top[0]

        if stop_after == "S":
            P.barrier()
            P.op("sp", lambda h: h.dma_start(out=dbg_d[:, 0:16], in_=hp), dma=True)
            P.op("sp", lambda h: h.dma_start(out=dbg_d[:, 16:1060], in_=rows), dma=True)
            P.op("sp", lambda h: h.dma_start(out=dbg_d[:, 1060:1220], in_=wrg.rearrange("p c n -> p (c n)")), dma=True)
            nhalves = 0

        wrb = carve(80, BF16, "p (c n) -> p c n", c=8)
        P.op("dve", lambda h: h.tensor_copy(out=wrb, in_=wr), reads=["wr"], writes=["wrb"])
        base_top = top[0]

        def carve_at(off, words, dtype=F32, pattern=None, **kw):
            sv = top[0]
            top[0] = off
            ap = carve(words, dtype, pattern, **kw)
            top[0] = sv
            return ap

        def do_half(hf):
            top[0] = base_top
            bufA_off = top[0]
            bufA = carve(8192, BF16, "p (c n) -> p c n", c=8)
            bufY_off = top[0]
            bufY = carve(8192, BF16, "p (c n) -> p c n", c=8)
            xres_off = top[0]
            xres = carve(16384, F32, "p (i n) -> p i n", i=16)
            rstd3 = carve(32, F32, "p (i n) -> p i n", i=16)
            RT0 = top[0]
            tok0 = hf * SEQ

            def akey(i):
                return "A.%d" % i

            def ykey(c, j):
                return "Y.%d.%d" % (c, j)

            def xkey(i):
                return "X.%d" % i

            win = carve_at(xres_off, 14336, BF16, "p (c n) -> p c n", c=8)
            vtok = carve_at(xres_off + 14336, 2048, BF16, "p (c n) -> p c n", c=8)
            for s in (0, 1, 2, 5, 4, 3, 6):
                P.op("pool", lambda h, s=s: h.dma_start(out=win[:, :, s * 512:(s + 1) * 512], in_=wview(w_in_d, s * 512, (s + 1) * 512)),
                     writes=["win%d" % s], dma=True)

            top[0] = RT0
            xs = [carve(1024) for _ in range(3)]
            xn3 = [carve(512, BF16) for _ in range(3)]
            for i in range(NT + 1):
                if i < NT:
                    xk = "xs%d" % (i % 3)
                    P.op("sp", lambda h, i=i: h.dma_start(out=xs[i % 3], in_=x_d[tok0 + i * 128:tok0 + (i + 1) * 128, :]),
                         writes=[xk], dma=True)
                    norm_a(xs[i % 3], xk, rstd3[:, i, :], "st.%d" % i, xn3[i % 3], "xn%d" % (i % 3))
                if i >= 1:
                    norm_b(xn3[(i - 1) % 3], "xn%d" % ((i - 1) % 3), 0, bufA, akey(i - 1), i - 1)
            P.barrier()
            if stop_after == "A1":
                for c in range(8):
                    P.op("pool", lambda h, c=c: h.dma_start(out=dbg_d[:, c * 1024:(c + 1) * 1024], in_=bufA[:, c, 0:1024]), dma=True)
                return True

            top[0] = RT0
            wout = carve(4096, BF16, "p (c n) -> p c n", c=8)
            P.op("pool", lambda h: h.dma_start(out=wout, in_=wview(w_out_d, 0, D)), writes=["wout"], dma=True)
            Sst = carve(512, F32, "p (h n) -> p h n", h=4)
            ubuf = carve(4 * 514, F32, "p (c n) -> p c n", c=4)
            P.op("pool", lambda h: h.memset(Sst.rearrange("p h n -> p (h n)"), 0.0), writes=["S0", "S1", "S2", "S3"])
            P.op("pool", lambda h: h.memset(ubuf.rearrange("p c n -> p (c n)"), 0.0), writes=["u0", "u1", "u2", "u3"])
            f_t1 = carve(512)
            f_t2 = carve(512)
            DSb = [carve(64, BF16) for _ in range(8)]
            NSET = 2
            TS = []
            for q in range(NSET):
                TS.append(dict(
                    e=carve(512), l2=carve(512), l1=carve(512), lnk=carve(512), d=carve(512), dec=carve(8),
                    qt=carve(256, BF16), khT=carve(256, BF16),
                    khtok=carve(512, BF16, "p (c n) -> p c n", c=8), scm=carve(256, BF16, "p (c n) -> p c n", c=8)))

            def conv_chunk(j, cch):
                T0 = j * 512
                rdA = [akey(4 * j + t) for t in range(4)]
                th = []
                bks = [None, None, None]

                def mm(s):
                    def f():
                        bks[s] = Bank("f")
                        bk = bks[s]
                        for c in range(8):
                            P.op("pe", lambda h, c=c: h.matmul(
                                bk.t[:, :], lhsT=win[:, c, s * 512 + cch * 128:s * 512 + (cch + 1) * 128],
                                rhs=bufA[:, c, T0:T0 + 512], start=(c == 0), stop=(c == 7)),
                                reads=rdA + ["win%d" % s], writes=[bk.k()])
                    return f
                uk = "u%d" % cch
                cw0 = PC_CONV + cch * 3

                def ew1():
                    if j > 0:
                        P.op("pool", lambda h: h.tensor_copy(out=ubuf[:, cch, 0:2], in_=ubuf[:, cch, 512:514]), reads=[uk], writes=[uk])
                    P.op("act", lambda h: h.activation(out=ubuf[:, cch, 2:514], in_=bks[1].t[:, :], func=AF.Copy), reads=[bks[1].k()], writes=[uk])

                def ew2():
                    P.op("dve", lambda h: h.tensor_tensor(out=ubuf[:, cch, 2:514], in0=bks[2].t[:, :], in1=ubuf[:, cch, 2:514], op=ALU.mult),
                         reads=[bks[2].k(), uk], writes=[uk])
                    P.op("dve", lambda h: h.tensor_scalar(out=f_t1, in0=ubuf[:, cch, 2:514], scalar1=pc[:, cw0 + 2:cw0 + 3],
                                                          scalar2=None, op0=ALU.mult), reads=[uk, "pc"], writes=["f_t1"])
                    P.op("dve", lambda h: h.scalar_tensor_tensor(out=f_t2, in0=ubuf[:, cch, 1:513], scalar=pc[:, cw0 + 1:cw0 + 2],
                                                                 in1=f_t1, op0=ALU.mult, op1=ALU.add), reads=[uk, "pc", "f_t1"], writes=["f_t2"])
                    P.op("dve", lambda h: h.scalar_tensor_tensor(out=f_t1, in0=ubuf[:, cch, 0:512], scalar=pc[:, cw0:cw0 + 1],
                                                                 in1=f_t2, op0=ALU.mult, op1=ALU.add), reads=[uk, "pc", "f_t2"], writes=["f_t1"])

                def ew3():
                    P.op("dve", lambda h: h.tensor_tensor(out=bufY[:, cch, T0:T0 + 512], in0=bks[0].t[:, :], in1=f_t1, op=ALU.mult),
                         reads=[bks[0].k(), "f_t1"], writes=[ykey(cch, j)])
                def seq(*fs):
                    def f():
                        for g_ in fs:
                            g_()
                    return f
                return [seq(mm(1), ew1), seq(mm(2), ew2), seq(mm(0), ew3)]

            def v_group(j, c8):
                T0 = j * 512
                rdA = [akey(4 * j + t) for t in range(4)]

                def f():
                    bk = Bank("f")
                    for c in range(8):
                        P.op("pe", lambda h, c=c: h.matmul(
                            bk.t[0:64, :], lhsT=bufA[:, c, T0 + c8 * 64:T0 + (c8 + 1) * 64], rhs=win[:, c, 5 * 512:6 * 512],
                            start=(c == 0), stop=(c == 7)), reads=rdA + ["win5"], writes=[bk.k()])
                    P.op("act", lambda h: h.activation(out=vtok[0:64, c8, :], in_=bk.t[0:64, :], func=AF.Copy),
                         reads=[bk.k()], writes=["vtok%d" % c8])
                return f

            def head_unit(j, hd, q):
                T0 = j * 512
                rdA = [akey(4 * j + t) for t in range(4)]
                t = TS[q]
                K = lambda n: "%s.%d" % (n, q)
                st_ = {}
                lbh = hp[:, hd:hd + 1]
                lnomlh = hp[:, 8 + hd:9 + hd]
                b3 = t["e"].rearrange("p (c n) -> p c n", c=8)
                sk = "S%d" % hd
                E = []

                def proj(name, s):
                    def f():
                        bk = Bank("f")
                        st_[name] = bk
                        for c in range(8):
                            P.op("pe", lambda h, c=c: h.matmul(
                                bk.t[:, :], lhsT=win[:, c, s * 512 + hd * 128:s * 512 + (hd + 1) * 128],
                                rhs=bufA[:, c, T0:T0 + 512], start=(c == 0), stop=(c == 7)),
                                reads=rdA + ["win%d" % s], writes=[bk.k()])
                    return f
                pz_ = proj("z", 4)

                def e1():
                    bz = st_["z"]
                    P.op("act", lambda h: h.activation(out=t["e"], in_=bz.t[:, :], func=AF.Exp, scale=-1.0), reads=[bz.k()], writes=[K("e")])
                    P.op("act", lambda h: h.activation(out=t["l2"], in_=t["e"], func=AF.Ln, bias=one_ap), reads=[K("e"), "one"], writes=[K("l2")])
                    P.op("act", lambda h: h.activation(out=t["l1"], in_=t["e"], func=AF.Ln, scale=lbh, bias=one_ap),
                         reads=[K("e"), "one", "hp_lb"], writes=[K("l1")])

                def e2():
                    bz = st_["z"]
                    P.op("pool", lambda h: h.tensor_tensor(out=t["l1"], in0=t["l1"], in1=t["l2"], op=ALU.subtract), reads=[K("l1"), K("l2")], writes=[K("l1")])
                    P.op("dve", lambda h: h.scalar_tensor_tensor(out=t["lnk"], in0=bz.t[:, :], scalar=-1.0, in1=t["l2"], op0=ALU.mult, op1=ALU.subtract),
                         reads=[bz.k(), K("l2")], writes=[K("lnk")])
                pq_ = proj("q", 3)

                def e3():
                    P.op("dve", lambda h: h.tensor_tensor_scan(out=t["e"], data0=rmask, data1=t["l1"], initial=0.0, op0=ALU.mult, op1=ALU.add),
                         reads=["cn", K("l1"), K("e")], writes=[K("e")])
                    P.op("pool", lambda h: h.tensor_tensor(out=t["d"].rearrange("p (c n) -> p c n", c=8), in0=b3,
                                                           in1=b3[:, :, 63:64].to_broadcast([128, 8, 64]), op=ALU.subtract),
                         reads=[K("e")], writes=[K("d")])

                def e4():
                    bq = st_["q"]
                    P.op("act", lambda h: h.activation(out=t["l2"], in_=t["d"], func=AF.Exp), reads=[K("d"), K("l2")], writes=[K("l2")])
                    P.op("dve", lambda h: h.tensor_tensor(out=t["qt"], in0=bq.t[:, :], in1=t["l2"], op=ALU.mult), reads=[bq.k(), K("l2")], writes=[K("qt")])
                pg_ = proj("g", 6)

                def e5():
                    P.op("pool", lambda h: h.tensor_tensor(out=t["lnk"], in0=t["lnk"], in1=t["d"], op=ALU.subtract), reads=[K("lnk"), K("d")], writes=[K("lnk")])
                    P.op("act", lambda h: h.activation(out=t["khT"], in_=t["lnk"], func=AF.Exp, bias=lnomlh),
                         reads=[K("lnk"), "hp_ln"], writes=[K("khT")])
                    P.op("act", lambda h: h.activation(out=t["dec"], in_=b3[:, :, 63], func=AF.Exp), reads=[K("e")], writes=[K("dec")])

                def e6():
                    bg = st_["g"]
                    P.op("act", lambda h: h.activation(out=t["l1"], in_=bg.t[:, :], func=AF.Exp, scale=-1.0), reads=[bg.k(), K("l1")], writes=[K("l1")])
                    P.op("act", lambda h: h.activation(out=t["l1"], in_=t["l1"], func=AF.Ln, bias=one_ap), reads=[K("l1"), "one"], writes=[K("l1")])
                    P.op("act", lambda h: h.activation(out=t["l1"], in_=t["l1"], func=AF.Exp, scale=-1.0), reads=[K("l1")], writes=[K("l1")])
                    P.op("dve", lambda h: h.tensor_tensor(out=t["l1"], in0=bg.t[:, :], in1=t["l1"], op=ALU.mult), reads=[bg.k(), K("l1")], writes=[K("l1")])

                def grp(*fs):
                    def f():
                        for g_ in fs:
                            g_()
                    return f
                E.append(grp(pz_, e1, e2))
                E.append(grp(pq_, e3, e4))
                E.append(grp(pg_, e5, e6))

                C = []

                def c0():
                    bkk = Bank("b")
                    for c8 in range(8):
                        P.op("pe", lambda h, c8=c8: h.transpose(out=bkk.t[0:64, c8 * 128:(c8 + 1) * 128], in_=t["khT"][:, c8 * 64:(c8 + 1) * 64],
                                                               identity=identb), reads=[K("khT"), "identb"], writes=[bkk.k()])
                    P.op("act", lambda h: h.activation(out=t["khtok"][0:64, :, :].rearrange("p c n -> p (c n)"), in_=bkk.t[0:64, :], func=AF.Copy),
                         reads=[bkk.k()], writes=[K("khtok")])
                    st_["o"] = Bank("f", hold=True)
                    st_["sc"] = Bank("f", hold=True)
                C.append(c0)

                def c1():
                    bsc = st_["sc"]
                    st_["ds0"] = Bank("f", hold=True)
                    st_["ds1"] = Bank("f", hold=True)
                    for c8 in range(8):
                        cs = slice(c8 * 64, (c8 + 1) * 64)
                        P.op("pe", lambda h, cs=cs: h.matmul(bsc.t[0:64, cs], lhsT=t["khT"][:, cs], rhs=t["qt"][:, cs], start=True, stop=True),
                             reads=[K("khT"), K("qt")], writes=[bsc.k(c8)])
                    for c8 in range(8):
                        bds = st_["ds%d" % (c8 // 4)]
                        ds_cols = slice((c8 % 4) * 128, (c8 % 4 + 1) * 128)
                        P.op("pe", lambda h, c8=c8, bds=bds, ds_cols=ds_cols: h.matmul(bds.t[:, ds_cols], lhsT=t["khtok"][0:64, c8, :],
                                                                                        rhs=vtok[0:64, c8, hd * 128:(hd + 1) * 128], start=True, stop=True),
                             reads=[K("khtok"), "vtok%d" % c8], writes=[bds.k(c8 % 4)])
                C.append(c1)

                def c2():
                    bsc = st_["sc"]
                    for c8 in range(8):
                        cs = slice(c8 * 64, (c8 + 1) * 64)
                        P.op("dve", lambda h, c8=c8, cs=cs: h.tensor_tensor(out=t["scm"][0:64, c8, :], in0=bsc.t[0:64, cs], in1=maskb[0:64, :], op=ALU.mult),
                             reads=[bsc.k(c) for c in range(8)] + ["maskb"], writes=[K("scm%d" % c8)])
                    for c8 in range(8):
                        bds = st_["ds%d" % (c8 // 4)]
                        ds_cols = slice((c8 % 4) * 128, (c8 % 4 + 1) * 128)
                        P.op("dve", lambda h, c8=c8: h.tensor_scalar(out=DSb[c8], in0=Sst[:, hd, :], scalar1=t["dec"][:, c8:c8 + 1], scalar2=None, op0=ALU.mult),
                             reads=[sk, K("dec")], writes=["DSb%d" % c8])
                        P.op("dve", lambda h, c8=c8, bds=bds, ds_cols=ds_cols: h.scalar_tensor_tensor(
                            out=Sst[:, hd, :], in0=Sst[:, hd, :], scalar=t["dec"][:, c8:c8 + 1], in1=bds.t[:, ds_cols], op0=ALU.mult, op1=ALU.add),
                            reads=[sk, K("dec")] + [bds.k(c) for c in range(4)], writes=[sk])
                    st_["sc"].done()
                    st_["ds0"].done()
                    st_["ds1"].done()
                C.append(c2)

                def c3():
                    bo = st_["o"]
                    for c8 in range(8):
                        cs = slice(c8 * 64, (c8 + 1) * 64)
                        P.op("pe", lambda h, c8=c8, cs=cs: h.matmul(bo.t[:, cs], lhsT=DSb[c8], rhs=t["qt"][:, cs], start=True, stop=False),
                             reads=["DSb%d" % c8, K("qt")], writes=[bo.k(c8)])
                        P.op("pe", lambda h, c8=c8, cs=cs: h.matmul(bo.t[:, cs], lhsT=vtok[0:64, c8, hd * 128:(hd + 1) * 128], rhs=t["scm"][0:64, c8, :],
                                                                    start=False, stop=True),
                             reads=["vtok%d" % c8, K("scm%d" % c8)], writes=[bo.k(c8)])
                C.append(c3)

                def c9():
                    bo = st_["o"]
                    okeys = [bo.k(c8) for c8 in range(8)]
                    P.op("act", lambda h: h.activation(out=t["khT"], in_=bo.t[:, :], func=AF.Square), reads=okeys + [K("khT")], writes=[K("khT")])
                    bss = Bank("f")
                    P.op("pe", lambda h: h.matmul(bss.t[:, :], lhsT=onesb, rhs=t["khT"], start=True, stop=True), reads=["onesb", K("khT")], writes=[bss.k()])
                    P.op("act", lambda h: h.activation(out=t["l2"], in_=bss.t[:, :], func=AF.Ln, scale=1.0 / 128, bias=eps_ap),
                         reads=[bss.k(), "eps", K("l2")], writes=[K("l2")])
                    P.op("act", lambda h: h.activation(out=t["l2"], in_=t["l2"], func=AF.Exp, scale=-0.5), reads=[K("l2")], writes=[K("l2")])
                    P.op("dve", lambda h: h.tensor_tensor(out=t["d"], in0=bo.t[:, :], in1=t["l2"], op=ALU.mult), reads=okeys + [K("l2"), K("d")], writes=[K("d")])
                    P.op("dve", lambda h: h.scalar_tensor_tensor(out=bufY[:, 4 + hd, T0:T0 + 512], in0=t["d"], scalar=pc[:, PC_HN + hd:PC_HN + hd + 1],
                                                                 in1=t["l1"], op0=ALU.mult, op1=ALU.mult),
                         reads=[K("d"), K("l1"), "pc"], writes=[ykey(4 + hd, j)])
                    bo.done()
                C.append(c9)
                return E, C

            def interleave(primary, fillers):
                n, m = len(primary), len(fillers)
                fi = 0
                for i, f in enumerate(primary):
                    f()
                    want = ((i + 1) * m) // max(n, 1)
                    while fi < want:
                        fillers[fi]()
                        fi += 1
                while fi < m:
                    fillers[fi]()
                    fi += 1

            for cch in range(4):
                for f in conv_chunk(0, cch):
                    f()
            for c8 in range(8):
                v_group(0, c8)()
            units = [(j, hd) for j in range(NJ) for hd in range(4)]
            E0, Cprev = head_unit(0, 0, 0)
            for f in E0:
                f()
            for u in range(len(units)):
                j, hd = units[u]
                fill = []
                if u + 1 < len(units):
                    jn, hn = units[u + 1]
                    En, Cn = head_unit(jn, hn, (u + 1) % NSET)
                    if jn == j:
                        fill += En
                else:
                    En, Cn = [], []
                if j + 1 < NJ:
                    fill += conv_chunk(j + 1, hd)
                for f in Cprev[0:3]:
                    f()
                for f in fill:
                    f()
                for f in Cprev[3:]:
                    f()
                if u + 1 < len(units) and units[u + 1][0] != j:
                    for c8 in range(8):
                        v_group(j + 1, c8)()
                    for f in En:
                        f()
                Cprev = Cn
            P.barrier()
            if stop_after == "A2":
                for c in (4, 5, 6, 7, 0, 1, 2, 3):
                    for jj in range(2):
                        P.op("pool", lambda h, c=c, jj=jj: h.dma_start(out=dbg_d[:, c * 1024 + jj * 512:c * 1024 + (jj + 1) * 512],
                                                                      in_=bufY[:, c, jj * 512:(jj + 1) * 512]), dma=True)
                return True

            top[0] = RT0
            wout = carve(4096, BF16, "p (c n) -> p c n", c=8)
            xsb = [carve(1024) for _ in range(3)]
            xnb = [carve(512, BF16) for _ in range(3)]
            wq_off = top[0]
            wq = carve(4096, BF16, "p (c n) -> p c n", c=8)
            wo = carve(4096, BF16, "p (c n) -> p c n", c=8)
            P.op("pool", lambda h: h.dma_start(out=wq, in_=wview(w_q_d, 0, D)), writes=["wq"], dma=True)
            P.op("pool", lambda h: h.dma_start(out=wo, in_=wview(w_o_d, 0, D)), writes=["wo"], dma=True)

            def b1_stage1(i):
                xk = "xsb%d" % (i % 3)
                P.op("sp", lambda h: h.dma_start(out=xsb[i % 3], in_=x_d[tok0 + i * 128:tok0 + (i + 1) * 128, :]), writes=[xk], dma=True)
                for n2 in range(2):
                    bk = Bank("f")
                    for c in range(8):
                        P.op("pe", lambda h, c=c, n2=n2, bk=bk: h.matmul(bk.t[:, :], lhsT=bufY[:, c, i * 128:(i + 1) * 128],
                                                                         rhs=wout[:, c, n2 * 512:(n2 + 1) * 512], start=(c == 0), stop=(c == 7)),
                             reads=["wout"], writes=[bk.k()])
                    P.op("dve", lambda h, n2=n2, bk=bk: h.tensor_tensor(out=xres[:, i, n2 * 512:(n2 + 1) * 512], in0=bk.t[:, :],
                                                                        in1=xsb[i % 3][:, n2 * 512:(n2 + 1) * 512], op=ALU.add),
                         reads=[bk.k(), xk], writes=[xkey(i)])

            b1_stage1(0)
            b1_stage1(1)
            for i in range(NT + 1):
                if i + 2 < NT:
                    b1_stage1(i + 2)
                if i < NT:
                    norm_a(xres[:, i, :], xkey(i), rstd3[:, i, :], "st.%d" % i, xnb[i % 3], "xnb%d" % (i % 3))
                if i >= 1:
                    norm_b(xnb[(i - 1) % 3], "xnb%d" % ((i - 1) % 3), 1, bufA, akey(i - 1), i - 1)
            P.barrier()
            if stop_after == "B1":
                for i in range(8):
                    P.op("pool", lambda h, i=i: h.dma_start(out=dbg_d[:, i * 1024:(i + 1) * 1024], in_=xres[:, i, :]), dma=True)
                return True

            KT = carve_at(RT0, 1024, BF16, "p (c n) -> p c n", c=8)
            Vt = carve_at(RT0 + 1024, 1024, BF16, "p (c n) -> p c n", c=2)
            mstat = carve_at(RT0 + 2048, 4)
            wq = carve_at(wq_off, 4096, BF16, "p (c n) -> p c n", c=8)
            wo = carve_at(wq_off + 4096, 4096, BF16, "p (c n) -> p c n", c=8)
            top[0] = bufY_off
            wkvb = [carve(2048, BF16, "p (c n) -> p c n", c=8) for _ in range(2)]
            mT = carve(1024, BF16, "p (c n) -> p c n", c=8)
            mst = [carve(1024) for _ in range(2)]
            xn2 = [carve(512, BF16) for _ in range(2)]
            assert top[0] <= bufY_off + 8192
            for t in range(2):
                P.op("sp", lambda h, t=t: h.dma_start(out=mst[t], in_=mem_d[hf * 256 + t * 128:hf * 256 + (t + 1) * 128, :]),
                     writes=["mst%d" % t], dma=True)
            for blk in range(2):
                P.op("pool", lambda h, blk=blk: h.dma_start(out=wkvb[blk], in_=wview(w_kv_d, blk * 512, (blk + 1) * 512)), writes=["wkv%d" % blk], dma=True)
            for t in range(2):
                norm_tile(mst[t], "mst%d" % t, 2, mT, "mT", mstat[:, 2 * t:2 * t + 2], "mstat%d" % t, xn2[t], "xn%d" % t, t)
            for blk in range(4):
                wb = wkvb[blk % 2]
                wk = "wkv%d" % (blk % 2)
                if blk >= 2:
                    P.op("pool", lambda h, blk=blk, wb=wb: h.dma_start(out=wb, in_=wview(w_kv_d, blk * 512, (blk + 1) * 512)), writes=[wk], dma=True)
                if blk < 2:
                    for e4 in range(4):
                        ec = blk * 4 + e4
                        bk = Bank("f")
                        for c in range(8):
                            P.op("pe", lambda h, c=c, e4=e4, wb=wb, bk=bk: h.matmul(bk.t[:, 0:256], lhsT=wb[:, c, e4 * 128:(e4 + 1) * 128], rhs=mT[:, c, :],
                                                                                   start=(c == 0), stop=(c == 7)), reads=[wk, "mT"], writes=[bk.k()])
                        P.op("act", lambda h, ec=ec, bk=bk: h.activation(out=KT[:, ec, :], in_=bk.t[:, 0:256], func=AF.Copy), reads=[bk.k()], writes=["KT"])
                else:
                    n2 = blk - 2
                    for mc in range(2):
                        bk = Bank("f")
                        for c in range(8):
                            P.op("pe", lambda h, c=c, mc=mc, wb=wb, bk=bk: h.matmul(bk.t[:, :], lhsT=mT[:, c, mc * 128:(mc + 1) * 128], rhs=wb[:, c, :],
                                                                                   start=(c == 0), stop=(c == 7)), reads=[wk, "mT"], writes=[bk.k()])
                        P.op("act", lambda h, mc=mc, n2=n2, bk=bk: h.activation(out=Vt[:, mc, n2 * 512:(n2 + 1) * 512], in_=bk.t[:, :], func=AF.Copy),
                             reads=[bk.k()], writes=["Vt"])
            P.barrier()
            top[0] = bufY_off
            QT = carve(2048, BF16, "p (c n) -> p c n", c=8)
            OT = carve(2048, BF16, "p (c n) -> p c n", c=8)
            ET = [carve(512, BF16, "p (c n) -> p c n", c=2) for _ in range(2)]
            rden = [carve(512) for _ in range(2)]

            def q_proj(j):
                T0 = j * 512
                rdA = [akey(4 * j + t) for t in range(4)]
                for ec in range(8):
                    bk = Bank("f")
                    for c in range(8):
                        P.op("pe", lambda h, c=c, ec=ec, bk=bk: h.matmul(bk.t[:, :], lhsT=wq[:, c, ec * 128:(ec + 1) * 128], rhs=bufA[:, c, T0:T0 + 512],
                                                                        start=(c == 0), stop=(c == 7)), reads=rdA + ["wq"], writes=[bk.k()])
                    if ec % 2 == 0:
                        P.op("act", lambda h, ec=ec, bk=bk: h.activation(out=QT[:, ec, :], in_=bk.t[:, :], func=AF.Copy, scale=1.0 / 16.0),
                             reads=[bk.k()], writes=["QT%d" % ec])
                    else:
                        P.op("dve", lambda h, ec=ec, bk=bk: h.tensor_scalar(out=QT[:, ec, :], in0=bk.t[:, :], scalar1=1.0 / 16.0, scalar2=None, op0=ALU.mult),
                             reads=[bk.k()], writes=["QT%d" % ec])

            def s_exp(hd):
                et = ET[hd % 2]
                ek = "ET%d" % (hd % 2)
                for mc in range(2):
                    bk = Bank("f")
                    for k2 in range(2):
                        P.op("pe", lambda h, mc=mc, k2=k2, bk=bk: h.matmul(bk.t[:, :], lhsT=KT[:, 2 * hd + k2, mc * 128:(mc + 1) * 128],
                                                                          rhs=QT[:, 2 * hd + k2, :], start=(k2 == 0), stop=(k2 == 1)),
                             reads=["KT", "QT%d" % (2 * hd + k2)], writes=[bk.k()])
                    P.op("act", lambda h, mc=mc, bk=bk: h.activation(out=et[:, mc, :], in_=bk.t[:, :], func=AF.Exp), reads=[bk.k()], writes=[ek + ".%d" % mc])

            def pv(hd):
                et = ET[hd % 2]
                ek = "ET%d" % (hd % 2)
                bden = Bank("f")
                for mc in range(2):
                    P.op("pe", lambda h, mc=mc: h.matmul(bden.t[:, :], lhsT=onesb, rhs=et[:, mc, :], start=(mc == 0), stop=(mc == 1)),
                         reads=["onesb", ek + ".%d" % mc], writes=[bden.k()])
                rd = rden[hd % 2]
                rk = "rden%d" % (hd % 2)
                P.op("dve", lambda h: h.reciprocal(out=rd, in_=bden.t[:, :]), reads=[bden.k()], writes=[rk])
                for k2 in range(2):
                    bk = Bank("f")
                    for mc in range(2):
                        P.op("pe", lambda h, mc=mc, k2=k2, bk=bk: h.matmul(
                            bk.t[:, :], lhsT=Vt[:, mc, (2 * hd + k2) * 128:(2 * hd + k2 + 1) * 128], rhs=et[:, mc, :], start=(mc == 0), stop=(mc == 1)),
                            reads=["Vt", ek + ".%d" % mc], writes=[bk.k()])
                    P.op("dve", lambda h, k2=k2, bk=bk: h.tensor_tensor(out=OT[:, 2 * hd + k2, :], in0=bk.t[:, :], in1=rd, op=ALU.mult),
                         reads=[bk.k(), rk], writes=["OT%d" % (2 * hd + k2)])

            def w_o(j):
                for tt in range(4):
                    i = 4 * j + tt
                    for n2 in range(2):
                        bk = Bank("f")
                        for ec in range(8):
                            P.op("pe", lambda h, ec=ec, tt=tt, n2=n2, bk=bk: h.matmul(bk.t[:, :], lhsT=OT[:, ec, tt * 128:(tt + 1) * 128],
                                                                                     rhs=wo[:, ec, n2 * 512:(n2 + 1) * 512], start=(ec == 0), stop=(ec == 7)),
                                 reads=["OT%d" % ec, "wo"], writes=[bk.k()])
                        P.op("dve", lambda h, i=i, n2=n2, bk=bk: h.tensor_tensor(out=xres[:, i, n2 * 512:(n2 + 1) * 512], in0=bk.t[:, :],
                                                                                 in1=xres[:, i, n2 * 512:(n2 + 1) * 512], op=ALU.add),
                             reads=[bk.k(), xkey(i)], writes=[xkey(i)])

            q_proj(0)
            for j in range(NJ):
                s_exp(0)
                for hd in range(4):
                    if hd + 1 < 4:
                        s_exp(hd + 1)
                    pv(hd)
                if j + 1 < NJ:
                    q_proj(j + 1)
                w_o(j)
            P.barrier()
            if stop_after == "B2":
                for i in range(8):
                    P.op("pool", lambda h, i=i: h.dma_start(out=dbg_d[:, i * 1024:(i + 1) * 1024], in_=xres[:, i, :]), dma=True)
                return True

            top[0] = bufY_off
            wg = [carve(2048, BF16, "p (c n) -> p c n", c=8) for _ in range(2)]
            wu = [carve(2048, BF16, "p (c n) -> p c n", c=8) for _ in range(2)]
            top[0] = RT0
            wd = [carve(2048, BF16, "p (c n) -> p c n", c=4) for _ in range(2)]
            xn2 = [carve(512, BF16) for _ in range(2)]
            lg = carve(320, F32, "p (i n) -> p i n", i=16)
            comb = carve(256, F32, "p (i n) -> p i n", i=16)
            r_t = [carve(256) for _ in range(4)]
            r_s = [carve(64) for _ in range(6)]
            hid = [carve(1024, BF16, "p (c n) -> p c n", c=4) for _ in range(2)]
            sgb = [carve(256, BF16) for _ in range(2)]

            def wload(e):
                b = e % 2
                P.op("pool", lambda h: h.dma_start(out=wg[b], in_=w_gate_d[e].rearrange("(c p) n -> p c n", p=128)), writes=["wg%d" % b], dma=True)
                P.op("pool", lambda h: h.dma_start(out=wu[b], in_=w_up_d[e].rearrange("(c p) n -> p c n", p=128)), writes=["wu%d" % b], dma=True)
                P.op("pool", lambda h: h.dma_start(out=wd[b], in_=w_down_d[e].rearrange("(c p) n -> p c n", p=128)), writes=["wd%d" % b], dma=True)

            xnc = xn2 + [carve(512, BF16)]

            def router(i):
                bk = Bank("f")
                for c in range(8):
                    P.op("pe", lambda h, c=c: h.matmul(bk.t[:, 0:20], lhsT=bufA[:, c, i * 128:(i + 1) * 128], rhs=wrb[:, c, :],
                                                       start=(c == 0), stop=(c == 7)), reads=[akey(i), "wrb"], writes=[bk.k()])
                P.op("dve", lambda h: h.tensor_tensor(out=lg[:, i, :], in0=bk.t[:, 0:20], in1=rows[:, 1024:1044], op=ALU.add),
                     reads=[bk.k(), "rows"], writes=["lg"])

            for i in range(NT + 1):
                if i < NT:
                    norm_a(xres[:, i, :], xkey(i), rstd3[:, i, :], "st.%d" % i, xnc[i % 3], "xnc%d" % (i % 3))
                    P.op("sp", lambda h, i=i: h.dma_start(out=h3_d[i * 128:(i + 1) * 128, :], in_=xnc[i % 3]), reads=["xnc%d" % (i % 3)],
                         writes=["H3.%d" % i], dma=True)
                if i >= 1:
                    norm_b(xnc[(i - 1) % 3], "xnc%d" % ((i - 1) % 3), 3, bufA, akey(i - 1), i - 1)
                    router(i - 1)
            LG = lg[:, :, 0:4]
            LE = lg[:, :, 4:20].rearrange("p i (j k) -> p i j k", j=4)
            gmax, gsum, m1, m2, w1, w2 = [r[:, 0:16] for r in r_s]
            gsh = r_t[0][:, 0:64].rearrange("p (i j) -> p i j", i=16)
            gm = r_t[1][:, 0:64].rearrange("p (i j) -> p i j", i=16)
            tmp4 = r_t[2].rearrange("p (i j k) -> p i j k", i=16, j=4)
            esel = r_t[3][:, 0:64].rearrange("p (i k) -> p i k", i=16)
            mk1 = r_t[3][:, 64:128].rearrange("p (i k) -> p i k", i=16)
            e2 = r_t[3][:, 128:192].rearrange("p (i k) -> p i k", i=16)
            mk2 = r_t[3][:, 192:256].rearrange("p (i k) -> p i k", i=16)
            cig = r_t[0][:, 64:128].rearrange("p (i k) -> p i k", i=16)
            tq = r_t[0][:, 128:192].rearrange("p (i k) -> p i k", i=16)

            def bc3(a):
                return a.unsqueeze(2).to_broadcast([128, 16, 4])

            R = lambda fn, eng="dve": P.op(eng, fn, reads=["lg", "rt"], writes=["rt"])
            R(lambda h: h.tensor_reduce(out=gmax, in_=LG, axis=AX.X, op=ALU.max))
            R(lambda h: h.tensor_tensor(out=gsh, in0=LG, in1=bc3(gmax), op=ALU.subtract))
            R(lambda h: h.tensor_single_scalar(out=gm, in_=gsh, scalar=0.0, op=ALU.is_ge))
            R(lambda h: h.activation(out=gsh, in_=gsh, func=AF.Exp), "act")
            R(lambda h: h.tensor_reduce(out=gsum, in_=gsh, axis=AX.X, op=ALU.add))
            R(lambda h: h.reciprocal(out=gsum, in_=gsum))
            R(lambda h: h.tensor_tensor(out=tmp4, in0=LE, in1=gm.unsqueeze(3).to_broadcast([128, 16, 4, 4]), op=ALU.mult))
            R(lambda h: h.tensor_reduce(out=esel, in_=tmp4.rearrange("p i j k -> p i k j"), axis=AX.X, op=ALU.add))
            R(lambda h: h.tensor_reduce(out=m1, in_=esel, axis=AX.X, op=ALU.max))
            R(lambda h: h.tensor_tensor(out=mk1, in0=esel, in1=bc3(m1), op=ALU.is_ge))
            R(lambda h: h.scalar_tensor_tensor(out=e2, in0=mk1, scalar=-1e30, in1=esel, op0=ALU.mult, op1=ALU.add))
            R(lambda h: h.tensor_reduce(out=m2, in_=e2, axis=AX.X, op=ALU.max))
            R(lambda h: h.tensor_tensor(out=mk2, in0=e2, in1=bc3(m2), op=ALU.is_ge))
            R(lambda h: h.tensor_tensor(out=w2, in0=m2, in1=m1, op=ALU.subtract))
            R(lambda h: h.activation(out=w2, in_=w2, func=AF.Exp), "act")
            R(lambda h: h.tensor_scalar_add(out=w1, in0=w2, scalar1=1.0))
            R(lambda h: h.reciprocal(out=w1, in_=w1))
            R(lambda h: h.tensor_tensor(out=w2, in0=w2, in1=w1, op=ALU.mult))
            R(lambda h: h.tensor_tensor(out=w1, in0=w1, in1=gsum, op=ALU.mult))
            R(lambda h: h.tensor_tensor(out=w2, in0=w2, in1=gsum, op=ALU.mult))
            R(lambda h: h.tensor_tensor(out=cig, in0=mk1, in1=bc3(w1), op=ALU.mult))
            R(lambda h: h.tensor_tensor(out=tq, in0=mk2, in1=bc3(w2), op=ALU.mult))
            R(lambda h: h.tensor_tensor(out=cig, in0=cig, in1=tq, op=ALU.add))
            P.op("dve", lambda h: h.tensor_tensor(out=comb.rearrange("p i (j k) -> p i j k", j=4), in0=gm.unsqueeze(3).to_broadcast([128, 16, 4, 4]),
                                                  in1=cig.unsqueeze(2).to_broadcast([128, 16, 4, 4]), op=ALU.mult), reads=["rt"], writes=["comb"])
            if stop_after == "R":
                P.barrier()
                P.op("pool", lambda h: h.dma_start(out=dbg_d[:, 0:256], in_=comb.rearrange("p i n -> p (i n)")), dma=True)
                P.op("pool", lambda h: h.dma_start(out=dbg_d[:, 256:576], in_=lg.rearrange("p i n -> p (i n)")), dma=True)
                return True

            NU = 23
            NSLOT = NU * 512
            oh = carve(512, F32, "p (a i e) -> p a i e", a=2, i=16)
            A_bf = carve(128, BF16)
            pit = carve(256, F32, "p (i e) -> p i e", i=16)
            tot = carve(256, F32, "p (i e) -> p i e", i=16)
            off = carve(256, F32, "p (i e) -> p i e", i=16)
            sm = carve(80)
            ne, un, cu, cui, base = [sm[:, k * 16:(k + 1) * 16] for k in range(5)]
            slf = carve(32, F32, "p (a i) -> p a i", a=2)
            sli = carve(32).bitcast(I32).rearrange("p (a i) -> p a i", a=2)
            cmpb = carve(NU * 16, F32, "p (s e) -> p s e", s=NU)
            euf = carve(32)
            wix = carve(32).bitcast(I32)
            tokid = carve(16).bitcast(I32)
            uidx = carve(92).bitcast(I32)
            zer = carve(92).bitcast(I32)
            Ub = carve(64, BF16)
            P.op("dve", lambda h: h.tensor_copy(out=Ub, in_=cn[:, CN_U:CN_U + 128]), reads=["cn"], writes=["Ub"])
            P.op("pool", lambda h: h.iota(tokid, pattern=[[128, 16]], base=0, channel_multiplier=1), writes=["tokid"])
            P.op("dve", lambda h: h.memset(zer, 0), writes=["zer"])
            P.op("sp", lambda h: h.dma_start(out=tokslot_d.rearrange("(p n) o -> p (n o)", p=128), in_=zer), reads=["zer"], writes=["ts0"], dma=True)
            S = lambda fn, eng="dve": P.op(eng, fn, reads=["rt", "cn", "Ub"], writes=["rt"])
            gm4 = gm.unsqueeze(3).to_broadcast([128, 16, 4, 4])
            S(lambda h: h.tensor_tensor(out=oh[:, 0].rearrange("p i (j k) -> p i j k", j=4), in0=gm4,
                                        in1=mk1.unsqueeze(2).to_broadcast([128, 16, 4, 4]), op=ALU.mult))
            S(lambda h: h.tensor_tensor(out=oh[:, 1].rearrange("p i (j k) -> p i j k", j=4), in0=gm4,
                                        in1=mk2.unsqueeze(2).to_broadcast([128, 16, 4, 4]), op=ALU.mult))
            S(lambda h: h.tensor_tensor(out=A_bf, in0=oh[:, 0].rearrange("p i e -> p (i e)"), in1=oh[:, 1].rearrange("p i e -> p (i e)"), op=ALU.add))
            bkp, bkt = Bank("f"), Bank("f")
            P.op("pe", lambda h: h.matmul(bkp.t[:, 0:256], lhsT=Ub, rhs=A_bf, start=True, stop=True), reads=["rt", "Ub"], writes=[bkp.k()])
            P.op("pe", lambda h: h.matmul(bkt.t[:, 0:256], lhsT=onesb, rhs=A_bf, start=True, stop=True), reads=["rt", "onesb"], writes=[bkt.k()])
            P.op("dve", lambda h: h.tensor_copy(out=pit.rearrange("p i e -> p (i e)"), in_=bkp.t[:, 0:256]), reads=[bkp.k(), "rt"], writes=["rt"])
            P.op("act", lambda h: h.activation(out=tot.rearrange("p i e -> p (i e)"), in_=bkt.t[:, 0:256], func=AF.Copy), reads=[bkt.k(), "rt"], writes=["rt"])
            S(lambda h: h.memset(off[:, 0, :], 0.0))
            for i in range(1, 16):
                S(lambda h, i=i: h.tensor_tensor(out=off[:, i, :], in0=off[:, i - 1, :], in1=tot[:, i - 1, :], op=ALU.add))
            S(lambda h: h.tensor_tensor(out=ne, in0=off[:, 15, :], in1=tot[:, 15, :], op=ALU.add))
            S(lambda h: h.tensor_single_scalar(out=un, in_=ne, scalar=0.0, op=ALU.is_gt))
            for thr in (512.0, 1024.0, 1536.0):
                S(lambda h, thr=thr: h.scalar_tensor_tensor(out=un, in0=ne, scalar=thr, in1=un, op0=ALU.is_gt, op1=ALU.add))
            S(lambda h: h.memset(cu[:, 0:1], 0.0))
            for e in range(1, 16):
                S(lambda h, e=e: h.tensor_tensor(out=cu[:, e:e + 1], in0=cu[:, e - 1:e], in1=un[:, e - 1:e], op=ALU.add))
            S(lambda h: h.tensor_tensor(out=cui, in0=cu, in1=un, op=ALU.add))
            S(lambda h: h.tensor_single_scalar(out=base, in_=cu, scalar=512.0, op=ALU.mult))
            S(lambda h: h.tensor_tensor(out=pit, in0=pit, in1=off, op=ALU.add))
            S(lambda h: h.tensor_tensor(out=pit, in0=pit, in1=base.unsqueeze(1).to_broadcast([128, 16, 16]), op=ALU.add))
            for a in range(2):
                S(lambda h, a=a: h.tensor_tensor(out=oh[:, a], in0=oh[:, a], in1=pit, op=ALU.mult))
                S(lambda h, a=a: h.tensor_reduce(out=slf[:, a, :], in_=oh[:, a], axis=AX.X, op=ALU.add))
            S(lambda h: h.tensor_copy(out=sli, in_=slf))
            S(lambda h: h.tensor_tensor(out=cmpb, in0=cui.unsqueeze(1).to_broadcast([128, NU, 16]),
                                        in1=cn[:, CN_IOTA:CN_IOTA + NU].unsqueeze(2).to_broadcast([128, NU, 16]), op=ALU.is_le))
            S(lambda h: h.tensor_reduce(out=euf[:, 0:NU], in_=cmpb, axis=AX.X, op=ALU.add))
            S(lambda h: h.tensor_scalar(out=euf[:, 0:NU], in0=euf[:, 0:NU], scalar1=15.0, scalar2=128.0, op0=ALU.min, op1=ALU.mult))
            S(lambda h: h.tensor_scalar(out=euf[:, 0:NU], in0=euf[:, 0:NU], scalar1=cn[:, CN_PIO:CN_PIO + 1], scalar2=None, op0=ALU.add))
            S(lambda h: h.tensor_copy(out=wix[:, 0:NU], in_=euf[:, 0:NU]))
            for a in range(2):
                for i in range(16):
                    P.op("pool", lambda h, a=a, i=i: h.indirect_dma_start(
                        out=tokslot_d, out_offset=bass.IndirectOffsetOnAxis(ap=sli[:, a, i:i + 1].bitcast(U32), axis=0),
                        in_=tokid[:, i:i + 1], in_offset=None), reads=["rt", "tokid", "ts0"], writes=["ts.%d.%d" % (a, i)], dma=True)
            tskeys = ["ts.%d.%d" % (a, i) for a in range(2) for i in range(16)]
            for col in range(NU * 4):
                P.op("sp", lambda h, col=col: h.dma_start(out=uidx[:, col:col + 1], in_=tokslot_d[col * 128:(col + 1) * 128, :]),
                     reads=tskeys + ["ts0"], writes=["uidx%d" % col], dma=True)
            if stop_after == "SL":
                P.barrier()
                P.op("sp", lambda h: h.dma_start(out=dbg_d[:, 0:32], in_=slf.rearrange("p a i -> p (a i)")), dma=True)
                P.op("sp", lambda h: h.dma_start(out=dbg_d[:, 32:64], in_=euf), dma=True)
                P.op("sp", lambda h: h.dma_start(out=dbg_d[:, 64:96], in_=w1.to_broadcast([128, 16]) if False else r_s[4][:, 0:32]), dma=True)
                return True
            P.barrier()

            xgT = [carve_at(bufA_off + q * 2048, 2048, BF16, "p (c n) -> p c n", c=8) for q in range(2)]
            xg = [carve_at(bufA_off + 4096 + q * 512, 512, BF16) for q in range(4)]
            yst = [carve_at(bufA_off + 6144 + q * 1024, 1024) for q in range(2)]
            H3keys = ["H3.%d" % i for i in range(NT)]

            def pre_a(u):
                b = u % 2
                for r in range(4):
                    col = u * 4 + r
                    P.op("pool", lambda h, r=r, col=col: h.indirect_dma_start(
                        out=xg[r], out_offset=None, in_=h3_d,
                        in_offset=bass.IndirectOffsetOnAxis(ap=uidx[:, col:col + 1].bitcast(U32), axis=0)),
                        reads=["uidx%d" % col] + H3keys, writes=["xg%d" % r], dma=True)
                    norm_b(xg[r], "xg%d" % r, 3, xgT[b], "xgT%d.%d" % (b, r), r)
                for (wl, dst, nm) in ((wgl_d, wg[b], "wg%d" % b), (wul_d, wu[b], "wu%d" % b)):
                    for hh in range(2):
                        P.op("pool", lambda h, wl=wl, dst=dst, hh=hh: h.indirect_dma_start(
                            out=dst[:, 4 * hh:4 * hh + 4, :].rearrange("p c n -> p (c n)"), out_offset=None, in_=wl[hh],
                            in_offset=bass.IndirectOffsetOnAxis(ap=wix[:, u:u + 1].bitcast(U32), axis=0)),
                            reads=["rt"], writes=[nm + ".%d" % hh], dma=True)

            def pre_d(u):
                b = u % 2
                for hh in range(2):
                    P.op("pool", lambda h, hh=hh: h.indirect_dma_start(
                        out=wd[b][:, 2 * hh:2 * hh + 2, :].rearrange("p c n -> p (c n)"), out_offset=None, in_=wdl_d[hh],
                        in_offset=bass.IndirectOffsetOnAxis(ap=wix[:, u:u + 1].bitcast(U32), axis=0)),
                        reads=["rt"], writes=["wd%d.%d" % (b, hh)], dma=True)

            def gate_up(u):
                b = u % 2
                hb = hid[b]
                rdx = ["xgT%d.%d" % (b, r) for r in range(4)]
                for f in range(4):
                    bg_, bu_ = Bank("f"), Bank("f")
                    for (bk, w, wk) in ((bg_, wg[b], "wg%d" % b), (bu_, wu[b], "wu%d" % b)):
                        for c in range(8):
                            P.op("pe", lambda h, c=c, f=f, bk=bk, w=w: h.matmul(bk.t[:, :], lhsT=w[:, c, f * 128:(f + 1) * 128], rhs=xgT[b][:, c, :],
                                                                                start=(c == 0), stop=(c == 7)), reads=rdx + [wk + ".%d" % (c // 4)], writes=[bk.k()])
                    sg = sgb[f % 2]
                    P.op("act", lambda h, sg=sg, bg_=bg_: h.activation(out=sg, in_=bg_.t[:, :], func=AF.Silu), reads=[bg_.k()], writes=["sgb%d" % (f % 2)])
                    P.op("dve", lambda h, f=f, sg=sg, bu_=bu_: h.tensor_tensor(out=hb[:, f, :], in0=bu_.t[:, :], in1=sg, op=ALU.mult),
                         reads=[bu_.k(), "sgb%d" % (f % 2)], writes=["hid%d.%d" % (b, f)])

            def down(u):
                b = u % 2
                hb = hid[b]
                for tt in range(4):
                    ys = yst[tt % 2]
                    yk = "yst%d" % (tt % 2)
                    for n2 in range(2):
                        bk = Bank("f")
                        for f in range(4):
                            P.op("pe", lambda h, f=f, tt=tt, n2=n2, bk=bk: h.matmul(bk.t[:, :], lhsT=hb[:, f, tt * 128:(tt + 1) * 128],
                                                                                   rhs=wd[b][:, f, n2 * 512:(n2 + 1) * 512], start=(f == 0), stop=(f == 3)),
                                 reads=["hid%d.%d" % (b, f), "wd%d.%d" % (b, f // 2)], writes=[bk.k()])
                        if n2 == 0:
                            P.op("act", lambda h, ys=ys, bk=bk: h.activation(out=ys[:, 0:512], in_=bk.t[:, :], func=AF.Copy), reads=[bk.k()], writes=[yk + ".0"])
                        else:
                            P.op("dve", lambda h, ys=ys, bk=bk: h.tensor_copy(out=ys[:, 512:1024], in_=bk.t[:, :]), reads=[bk.k()], writes=[yk + ".1"])
                    row0 = (u * 4 + tt) * 128
                    P.op("sp", lambda h, ys=ys, row0=row0: h.dma_start(out=y_d[row0:row0 + 128, :], in_=ys), reads=[yk + ".0", yk + ".1"],
                         writes=["Y.%d" % (u * 4 + tt)], dma=True)

            pre_a(0)
            pre_d(0)
            for u in range(NU + 1):
                if u + 1 < NU:
                    pre_a(u + 1)
                if u < NU:
                    gate_up(u)
                if u >= 1:
                    down(u - 1)
                if u + 1 < NU:
                    pre_d(u + 1)
            P.barrier()
            yg = [carve_at(bufA_off + q * 1024, 1024) for q in range(4)]
            fin = rows[:, 0:1024]
            for i in range(NT):
                for a in range(2):
                    q = (2 * i + a) % 4
                    P.op("pool", lambda h, a=a, i=i, q=q: h.indirect_dma_start(
                        out=yg[q], out_offset=None, in_=y_d,
                        in_offset=bass.IndirectOffsetOnAxis(ap=sli[:, a, i:i + 1].bitcast(U32), axis=0)),
                        writes=["yg%d" % q], dma=True)
                    wa = (w1, w2)[a]
                    P.op("dve", lambda h, i=i, q=q, wa=wa: h.scalar_tensor_tensor(out=xres[:, i, :], in0=yg[q], scalar=wa[:, i:i + 1], in1=xres[:, i, :],
                                                                                 op0=ALU.mult, op1=ALU.add), reads=["yg%d" % q, xkey(i)], writes=[xkey(i)])
                stt = rstd3[:, i, :]
                sk_ = "st.%d" % i
                P.op("act", lambda h, i=i, stt=stt: h.activation(out=xn2[i % 2], in_=xres[:, i, :], func=AF.Square, accum_out=stt[:, 0:1]),
                     reads=[xkey(i)], writes=["xn%d" % (i % 2), sk_])
                P.op("act", lambda h, stt=stt: h.activation(out=stt[:, 1:2], in_=stt[:, 0:1], func=AF.Ln, scale=1.0 / D, bias=eps_ap), reads=[sk_, "eps"], writes=[sk_])
                P.op("act", lambda h, stt=stt: h.activation(out=stt[:, 1:2], in_=stt[:, 1:2], func=AF.Exp, scale=-0.5), reads=[sk_], writes=[sk_])
                P.op("dve", lambda h, i=i, stt=stt: h.scalar_tensor_tensor(out=xres[:, i, :], in0=xres[:, i, :], scalar=stt[:, 1:2], in1=fin, op0=ALU.mult, op1=ALU.mult),
                     reads=[xkey(i), sk_, "rows"], writes=[xkey(i)])
                P.op("sp", lambda h, i=i: h.dma_start(out=out_d[tok0 + i * 128:tok0 + (i + 1) * 128, :], in_=xres[:, i, :]),
                     reads=[xkey(i)], writes=["out.%d.%d" % (hf, i)], dma=True)
            P.barrier()
            return False

        for hf_ in range(nhalves):
            if do_half(hf_):
                break
        P.emit()
    return nc


_CACHE = {}


def _host_consts():
    cn = np.zeros((128, CN_N), np.float32)
    cn[:, CN_ID:CN_ID + 128] = np.eye(128, dtype=np.float32)
    cn[:, CN_ONE:CN_ONE + 128] = 1.0
    s = np.arange(64)[:, None]
    t = np.arange(64)[None, :]
    cn[0:64, CN_MASK:CN_MASK + 64] = (s <= t).astype(np.float32)
    rm = np.ones(512, np.float32)
    rm[::64] = 0.0
    cn[:, CN_RM:CN_RM + 512] = rm[None, :]
    kk = np.arange(128)[:, None]
    mm = np.arange(128)[None, :]
    cn[:, CN_U:CN_U + 128] = (kk < mm).astype(np.float32)
    cn[:, CN_IOTA:CN_IOTA + 32] = np.arange(32, dtype=np.float32)[None, :]
    cn[:, CN_PIO] = np.arange(128, dtype=np.float32)
    return cn


def _col(v, nchunk):
    return np.ascontiguousarray(np.asarray(v, np.float32).reshape(nchunk, 128).T)


def _rowlay(name, w, nchunk):
    E, R, n = w.shape
    t = w.reshape(E, nchunk, 128, n).transpose(0, 2, 1, 3).reshape(E * 128, nchunk * n)
    hlf = nchunk * n // 2
    return {name + "0": np.ascontiguousarray(t[:, :hlf]), name + "1": np.ascontiguousarray(t[:, hlf:])}


def make_in_maps(inputs):
    f = lambda k: np.asarray(inputs[k], np.float32)
    pcols = np.zeros((128, PC_N), np.float32)
    for gi, k in enumerate(["mix_norm", "xattn_norm", "mem_norm", "ffn_norm"]):
        pcols[:, PC_G + gi * 8:PC_G + gi * 8 + 8] = _col(f(k)[0], 8)
    cw = f("conv_w")[0]
    for cch in range(4):
        for jj in range(3):
            pcols[:, PC_CONV + cch * 3 + jj] = cw[jj, cch * 128:(cch + 1) * 128]
    lbr = f("hgrn_lb")
    for r in range(2):
        pcols[:, PC_LB + r * 4:PC_LB + r * 4 + 4] = _col(lbr[r], 4)
    pcols[:, PC_HN:PC_HN + 4] = _col(f("hgrn_norm")[0], 4)
    wrc = np.concatenate([f("w_group")[0], f("w_expert")[0]], axis=1)
    wr = np.ascontiguousarray(wrc.reshape(8, 128, 20).transpose(1, 0, 2).reshape(128, 160))
    rows = np.concatenate([f("final_norm").reshape(-1), f("b_group")[0], f("b_expert")[0]])[None, :].astype(np.float32)
    consts = _host_consts()
    x = f("x")
    mem = f("mem")
    shared = dict(
        w_in=np.ascontiguousarray(f("w_in")[0]), w_out=np.ascontiguousarray(f("w_out")[0]),
        w_q=np.ascontiguousarray(f("w_q")[0]), w_kv=np.ascontiguousarray(f("w_kv")[0]), w_o=np.ascontiguousarray(f("w_o")[0]),
        **_rowlay("wgl", f("w_gate")[0], 8), **_rowlay("wul", f("w_up")[0], 8), **_rowlay("wdl", f("w_down")[0], 4),
        pcols=pcols, wr=wr, rows=np.ascontiguousarray(rows), consts=consts)
    maps = []
    for c in range(NCORES):
        m = dict(shared)
        m["x"] = np.ascontiguousarray(x[2 * c:2 * c + 2].reshape(2 * SEQ, D))
        m["mem"] = np.ascontiguousarray(mem[2 * c:2 * c + 2].reshape(512, D))
        maps.append(m)
    return maps


def kernel(**inputs):
    if "nc" not in _CACHE:
        _CACHE["nc"] = build_program()
    nc = _CACHE["nc"]
    maps = make_in_maps(inputs)
    res = run_bass_kernel_spmd(nc, maps, core_ids=list(range(NCORES)))
    outs = [np.asarray(r["out"], np.float32).reshape(2, SEQ, D) for r in res.results]
    return np.concatenate(outs, axis=0)
```

```python
import contextlib
import numpy as np
import concourse.bass as bass
import concourse.mybir as mybir
from concourse.bass_utils import run_bass_kernel_spmd

F32 = mybir.dt.float32
BF16 = mybir.dt.bfloat16
I32 = mybir.dt.int32
U32 = mybir.dt.uint32
AF = mybir.ActivationFunctionType
ALU = mybir.AluOpType
AX = mybir.AxisListType

ENGS = ("pe", "act", "dve", "pool", "sp")
EPS = 1e-6
NCORES = 8
SEQ = 2048
D = 1024
NT = 16
NJ = 4
NEXP = 16


class Prog:
    NDMA_SEMS = 24
    NBG_SEMS = 6

    def __init__(self, nc):
        self.nc = nc
        self.ops = []
        self.last_w = {}
        self.readers = {}
        self.dma_rr = [0, 0]
        self.bg_rr = 0
        self.bg_last = {}
        self.dma_last = {}
        self.last_on = {}

    def alias(self, old_keys, new_key):
        s = set()
        for k in old_keys:
            w = self.last_w.get(k)
            if w is not None:
                s.add(w)
            s.update(self.readers.get(k, ()))
        self.last_w[new_key] = None
        self.readers[new_key] = list(s)

    def op(self, eng, fn, reads=(), writes=(), dma=False, extra=(), bg=False):
        oid = len(self.ops)
        deps = set(extra)
        for k in reads:
            w = self.last_w.get(k)
            if w is not None:
                deps.add(w)
        for k in writes:
            w = self.last_w.get(k)
            if w is not None:
                deps.add(w)
            for r in self.readers.get(k, ()):
                deps.add(r)
        deps.discard(oid)
        rec = dict(id=oid, eng=eng, fn=fn, deps=deps, dma=dma, sig=False, dsem=None)
        if dma and bg:
            s = self.NDMA_SEMS + (self.bg_rr % self.NBG_SEMS)
            self.bg_rr += 1
            rec["dsem"] = s
            prev = self.bg_last.get(s)
            if prev is not None:
                deps.add(prev)
            self.bg_last[s] = oid
        elif dma:
            half = self.NDMA_SEMS // 2
            q = 0 if eng == "pool" else 1
            s = q * half + (self.dma_rr[q] % half)
            self.dma_rr[q] += 1
            rec["dsem"] = s
            prev = self.dma_last.get(s)
            if prev is not None:
                deps.add(prev)
            self.dma_last[s] = oid
        self.ops.append(rec)
        if fn is not None and not bg:
            self.last_on[eng] = oid
        for k in writes:
            self.last_w[k] = oid
            self.readers[k] = []
        for k in reads:
            if k not in writes:
                self.readers.setdefault(k, []).append(oid)
        return oid

    def barrier(self):
        tails = set(self.last_on.values()) | set(self.dma_last.values())
        for e in ENGS:
            self.op(e, None, extra=set(tails))
        keepw = {k: v for k, v in self.last_w.items() if k.startswith("keep:")}
        self.last_w = keepw
        self.readers = {}

    def emit(self):
        nc = self.nc
        ops = self.ops
        for o in ops:
            if o["eng"] == "pe" and not o["dma"]:
                o["deps"] = {d for d in o["deps"] if ops[d]["dma"] or ops[d]["eng"] != "pe"}
        for o in ops:
            for d in o["deps"]:
                ops[d]["sig"] = True
        cnt = {e: 0 for e in ENGS}
        dcnt = {}
        for o in ops:
            if o["dma"]:
                s = o["dsem"]
                dcnt[s] = dcnt.get(s, 0) + 16
                o["ev"] = ("d%d" % s, dcnt[s])
            elif o["sig"]:
                cnt[o["eng"]] += 1
                o["ev"] = (o["eng"], cnt[o["eng"]])
        with contextlib.ExitStack() as st:
            sems = {}
            for e in ENGS:
                sems[e] = st.enter_context(nc.semaphore("sem_" + e))
            for s in range(self.NDMA_SEMS + self.NBG_SEMS):
                sems["d%d" % s] = st.enter_context(nc.semaphore("sem_d%d" % s))
            block = st.enter_context(nc.Block())
            per = {e: [o for o in ops if o["eng"] == e] for e in ENGS}

            def run(e, handle):
                waited = {}
                for o in per[e]:
                    need = {}
                    for d in o["deps"]:
                        sname, val = ops[d]["ev"]
                        if need.get(sname, 0) < val:
                            need[sname] = val
                    for sname, val in need.items():
                        if waited.get(sname, 0) >= val:
                            continue
                        handle.wait_ge(sems[sname], val)
                        waited[sname] = val
                    if o["fn"] is None:
                        continue
                    ins = o["fn"](handle)
                    if o["dma"]:
                        ins.then_inc(sems[o["ev"][0]], 16)
                    elif o["sig"]:
                        ins.then_inc(sems[e], 1)

            @block.tensor
            def _(h):
                run("pe", h)

            @block.scalar
            def _(h):
                run("act", h)

            @block.vector
            def _(h):
                run("dve", h)

            @block.gpsimd
            def _(h):
                run("pool", h)

            @block.sync
            def _(h):
                run("sp", h)
                for s, v in dcnt.items():
                    h.wait_ge(sems["d%d" % s], v)


PC_G = 0
PC_CONV = 32
PC_LB = 44
PC_HN = 52
PC_N = 56
CN_ID = 0
CN_ONE = 128
CN_MASK = 256
CN_RM = 320
CN_U = 832
CN_IOTA = 960
CN_PIO = 992
CN_N = 1024


def build_program(stop_after=None, nhalves=2):
    nc = bass.Bass("TRN2", target_bir_lowering=False)

    def dram(name, shape, kind="ExternalInput"):
        return nc.dram_tensor(name, shape, F32, kind=kind).ap()

    x_d = dram("x", [2 * SEQ, D])
    mem_d = dram("mem", [512, D])
    w_in_d = dram("w_in", [D, 3584])
    w_out_d = dram("w_out", [D, D])
    w_q_d = dram("w_q", [D, D])
    w_kv_d = dram("w_kv", [D, 2 * D])
    w_o_d = dram("w_o", [D, D])
    wgl_d = [dram("wgl%d" % q, [NEXP * 128, 2048]) for q in range(2)]
    wul_d = [dram("wul%d" % q, [NEXP * 128, 2048]) for q in range(2)]
    wdl_d = [dram("wdl%d" % q, [NEXP * 128, 2048]) for q in range(2)]
    wgb_d = [nc.dram_tensor("wgb%d" % q, [NEXP * 128, 2048], BF16, kind="Internal").ap() for q in range(2)]
    wub_d = [nc.dram_tensor("wub%d" % q, [NEXP * 128, 2048], BF16, kind="Internal").ap() for q in range(2)]
    wdb_d = [nc.dram_tensor("wdb%d" % q, [NEXP * 128, 2048], BF16, kind="Internal").ap() for q in range(2)]
    h3_d = nc.dram_tensor("h3s", [SEQ, D], BF16, kind="Internal").ap()
    tokslot_d = nc.dram_tensor("tokslot", [23 * 512, 1], I32, kind="Internal").ap()
    y_d = nc.dram_tensor("ysc", [23 * 512, D], F32, kind="Internal").ap()
    pcols_d = dram("pcols", [128, PC_N])
    wr_d = dram("wr", [128, 160])
    rows_d = dram("rows", [1, 1044])
    consts_d = dram("consts", [128, CN_N])
    out_d = dram("out", [2 * SEQ, D], kind="ExternalOutput")
    dbg_d = dram("dbg", [128, 8192], kind="ExternalOutput") if stop_after else None

    st = contextlib.ExitStack()
    with st:
        NW = 52800
        big = st.enter_context(nc.sbuf_tensor("big", [128, NW], F32))
        psf = [st.enter_context(nc.psum_tensor("psf%d" % i, [128, 512], F32)) for i in range(6)]
        psb = [st.enter_context(nc.psum_tensor("psb%d" % i, [128, 1024], BF16)) for i in range(2)]
        P = Prog(nc)

        top = [0]

        def carve(words, dtype=F32, pattern=None, **kw):
            off = top[0]
            top[0] += words
            assert top[0] <= NW, "SBUF arena overflow %d" % top[0]
            ap = big[:, off:off + words]
            if dtype == BF16:
                ap = ap.bitcast(BF16)
            if pattern:
                ap = ap.rearrange(pattern, **kw)
            return ap

        bank_state = {}
        rr = {"f": 0, "b": 0}

        held = set()

        class Bank:
            def __init__(self, kind, hold=False):
                n = 6 if kind == "f" else 2
                for _ in range(n):
                    self.idx = rr[kind] % n
                    rr[kind] += 1
                    if (kind, self.idx) not in held:
                        break
                else:
                    raise RuntimeError("all PSUM banks held")
                self.kind = kind
                self.gen = rr[kind]
                self.t = psf[self.idx] if kind == "f" else psb[self.idx]
                self.name = "%s%d" % (kind, self.idx)
                self.old = bank_state.get(self.name, [])
                self.keys = {}
                bank_state[self.name] = []
                if hold:
                    held.add((kind, self.idx))

            def done(self):
                held.discard((self.kind, self.idx))

            def k(self, sub=0):
                if sub not in self.keys:
                    key = "ps.%s.%d.%s" % (self.name, self.gen, sub)
                    P.alias(self.old, key)
                    self.keys[sub] = key
                    bank_state[self.name].append(key)
                return self.keys[sub]

        pc = carve(PC_N)
        cn = carve(CN_N)
        identb = carve(64, BF16)
        onesb = carve(64, BF16)
        maskb = carve(32, BF16)
        wr = carve(160, F32, "p (c n) -> p c n", c=8)
        wrg = carve(160, F32, "p (c n) -> p c n", c=8)
        rows = carve(1044)
        hp = carve(16)
        ident = cn[:, CN_ID:CN_ID + 128]
        rmask = cn[:, CN_RM:CN_RM + 512]
        base_top = top[0]

        P.op("sp", lambda h: h.dma_start(out=pc, in_=pcols_d), writes=["pc"], dma=True)
        P.op("sp", lambda h: h.dma_start(out=cn, in_=consts_d), writes=["cn"], dma=True)
        P.op("sp", lambda h: h.dma_start(out=wr.rearrange("p c n -> p (c n)"), in_=wr_d), writes=["wr"], dma=True)
        P.op("sp", lambda h: h.dma_start(out=rows, in_=rows_d.partition_broadcast(128)), writes=["rows"], dma=True)
        P.op("dve", lambda h: h.tensor_copy(out=identb, in_=cn[:, CN_ID:CN_ID + 128]), reads=["cn"], writes=["identb"])
        P.op("dve", lambda h: h.tensor_copy(out=onesb, in_=cn[:, CN_ONE:CN_ONE + 128]), reads=["cn"], writes=["onesb"])
        P.op("dve", lambda h: h.tensor_copy(out=maskb, in_=cn[:, CN_MASK:CN_MASK + 64]), reads=["cn"], writes=["maskb"])
        P.op("dve", lambda h: h.tensor_tensor(out=hp[:, 12:16], in0=pc[:, PC_LB + 4:PC_LB + 8], in1=pc[:, PC_LB:PC_LB + 4],
                                              op=ALU.subtract), reads=["pc"], writes=["hp_t"])
        P.op("act", lambda h: h.activation(out=hp[:, 12:16], in_=hp[:, 12:16], func=AF.Exp), reads=["hp_t"], writes=["hp_t"])
        P.op("dve", lambda h: h.tensor_scalar_add(out=hp[:, 12:16], in0=hp[:, 12:16], scalar1=1.0), reads=["hp_t"], writes=["hp_t"])
        P.op("dve", lambda h: h.reciprocal(out=hp[:, 0:4], in_=hp[:, 12:16]), reads=["hp_t"], writes=["hp_lb"])
        P.op("dve", lambda h: h.tensor_scalar(out=hp[:, 4:8], in0=hp[:, 0:4], scalar1=-1.0, scalar2=1.0, op0=ALU.mult, op1=ALU.add),
             reads=["hp_lb"], writes=["hp_oml"])
        P.op("act", lambda h: h.activation(out=hp[:, 8:12], in_=hp[:, 4:8], func=AF.Ln), reads=["hp_oml"], writes=["hp_ln"])
        for c in range(8):
            P.op("dve", lambda h, c=c: h.tensor_scalar(out=wrg[:, c, :], in0=wr[:, c, :], scalar1=pc[:, PC_G + 24 + c:PC_G + 25 + c],
                                                       scalar2=None, op0=ALU.mult), reads=["wr", "pc"], writes=["wrg"])

        def wview(w2d, c0, c1):
            return w2d.rearrange("(c p) n -> p c n", p=128)[:, :, c0:c1]

        def gbc(gi):
            return pc[:, PC_G + gi * 8:PC_G + gi * 8 + 8].unsqueeze(2).to_broadcast([128, 8, 128])

        def norm_a(src, src_key, stat, stat_key, xn, xn_key):
            ss = stat[:, 0:1]
            rs = stat[:, 1:2]
            P.op("act", lambda h: h.activation(out=xn, in_=src, func=AF.Square, accum_out=ss),
                 reads=[src_key], writes=[xn_key, stat_key])
            P.op("act", lambda h: h.activation(out=rs, in_=ss, func=AF.Ln, scale=1.0 / D, bias=eps_ap), reads=[stat_key, "eps"], writes=[stat_key])
            P.op("act", lambda h: h.activation(out=rs, in_=rs, func=AF.Exp, scale=-0.5), reads=[stat_key], writes=[stat_key])
            P.op("dve", lambda h: h.tensor_scalar(out=xn, in0=src, scalar1=rs, scalar2=None, op0=ALU.mult),
                 reads=[src_key, stat_key], writes=[xn_key])

        def norm_b(xn, xn_key, gi, dst3, dst_key, ti):
            bk = Bank("b")
            for c in range(8):
                P.op("pe", lambda h, c=c: h.transpose(out=bk.t[:, c * 128:(c + 1) * 128], in_=xn[:, c * 128:(c + 1) * 128], identity=identb),
                     reads=[xn_key, "identb"], writes=[bk.k()])
            P.op("dve", lambda h: h.tensor_tensor(out=dst3[:, :, ti * 128:(ti + 1) * 128],
                                                  in0=bk.t[:, :].rearrange("p (c n) -> p c n", c=8), in1=gbc(gi), op=ALU.mult),
                 reads=[bk.k(), "pc"], writes=[dst_key])

        def norm_tile(src, src_key, gi, dst3, dst_key, stat, stat_key, xn, xn_key, ti):
            norm_a(src, src_key, stat, stat_key, xn, xn_key)
            norm_b(xn, xn_key, gi, dst3, dst_key, ti)

        eps_ap = carve(1)
        P.op("dve", lambda h: h.memset(eps_ap, EPS), writes=["eps"])
        one_ap = carve(1)
        P.op("dve", lambda h: h.memset(one_ap, 1.0), writes=["one"])
        base_top = top[0]

        if stop_after == "S":
            P.barrier()
            P.op("sp", lambda h: h.dma_start(out=dbg_d[:, 0:16], in_=hp), dma=True)
            P.op("sp", lambda h: h.dma_start(out=dbg_d[:, 16:1060], in_=rows), dma=True)
            P.op("sp", lambda h: h.dma_start(out=dbg_d[:, 1060:1220], in_=wrg.rearrange("p c n -> p (c n)")), dma=True)
            nhalves = 0

        wrb = carve(80, BF16, "p (c n) -> p c n", c=8)
        P.op("dve", lambda h: h.tensor_copy(out=wrb, in_=wr), reads=["wr"], writes=["wrb"])
        base_top = top[0]

        def carve_at(off, words, dtype=F32, pattern=None, **kw):
            sv = top[0]
            top[0] = off
            ap = carve(words, dtype, pattern, **kw)
            top[0] = sv
            return ap

        cast_jobs = []
        for (src, dst, nm) in ((wgl_d, wgb_d, "g"), (wul_d, wub_d, "u"), (wdl_d, wdb_d, "d")):
            for q in range(2):
                for ch in range(8):
                    cast_jobs.append((src[q], dst[q], ch, "keep:wc.%s%d" % (nm, q)))
        cast_pos = [0]

        def emit_casts(n):
            for _ in range(n):
                if cast_pos[0] >= len(cast_jobs):
                    return
                src, dst, ch, key = cast_jobs[cast_pos[0]]
                cast_pos[0] += 1
                P.op("pool", lambda h, src=src, dst=dst, ch=ch: h.dma_start(out=dst[ch * 256:(ch + 1) * 256, :], in_=src[ch * 256:(ch + 1) * 256, :]),
                     writes=[key + ".%d" % ch], dma=True, bg=True)

        def cast_keys(nm, q):
            return ["keep:wc.%s%d.%d" % (nm, q, ch) for ch in range(8)]

        def do_half(hf):
            top[0] = base_top
            bufA_off = top[0]
            bufA = carve(8192, BF16, "p (c n) -> p c n", c=8)
            bufY_off = top[0]
            bufY = carve(8192, BF16, "p (c n) -> p c n", c=8)
            xres_off = top[0]
            xres = carve(16384, F32, "p (i n) -> p i n", i=16)
            rstd3 = carve(32, F32, "p (i n) -> p i n", i=16)
            RT0 = top[0]
            tok0 = hf * SEQ

            def akey(i):
                return "A.%d" % i

            def ykey(c, j):
                return "Y.%d.%d" % (c, j)

            def xkey(i):
                return "X.%d" % i

            win = carve_at(xres_off, 14336, BF16, "p (c n) -> p c n", c=8)
            vtok = carve_at(xres_off + 14336, 2048, BF16, "p (c n) -> p c n", c=8)
            for s in (0, 1, 2, 5, 4, 3, 6):
                P.op("pool", lambda h, s=s: h.dma_start(out=win[:, :, s * 512:(s + 1) * 512], in_=wview(w_in_d, s * 512, (s + 1) * 512)),
                     writes=["win%d" % s], dma=True)

            top[0] = RT0
            xs = [carve(1024) for _ in range(3)]
            xn3 = [carve(512, BF16) for _ in range(3)]
            for i in range(NT + 1):
                if i < NT:
                    xk = "xs%d" % (i % 3)
                    P.op("sp", lambda h, i=i: h.dma_start(out=xs[i % 3], in_=x_d[tok0 + i * 128:tok0 + (i + 1) * 128, :]),
                         writes=[xk], dma=True)
                    norm_a(xs[i % 3], xk, rstd3[:, i, :], "st.%d" % i, xn3[i % 3], "xn%d" % (i % 3))
                if i >= 1:
                    norm_b(xn3[(i - 1) % 3], "xn%d" % ((i - 1) % 3), 0, bufA, akey(i - 1), i - 1)
            P.barrier()
            if stop_after == "A1":
                for c in range(8):
                    P.op("pool", lambda h, c=c: h.dma_start(out=dbg_d[:, c * 1024:(c + 1) * 1024], in_=bufA[:, c, 0:1024]), dma=True)
                return True

            top[0] = RT0
            wout = carve(4096, BF16, "p (c n) -> p c n", c=8)
            P.op("pool", lambda h: h.dma_start(out=wout, in_=wview(w_out_d, 0, D)), writes=["wout"], dma=True)
            emit_casts(20)
            Sst = carve(512, F32, "p (h n) -> p h n", h=4)
            ubuf = carve(4 * 514, F32, "p (c n) -> p c n", c=4)
            P.op("pool", lambda h: h.memset(Sst.rearrange("p h n -> p (h n)"), 0.0), writes=["S0", "S1", "S2", "S3"])
            P.op("pool", lambda h: h.memset(ubuf.rearrange("p c n -> p (c n)"), 0.0), writes=["u0", "u1", "u2", "u3"])
            f_t1 = carve(512)
            f_t2 = carve(512)
            DSb = [carve(64, BF16) for _ in range(8)]
            NSET = 2
            TS = []
            for q in range(NSET):
                TS.append(dict(
                    e=carve(512), l2=carve(512), l1=carve(512), lnk=carve(512), d=carve(512), dec=carve(8),
                    qt=carve(256, BF16), khT=carve(256, BF16),
                    khtok=carve(512, BF16, "p (c n) -> p c n", c=8), scm=carve(256, BF16, "p (c n) -> p c n", c=8)))

            def conv_chunk(j, cch):
                T0 = j * 512
                rdA = [akey(4 * j + t) for t in range(4)]
                th = []
                bks = [None, None, None]

                def mm(s):
                    def f():
                        bks[s] = Bank("f")
                        bk = bks[s]
                        for c in range(8):
                            P.op("pe", lambda h, c=c: h.matmul(
                                bk.t[:, :], lhsT=win[:, c, s * 512 + cch * 128:s * 512 + (cch + 1) * 128],
                                rhs=bufA[:, c, T0:T0 + 512], start=(c == 0), stop=(c == 7)),
                                reads=rdA + ["win%d" % s], writes=[bk.k()])
                    return f
                uk = "u%d" % cch
                cw0 = PC_CONV + cch * 3

                def ew1():
                    if j > 0:
                        P.op("pool", lambda h: h.tensor_copy(out=ubuf[:, cch, 0:2], in_=ubuf[:, cch, 512:514]), reads=[uk], writes=[uk])
                    P.op("act", lambda h: h.activation(out=ubuf[:, cch, 2:514], in_=bks[1].t[:, :], func=AF.Copy), reads=[bks[1].k()], writes=[uk])

                def ew2():
                    P.op("dve", lambda h: h.tensor_tensor(out=ubuf[:, cch, 2:514], in0=bks[2].t[:, :], in1=ubuf[:, cch, 2:514], op=ALU.mult),
                         reads=[bks[2].k(), uk], writes=[uk])
                    P.op("dve", lambda h: h.tensor_scalar(out=f_t1, in0=ubuf[:, cch, 2:514], scalar1=pc[:, cw0 + 2:cw0 + 3],
                                                          scalar2=None, op0=ALU.mult), reads=[uk, "pc"], writes=["f_t1"])
                    P.op("dve", lambda h: h.scalar_tensor_tensor(out=f_t2, in0=ubuf[:, cch, 1:513], scalar=pc[:, cw0 + 1:cw0 + 2],
                                                                 in1=f_t1, op0=ALU.mult, op1=ALU.add), reads=[uk, "pc", "f_t1"], writes=["f_t2"])
                    P.op("dve", lambda h: h.scalar_tensor_tensor(out=f_t1, in0=ubuf[:, cch, 0:512], scalar=pc[:, cw0:cw0 + 1],
                                                                 in1=f_t2, op0=ALU.mult, op1=ALU.add), reads=[uk, "pc", "f_t2"], writes=["f_t1"])

                def ew3():
                    P.op("dve", lambda h: h.tensor_tensor(out=bufY[:, cch, T0:T0 + 512], in0=bks[0].t[:, :], in1=f_t1, op=ALU.mult),
                         reads=[bks[0].k(), "f_t1"], writes=[ykey(cch, j)])
                def seq(*fs):
                    def f():
                        for g_ in fs:
                            g_()
                    return f
                return [seq(mm(1), ew1), seq(mm(2), ew2), seq(mm(0), ew3)]

            def v_group(j, c8):
                T0 = j * 512
                rdA = [akey(4 * j + t) for t in range(4)]

                def f():
                    bk = Bank("f")
                    for c in range(8):
                        P.op("pe", lambda h, c=c: h.matmul(
                            bk.t[0:64, :], lhsT=bufA[:, c, T0 + c8 * 64:T0 + (c8 + 1) * 64], rhs=win[:, c, 5 * 512:6 * 512],
                            start=(c == 0), stop=(c == 7)), reads=rdA + ["win5"], writes=[bk.k()])
                    P.op("act", lambda h: h.activation(out=vtok[0:64, c8, :], in_=bk.t[0:64, :], func=AF.Copy),
                         reads=[bk.k()], writes=["vtok%d" % c8])
                return f

            def head_unit(j, hd, q):
                T0 = j * 512
                rdA = [akey(4 * j + t) for t in range(4)]
                t = TS[q]
                K = lambda n: "%s.%d" % (n, q)
                st_ = {}
                lbh = hp[:, hd:hd + 1]
                lnomlh = hp[:, 8 + hd:9 + hd]
                b3 = t["e"].rearrange("p (c n) -> p c n", c=8)
                sk = "S%d" % hd
                E = []

                def proj(name, s):
                    def f():
                        bk = Bank("f")
                        st_[name] = bk
                        for c in range(8):
                            P.op("pe", lambda h, c=c: h.matmul(
                                bk.t[:, :], lhsT=win[:, c, s * 512 + hd * 128:s * 512 + (hd + 1) * 128],
                                rhs=bufA[:, c, T0:T0 + 512], start=(c == 0), stop=(c == 7)),
                                reads=rdA + ["win%d" % s], writes=[bk.k()])
                    return f
                pz_ = proj("z", 4)

                def e1():
                    bz = st_["z"]
                    P.op("act", lambda h: h.activation(out=t["e"], in_=bz.t[:, :], func=AF.Exp, scale=-1.0), reads=[bz.k()], writes=[K("e")])
                    P.op("act", lambda h: h.activation(out=t["l2"], in_=t["e"], func=AF.Ln, bias=one_ap), reads=[K("e"), "one"], writes=[K("l2")])
                    P.op("act", lambda h: h.activation(out=t["l1"], in_=t["e"], func=AF.Ln, scale=lbh, bias=one_ap),
                         reads=[K("e"), "one", "hp_lb"], writes=[K("l1")])

                def e2():
                    bz = st_["z"]
                    P.op("pool", lambda h: h.tensor_tensor(out=t["l1"], in0=t["l1"], in1=t["l2"], op=ALU.subtract), reads=[K("l1"), K("l2")], writes=[K("l1")])
                    P.op("dve", lambda h: h.scalar_tensor_tensor(out=t["lnk"], in0=bz.t[:, :], scalar=-1.0, in1=t["l2"], op0=ALU.mult, op1=ALU.subtract),
                         reads=[bz.k(), K("l2")], writes=[K("lnk")])
                pq_ = proj("q", 3)

                def e3():
                    P.op("dve", lambda h: h.tensor_tensor_scan(out=t["e"], data0=rmask, data1=t["l1"], initial=0.0, op0=ALU.mult, op1=ALU.add),
                         reads=["cn", K("l1"), K("e")], writes=[K("e")])
                    P.op("pool", lambda h: h.tensor_tensor(out=t["d"].rearrange("p (c n) -> p c n", c=8), in0=b3,
                                                           in1=b3[:, :, 63:64].to_broadcast([128, 8, 64]), op=ALU.subtract),
                         reads=[K("e")], writes=[K("d")])

                def e4():
                    bq = st_["q"]
                    P.op("act", lambda h: h.activation(out=t["l2"], in_=t["d"], func=AF.Exp), reads=[K("d"), K("l2")], writes=[K("l2")])
                    P.op("dve", lambda h: h.tensor_tensor(out=t["qt"], in0=bq.t[:, :], in1=t["l2"], op=ALU.mult), reads=[bq.k(), K("l2")], writes=[K("qt")])
                pg_ = proj("g", 6)

                def e5():
                    P.op("pool", lambda h: h.tensor_tensor(out=t["lnk"], in0=t["lnk"], in1=t["d"], op=ALU.subtract), reads=[K("lnk"), K("d")], writes=[K("lnk")])
                    P.op("act", lambda h: h.activation(out=t["khT"], in_=t["lnk"], func=AF.Exp, bias=lnomlh),
                         reads=[K("lnk"), "hp_ln"], writes=[K("khT")])
                    P.op("act", lambda h: h.activation(out=t["dec"], in_=b3[:, :, 63], func=AF.Exp), reads=[K("e")], writes=[K("dec")])

                def e6():
                    bg = st_["g"]
                    P.op("act", lambda h: h.activation(out=t["l1"], in_=bg.t[:, :], func=AF.Exp, scale=-1.0), reads=[bg.k(), K("l1")], writes=[K("l1")])
                    P.op("act", lambda h: h.activation(out=t["l1"], in_=t["l1"], func=AF.Ln, bias=one_ap), reads=[K("l1"), "one"], writes=[K("l1")])
                    P.op("act", lambda h: h.activation(out=t["l1"], in_=t["l1"], func=AF.Exp, scale=-1.0), reads=[K("l1")], writes=[K("l1")])
                    P.op("dve", lambda h: h.tensor_tensor(out=t["l1"], in0=bg.t[:, :], in1=t["l1"], op=ALU.mult), reads=[bg.k(), K("l1")], writes=[K("l1")])

                def grp(*fs):
                    def f():
                        for g_ in fs:
                            g_()
                    return f
                E.append(grp(pz_, e1, e2))
                E.append(grp(pq_, e3, e4))
                E.append(grp(pg_, e5, e6))

                C = []

                def c0():
                    bkk = Bank("b")
                    for c8 in range(8):
                        P.op("pe", lambda h, c8=c8: h.transpose(out=bkk.t[0:64, c8 * 128:(c8 + 1) * 128], in_=t["khT"][:, c8 * 64:(c8 + 1) * 64],
                                                               identity=identb), reads=[K("khT"), "identb"], writes=[bkk.k()])
                    P.op("act", lambda h: h.activation(out=t["khtok"][0:64, :, :].rearrange("p c n -> p (c n)"), in_=bkk.t[0:64, :], func=AF.Copy),
                         reads=[bkk.k()], writes=[K("khtok")])
                    st_["o"] = Bank("f", hold=True)
                    st_["sc"] = Bank("f", hold=True)
                C.append(c0)

                def c1():
                    bsc = st_["sc"]
                    st_["ds0"] = Bank("f", hold=True)
                    st_["ds1"] = Bank("f", hold=True)
                    for c8 in range(8):
                        cs = slice(c8 * 64, (c8 + 1) * 64)
                        P.op("pe", lambda h, cs=cs: h.matmul(bsc.t[0:64, cs], lhsT=t["khT"][:, cs], rhs=t["qt"][:, cs], start=True, stop=True),
                             reads=[K("khT"), K("qt")], writes=[bsc.k(c8)])
                    for c8 in range(8):
                        bds = st_["ds%d" % (c8 // 4)]
                        ds_cols = slice((c8 % 4) * 128, (c8 % 4 + 1) * 128)
                        P.op("pe", lambda h, c8=c8, bds=bds, ds_cols=ds_cols: h.matmul(bds.t[:, ds_cols], lhsT=t["khtok"][0:64, c8, :],
                                                                                        rhs=vtok[0:64, c8, hd * 128:(hd + 1) * 128], start=True, stop=True),
                             reads=[K("khtok"), "vtok%d" % c8], writes=[bds.k(c8 % 4)])
                C.append(c1)

                def c2():
                    bsc = st_["sc"]
                    for c8 in range(8):
                        cs = slice(c8 * 64, (c8 + 1) * 64)
                        P.op("dve", lambda h, c8=c8, cs=cs: h.tensor_tensor(out=t["scm"][0:64, c8, :], in0=bsc.t[0:64, cs], in1=maskb[0:64, :], op=ALU.mult),
                             reads=[bsc.k(c) for c in range(8)] + ["maskb"], writes=[K("scm%d" % c8)])
                    for c8 in range(8):
                        bds = st_["ds%d" % (c8 // 4)]
                        ds_cols = slice((c8 % 4) * 128, (c8 % 4 + 1) * 128)
                        P.op("dve", lambda h, c8=c8: h.tensor_scalar(out=DSb[c8], in0=Sst[:, hd, :], scalar1=t["dec"][:, c8:c8 + 1], scalar2=None, op0=ALU.mult),
                             reads=[sk, K("dec")], writes=["DSb%d" % c8])
                        P.op("dve", lambda h, c8=c8, bds=bds, ds_cols=ds_cols: h.scalar_tensor_tensor(
                            out=Sst[:, hd, :], in0=Sst[:, hd, :], scalar=t["dec"][:, c8:c8 + 1], in1=bds.t[:, ds_cols], op0=ALU.mult, op1=ALU.add),
                            reads=[sk, K("dec")] + [bds.k(c) for c in range(4)], writes=[sk])
                    st_["sc"].done()
                    st_["ds0"].done()
                    st_["ds1"].done()
                C.append(c2)

                def c3():
                    bo = st_["o"]
                    for c8 in range(8):
                        cs = slice(c8 * 64, (c8 + 1) * 64)
                        P.op("pe", lambda h, c8=c8, cs=cs: h.matmul(bo.t[:, cs], lhsT=DSb[c8], rhs=t["qt"][:, cs], start=True, stop=False),
                             reads=["DSb%d" % c8, K("qt")], writes=[bo.k(c8)])
                        P.op("pe", lambda h, c8=c8, cs=cs: h.matmul(bo.t[:, cs], lhsT=vtok[0:64, c8, hd * 128:(hd + 1) * 128], rhs=t["scm"][0:64, c8, :],
                                                                    start=False, stop=True),
                             reads=["vtok%d" % c8, K("scm%d" % c8)], writes=[bo.k(c8)])
                C.append(c3)

                def c9():
                    bo = st_["o"]
                    okeys = [bo.k(c8) for c8 in range(8)]
                    P.op("act", lambda h: h.activation(out=t["khT"], in_=bo.t[:, :], func=AF.Square), reads=okeys + [K("khT")], writes=[K("khT")])
                    bss = Bank("f")
                    P.op("pe", lambda h: h.matmul(bss.t[:, :], lhsT=onesb, rhs=t["khT"], start=True, stop=True), reads=["onesb", K("khT")], writes=[bss.k()])
                    P.op("act", lambda h: h.activation(out=t["l2"], in_=bss.t[:, :], func=AF.Ln, scale=1.0 / 128, bias=eps_ap),
                         reads=[bss.k(), "eps", K("l2")], writes=[K("l2")])
                    P.op("act", lambda h: h.activation(out=t["l2"], in_=t["l2"], func=AF.Exp, scale=-0.5), reads=[K("l2")], writes=[K("l2")])
                    P.op("dve", lambda h: h.tensor_tensor(out=t["d"], in0=bo.t[:, :], in1=t["l2"], op=ALU.mult), reads=okeys + [K("l2"), K("d")], writes=[K("d")])
                    P.op("dve", lambda h: h.scalar_tensor_tensor(out=bufY[:, 4 + hd, T0:T0 + 512], in0=t["d"], scalar=pc[:, PC_HN + hd:PC_HN + hd + 1],
                                                                 in1=t["l1"], op0=ALU.mult, op1=ALU.mult),
                         reads=[K("d"), K("l1"), "pc"], writes=[ykey(4 + hd, j)])
                    bo.done()
                C.append(c9)
                return E, C

            def interleave(primary, fillers):
                n, m = len(primary), len(fillers)
                fi = 0
                for i, f in enumerate(primary):
                    f()
                    want = ((i + 1) * m) // max(n, 1)
                    while fi < want:
                        fillers[fi]()
                        fi += 1
                while fi < m:
                    fillers[fi]()
                    fi += 1

            for cch in range(4):
                for f in conv_chunk(0, cch):
                    f()
            for c8 in range(8):
                v_group(0, c8)()
            units = [(j, hd) for j in range(NJ) for hd in range(4)]
            E0, Cprev = head_unit(0, 0, 0)
            for f in E0:
                f()
            for u in range(len(units)):
                j, hd = units[u]
                fill = []
                if u + 1 < len(units):
                    jn, hn = units[u + 1]
                    En, Cn = head_unit(jn, hn, (u + 1) % NSET)
                    if jn == j:
                        fill += En
                else:
                    En, Cn = [], []
                if j + 1 < NJ:
                    fill += conv_chunk(j + 1, hd)
                for f in Cprev[0:3]:
                    f()
                for f in fill:
                    f()
                for f in Cprev[3:]:
                    f()
                if u + 1 < len(units) and units[u + 1][0] != j:
                    for c8 in range(8):
                        v_group(j + 1, c8)()
                    for f in En:
                        f()
                Cprev = Cn
            P.barrier()
            if stop_after == "A2":
                for c in (4, 5, 6, 7, 0, 1, 2, 3):
                    for jj in range(2):
                        P.op("pool", lambda h, c=c, jj=jj: h.dma_start(out=dbg_d[:, c * 1024 + jj * 512:c * 1024 + (jj + 1) * 512],
                                                                      in_=bufY[:, c, jj * 512:(jj + 1) * 512]), dma=True)
                return True

            top[0] = RT0
            wout = carve(4096, BF16, "p (c n) -> p c n", c=8)
            xsb = [carve(1024) for _ in range(3)]
            xnb = [carve(512, BF16) for _ in range(3)]
            wq_off = top[0]
            wq = carve(4096, BF16, "p (c n) -> p c n", c=8)
            wo = carve(4096, BF16, "p (c n) -> p c n", c=8)
            P.op("pool", lambda h: h.dma_start(out=wq, in_=wview(w_q_d, 0, D)), writes=["wq"], dma=True)
            P.op("pool", lambda h: h.dma_start(out=wo, in_=wview(w_o_d, 0, D)), writes=["wo"], dma=True)
            emit_casts(14)

            def b1_stage1(i):
                xk = "xsb%d" % (i % 3)
                P.op("sp", lambda h: h.dma_start(out=xsb[i % 3], in_=x_d[tok0 + i * 128:tok0 + (i + 1) * 128, :]), writes=[xk], dma=True)
                for n2 in range(2):
                    bk = Bank("f")
                    for c in range(8):
                        P.op("pe", lambda h, c=c, n2=n2, bk=bk: h.matmul(bk.t[:, :], lhsT=bufY[:, c, i * 128:(i + 1) * 128],
                                                                         rhs=wout[:, c, n2 * 512:(n2 + 1) * 512], start=(c == 0), stop=(c == 7)),
                             reads=["wout"], writes=[bk.k()])
                    P.op("dve", lambda h, n2=n2, bk=bk: h.tensor_tensor(out=xres[:, i, n2 * 512:(n2 + 1) * 512], in0=bk.t[:, :],
                                                                        in1=xsb[i % 3][:, n2 * 512:(n2 + 1) * 512], op=ALU.add),
                         reads=[bk.k(), xk], writes=[xkey(i)])

            b1_stage1(0)
            b1_stage1(1)
            for i in range(NT + 1):
                if i + 2 < NT:
                    b1_stage1(i + 2)
                if i < NT:
                    norm_a(xres[:, i, :], xkey(i), rstd3[:, i, :], "st.%d" % i, xnb[i % 3], "xnb%d" % (i % 3))
                if i >= 1:
                    norm_b(xnb[(i - 1) % 3], "xnb%d" % ((i - 1) % 3), 1, bufA, akey(i - 1), i - 1)
            P.barrier()
            if stop_after == "B1":
                for i in range(8):
                    P.op("pool", lambda h, i=i: h.dma_start(out=dbg_d[:, i * 1024:(i + 1) * 1024], in_=xres[:, i, :]), dma=True)
                return True

            KT = carve_at(RT0, 1024, BF16, "p (c n) -> p c n", c=8)
            Vt = carve_at(RT0 + 1024, 1024, BF16, "p (c n) -> p c n", c=2)
            mstat = carve_at(RT0 + 2048, 4)
            wq = carve_at(wq_off, 4096, BF16, "p (c n) -> p c n", c=8)
            wo = carve_at(wq_off + 4096, 4096, BF16, "p (c n) -> p c n", c=8)
            top[0] = bufY_off
            wkvb = [carve(2048, BF16, "p (c n) -> p c n", c=8) for _ in range(2)]
            mT = carve(1024, BF16, "p (c n) -> p c n", c=8)
            mst = [carve(1024) for _ in range(2)]
            xn2 = [carve(512, BF16) for _ in range(2)]
            assert top[0] <= bufY_off + 8192
            for t in range(2):
                P.op("sp", lambda h, t=t: h.dma_start(out=mst[t], in_=mem_d[hf * 256 + t * 128:hf * 256 + (t + 1) * 128, :]),
                     writes=["mst%d" % t], dma=True)
            for blk in range(2):
                P.op("pool", lambda h, blk=blk: h.dma_start(out=wkvb[blk], in_=wview(w_kv_d, blk * 512, (blk + 1) * 512)), writes=["wkv%d" % blk], dma=True)
            for t in range(2):
                norm_tile(mst[t], "mst%d" % t, 2, mT, "mT", mstat[:, 2 * t:2 * t + 2], "mstat%d" % t, xn2[t], "xn%d" % t, t)
            for blk in range(4):
                wb = wkvb[blk % 2]
                wk = "wkv%d" % (blk % 2)
                if blk >= 2:
                    P.op("pool", lambda h, blk=blk, wb=wb: h.dma_start(out=wb, in_=wview(w_kv_d, blk * 512, (blk + 1) * 512)), writes=[wk], dma=True)
                if blk < 2:
                    for e4 in range(4):
                        ec = blk * 4 + e4
                        bk = Bank("f")
                        for c in range(8):
                            P.op("pe", lambda h, c=c, e4=e4, wb=wb, bk=bk: h.matmul(bk.t[:, 0:256], lhsT=wb[:, c, e4 * 128:(e4 + 1) * 128], rhs=mT[:, c, :],
                                                                                   start=(c == 0), stop=(c == 7)), reads=[wk, "mT"], writes=[bk.k()])
                        P.op("act", lambda h, ec=ec, bk=bk: h.activation(out=KT[:, ec, :], in_=bk.t[:, 0:256], func=AF.Copy), reads=[bk.k()], writes=["KT"])
                else:
                    n2 = blk - 2
                    for mc in range(2):
                        bk = Bank("f")
                        for c in range(8):
                            P.op("pe", lambda h, c=c, mc=mc, wb=wb, bk=bk: h.matmul(bk.t[:, :], lhsT=mT[:, c, mc * 128:(mc + 1) * 128], rhs=wb[:, c, :],
                                                                                   start=(c == 0), stop=(c == 7)), reads=[wk, "mT"], writes=[bk.k()])
                        P.op("act", lambda h, mc=mc, n2=n2, bk=bk: h.activation(out=Vt[:, mc, n2 * 512:(n2 + 1) * 512], in_=bk.t[:, :], func=AF.Copy),
                             reads=[bk.k()], writes=["Vt"])
            emit_casts(100)
            P.barrier()
            top[0] = bufY_off
            QT = carve(2048, BF16, "p (c n) -> p c n", c=8)
            OT = carve(2048, BF16, "p (c n) -> p c n", c=8)
            ET = [carve(512, BF16, "p (c n) -> p c n", c=2) for _ in range(2)]
            rden = [carve(512) for _ in range(2)]

            def q_proj(j):
                T0 = j * 512
                rdA = [akey(4 * j + t) for t in range(4)]
                for ec in range(8):
                    bk = Bank("f")
                    for c in range(8):
                        P.op("pe", lambda h, c=c, ec=ec, bk=bk: h.matmul(bk.t[:, :], lhsT=wq[:, c, ec * 128:(ec + 1) * 128], rhs=bufA[:, c, T0:T0 + 512],
                                                                        start=(c == 0), stop=(c == 7)), reads=rdA + ["wq"], writes=[bk.k()])
                    if ec % 2 == 0:
                        P.op("act", lambda h, ec=ec, bk=bk: h.activation(out=QT[:, ec, :], in_=bk.t[:, :], func=AF.Copy, scale=1.0 / 16.0),
                             reads=[bk.k()], writes=["QT%d" % ec])
                    else:
                        P.op("dve", lambda h, ec=ec, bk=bk: h.tensor_scalar(out=QT[:, ec, :], in0=bk.t[:, :], scalar1=1.0 / 16.0, scalar2=None, op0=ALU.mult),
                             reads=[bk.k()], writes=["QT%d" % ec])

            def s_exp(hd):
                et = ET[hd % 2]
                ek = "ET%d" % (hd % 2)
                for mc in range(2):
                    bk = Bank("f")
                    for k2 in range(2):
                        P.op("pe", lambda h, mc=mc, k2=k2, bk=bk: h.matmul(bk.t[:, :], lhsT=KT[:, 2 * hd + k2, mc * 128:(mc + 1) * 128],
                                                                          rhs=QT[:, 2 * hd + k2, :], start=(k2 == 0), stop=(k2 == 1)),
                             reads=["KT", "QT%d" % (2 * hd + k2)], writes=[bk.k()])
                    P.op("act", lambda h, mc=mc, bk=bk: h.activation(out=et[:, mc, :], in_=bk.t[:, :], func=AF.Exp), reads=[bk.k()], writes=[ek + ".%d" % mc])

            def pv(hd):
                et = ET[hd % 2]
                ek = "ET%d" % (hd % 2)
                bden = Bank("f")
                for mc in range(2):
                    P.op("pe", lambda h, mc=mc: h.matmul(bden.t[:, :], lhsT=onesb, rhs=et[:, mc, :], start=(mc == 0), stop=(mc == 1)),
                         reads=["onesb", ek + ".%d" % mc], writes=[bden.k()])
                rd = rden[hd % 2]
                rk = "rden%d" % (hd % 2)
                P.op("dve", lambda h: h.reciprocal(out=rd, in_=bden.t[:, :]), reads=[bden.k()], writes=[rk])
                for k2 in range(2):
                    bk = Bank("f")
                    for mc in range(2):
                        P.op("pe", lambda h, mc=mc, k2=k2, bk=bk: h.matmul(
                            bk.t[:, :], lhsT=Vt[:, mc, (2 * hd + k2) * 128:(2 * hd + k2 + 1) * 128], rhs=et[:, mc, :], start=(mc == 0), stop=(mc == 1)),
                            reads=["Vt", ek + ".%d" % mc], writes=[bk.k()])
                    P.op("dve", lambda h, k2=k2, bk=bk: h.tensor_tensor(out=OT[:, 2 * hd + k2, :], in0=bk.t[:, :], in1=rd, op=ALU.mult),
                         reads=[bk.k(), rk], writes=["OT%d" % (2 * hd + k2)])

            def w_o(j):
                for tt in range(4):
                    i = 4 * j + tt
                    for n2 in range(2):
                        bk = Bank("f")
                        for ec in range(8):
                            P.op("pe", lambda h, ec=ec, tt=tt, n2=n2, bk=bk: h.matmul(bk.t[:, :], lhsT=OT[:, ec, tt * 128:(tt + 1) * 128],
                                                                                     rhs=wo[:, ec, n2 * 512:(n2 + 1) * 512], start=(ec == 0), stop=(ec == 7)),
                                 reads=["OT%d" % ec, "wo"], writes=[bk.k()])
                        P.op("dve", lambda h, i=i, n2=n2, bk=bk: h.tensor_tensor(out=xres[:, i, n2 * 512:(n2 + 1) * 512], in0=bk.t[:, :],
                                                                                 in1=xres[:, i, n2 * 512:(n2 + 1) * 512], op=ALU.add),
                             reads=[bk.k(), xkey(i)], writes=[xkey(i)])

            q_proj(0)
            for j in range(NJ):
                s_exp(0)
                for hd in range(4):
                    if hd + 1 < 4:
                        s_exp(hd + 1)
                    pv(hd)
                if j + 1 < NJ:
                    q_proj(j + 1)
                w_o(j)
            P.barrier()
            if stop_after == "B2":
                for i in range(8):
                    P.op("pool", lambda h, i=i: h.dma_start(out=dbg_d[:, i * 1024:(i + 1) * 1024], in_=xres[:, i, :]), dma=True)
                return True

            top[0] = bufY_off
            wg = [carve(2048, BF16, "p (c n) -> p c n", c=8) for _ in range(2)]
            wu = [carve(2048, BF16, "p (c n) -> p c n", c=8) for _ in range(2)]
            top[0] = RT0
            wd = [carve(2048, BF16, "p (c n) -> p c n", c=4) for _ in range(2)]
            xn2 = [carve(512, BF16) for _ in range(2)]
            lg = carve(320, F32, "p (i n) -> p i n", i=16)
            comb = carve(256, F32, "p (i n) -> p i n", i=16)
            r_t = [carve(256) for _ in range(4)]
            r_s = [carve(64) for _ in range(6)]
            hid = [carve(1024, BF16, "p (c n) -> p c n", c=4) for _ in range(2)]
            sgb = [carve(256, BF16) for _ in range(2)]

            def wload(e):
                b = e % 2
                P.op("pool", lambda h: h.dma_start(out=wg[b], in_=w_gate_d[e].rearrange("(c p) n -> p c n", p=128)), writes=["wg%d" % b], dma=True)
                P.op("pool", lambda h: h.dma_start(out=wu[b], in_=w_up_d[e].rearrange("(c p) n -> p c n", p=128)), writes=["wu%d" % b], dma=True)
                P.op("pool", lambda h: h.dma_start(out=wd[b], in_=w_down_d[e].rearrange("(c p) n -> p c n", p=128)), writes=["wd%d" % b], dma=True)

            xnc = xn2 + [carve(512, BF16)]

            def router(i):
                bk = Bank("f")
                for c in range(8):
                    P.op("pe", lambda h, c=c: h.matmul(bk.t[:, 0:20], lhsT=bufA[:, c, i * 128:(i + 1) * 128], rhs=wrb[:, c, :],
                                                       start=(c == 0), stop=(c == 7)), reads=[akey(i), "wrb"], writes=[bk.k()])
                P.op("dve", lambda h: h.tensor_tensor(out=lg[:, i, :], in0=bk.t[:, 0:20], in1=rows[:, 1024:1044], op=ALU.add),
                     reads=[bk.k(), "rows"], writes=["lg"])

            for i in range(NT + 1):
                if i < NT:
                    norm_a(xres[:, i, :], xkey(i), rstd3[:, i, :], "st.%d" % i, xnc[i % 3], "xnc%d" % (i % 3))
                    P.op("sp", lambda h, i=i: h.dma_start(out=h3_d[i * 128:(i + 1) * 128, :], in_=xnc[i % 3]), reads=["xnc%d" % (i % 3)],
                         writes=["H3.%d" % i], dma=True)
                if i >= 1:
                    norm_b(xnc[(i - 1) % 3], "xnc%d" % ((i - 1) % 3), 3, bufA, akey(i - 1), i - 1)
                    router(i - 1)
            LG = lg[:, :, 0:4]
            LE = lg[:, :, 4:20].rearrange("p i (j k) -> p i j k", j=4)
            gmax, gsum, m1, m2, w1, w2 = [r[:, 0:16] for r in r_s]
            gsh = r_t[0][:, 0:64].rearrange("p (i j) -> p i j", i=16)
            gm = r_t[1][:, 0:64].rearrange("p (i j) -> p i j", i=16)
            tmp4 = r_t[2].rearrange("p (i j k) -> p i j k", i=16, j=4)
            esel = r_t[3][:, 0:64].rearrange("p (i k) -> p i k", i=16)
            mk1 = r_t[3][:, 64:128].rearrange("p (i k) -> p i k", i=16)
            e2 = r_t[3][:, 128:192].rearrange("p (i k) -> p i k", i=16)
            mk2 = r_t[3][:, 192:256].rearrange("p (i k) -> p i k", i=16)
            cig = r_t[0][:, 64:128].rearrange("p (i k) -> p i k", i=16)
            tq = r_t[0][:, 128:192].rearrange("p (i k) -> p i k", i=16)

            def bc3(a):
                return a.unsqueeze(2).to_broadcast([128, 16, 4])

            R = lambda fn, eng="dve": P.op(eng, fn, reads=["lg", "rt"], writes=["rt"])
            R(lambda h: h.tensor_reduce(out=gmax, in_=LG, axis=AX.X, op=ALU.max))
            R(lambda h: h.tensor_tensor(out=gsh, in0=LG, in1=bc3(gmax), op=ALU.subtract))
            R(lambda h: h.tensor_single_scalar(out=gm, in_=gsh, scalar=0.0, op=ALU.is_ge))
            R(lambda h: h.activation(out=gsh, in_=gsh, func=AF.Exp), "act")
            R(lambda h: h.tensor_reduce(out=gsum, in_=gsh, axis=AX.X, op=ALU.add))
            R(lambda h: h.reciprocal(out=gsum, in_=gsum))
            R(lambda h: h.tensor_tensor(out=tmp4, in0=LE, in1=gm.unsqueeze(3).to_broadcast([128, 16, 4, 4]), op=ALU.mult))
            R(lambda h: h.tensor_reduce(out=esel, in_=tmp4.rearrange("p i j k -> p i k j"), axis=AX.X, op=ALU.add))
            R(lambda h: h.tensor_reduce(out=m1, in_=esel, axis=AX.X, op=ALU.max))
            R(lambda h: h.tensor_tensor(out=mk1, in0=esel, in1=bc3(m1), op=ALU.is_ge))
            R(lambda h: h.scalar_tensor_tensor(out=e2, in0=mk1, scalar=-1e30, in1=esel, op0=ALU.mult, op1=ALU.add))
            R(lambda h: h.tensor_reduce(out=m2, in_=e2, axis=AX.X, op=ALU.max))
            R(lambda h: h.tensor_tensor(out=mk2, in0=e2, in1=bc3(m2), op=ALU.is_ge))
            R(lambda h: h.tensor_tensor(out=w2, in0=m2, in1=m1, op=ALU.subtract))
            R(lambda h: h.activation(out=w2, in_=w2, func=AF.Exp), "act")
            R(lambda h: h.tensor_scalar_add(out=w1, in0=w2, scalar1=1.0))
            R(lambda h: h.reciprocal(out=w1, in_=w1))
            R(lambda h: h.tensor_tensor(out=w2, in0=w2, in1=w1, op=ALU.mult))
            R(lambda h: h.tensor_tensor(out=w1, in0=w1, in1=gsum, op=ALU.mult))
            R(lambda h: h.tensor_tensor(out=w2, in0=w2, in1=gsum, op=ALU.mult))
            R(lambda h: h.tensor_tensor(out=cig, in0=mk1, in1=bc3(w1), op=ALU.mult))
            R(lambda h: h.tensor_tensor(out=tq, in0=mk2, in1=bc3(w2), op=ALU.mult))
            R(lambda h: h.tensor_tensor(out=cig, in0=cig, in1=tq, op=ALU.add))
            P.op("dve", lambda h: h.tensor_tensor(out=comb.rearrange("p i (j k) -> p i j k", j=4), in0=gm.unsqueeze(3).to_broadcast([128, 16, 4, 4]),
                                                  in1=cig.unsqueeze(2).to_broadcast([128, 16, 4, 4]), op=ALU.mult), reads=["rt"], writes=["comb"])
            if stop_after == "R":
                P.barrier()
                P.op("pool", lambda h: h.dma_start(out=dbg_d[:, 0:256], in_=comb.rearrange("p i n -> p (i n)")), dma=True)
                P.op("pool", lambda h: h.dma_start(out=dbg_d[:, 256:576], in_=lg.rearrange("p i n -> p (i n)")), dma=True)
                return True

            NU = 23
            NSLOT = NU * 512
            oh = carve(512, F32, "p (a i e) -> p a i e", a=2, i=16)
            A_bf = carve(128, BF16)
            pit = carve(256, F32, "p (i e) -> p i e", i=16)
            tot = carve(256, F32, "p (i e) -> p i e", i=16)
            off = carve(256, F32, "p (i e) -> p i e", i=16)
            sm = carve(80)
            ne, un, cu, cui, base = [sm[:, k * 16:(k + 1) * 16] for k in range(5)]
            slf = carve(32, F32, "p (a i) -> p a i", a=2)
            sli = carve(32).bitcast(I32).rearrange("p (a i) -> p a i", a=2)
            cmpb = carve(NU * 16, F32, "p (s e) -> p s e", s=NU)
            euf = carve(32)
            wix = carve(32).bitcast(I32)
            tokid = carve(16).bitcast(I32)
            uidx = carve(92).bitcast(I32)
            zer = carve(92).bitcast(I32)
            Ub = carve(64, BF16)
            P.op("dve", lambda h: h.tensor_copy(out=Ub, in_=cn[:, CN_U:CN_U + 128]), reads=["cn"], writes=["Ub"])
            P.op("pool", lambda h: h.iota(tokid, pattern=[[128, 16]], base=0, channel_multiplier=1), writes=["tokid"])
            P.op("dve", lambda h: h.memset(zer, 0), writes=["zer"])
            P.op("sp", lambda h: h.dma_start(out=tokslot_d.rearrange("(p n) o -> p (n o)", p=128), in_=zer), reads=["zer"], writes=["ts0"], dma=True)
            S = lambda fn, eng="dve": P.op(eng, fn, reads=["rt", "cn", "Ub"], writes=["rt"])
            gm4 = gm.unsqueeze(3).to_broadcast([128, 16, 4, 4])
            S(lambda h: h.tensor_tensor(out=oh[:, 0].rearrange("p i (j k) -> p i j k", j=4), in0=gm4,
                                        in1=mk1.unsqueeze(2).to_broadcast([128, 16, 4, 4]), op=ALU.mult))
            S(lambda h: h.tensor_tensor(out=oh[:, 1].rearrange("p i (j k) -> p i j k", j=4), in0=gm4,
                                        in1=mk2.unsqueeze(2).to_broadcast([128, 16, 4, 4]), op=ALU.mult))
            S(lambda h: h.tensor_tensor(out=A_bf, in0=oh[:, 0].rearrange("p i e -> p (i e)"), in1=oh[:, 1].rearrange("p i e -> p (i e)"), op=ALU.add))
            bkp, bkt = Bank("f"), Bank("f")
            P.op("pe", lambda h: h.matmul(bkp.t[:, 0:256], lhsT=Ub, rhs=A_bf, start=True, stop=True), reads=["rt", "Ub"], writes=[bkp.k()])
            P.op("pe", lambda h: h.matmul(bkt.t[:, 0:256], lhsT=onesb, rhs=A_bf, start=True, stop=True), reads=["rt", "onesb"], writes=[bkt.k()])
            P.op("dve", lambda h: h.tensor_copy(out=pit.rearrange("p i e -> p (i e)"), in_=bkp.t[:, 0:256]), reads=[bkp.k(), "rt"], writes=["rt"])
            P.op("act", lambda h: h.activation(out=tot.rearrange("p i e -> p (i e)"), in_=bkt.t[:, 0:256], func=AF.Copy), reads=[bkt.k(), "rt"], writes=["rt"])
            S(lambda h: h.memset(off[:, 0, :], 0.0))
            for i in range(1, 16):
                S(lambda h, i=i: h.tensor_tensor(out=off[:, i, :], in0=off[:, i - 1, :], in1=tot[:, i - 1, :], op=ALU.add))
            S(lambda h: h.tensor_tensor(out=ne, in0=off[:, 15, :], in1=tot[:, 15, :], op=ALU.add))
            S(lambda h: h.tensor_single_scalar(out=un, in_=ne, scalar=0.0, op=ALU.is_gt))
            for thr in (512.0, 1024.0, 1536.0):
                S(lambda h, thr=thr: h.scalar_tensor_tensor(out=un, in0=ne, scalar=thr, in1=un, op0=ALU.is_gt, op1=ALU.add))
            S(lambda h: h.memset(cu[:, 0:1], 0.0))
            for e in range(1, 16):
                S(lambda h, e=e: h.tensor_tensor(out=cu[:, e:e + 1], in0=cu[:, e - 1:e], in1=un[:, e - 1:e], op=ALU.add))
            S(lambda h: h.tensor_tensor(out=cui, in0=cu, in1=un, op=ALU.add))
            S(lambda h: h.tensor_single_scalar(out=base, in_=cu, scalar=512.0, op=ALU.mult))
            S(lambda h: h.tensor_tensor(out=pit, in0=pit, in1=off, op=ALU.add))
            S(lambda h: h.tensor_tensor(out=pit, in0=pit, in1=base.unsqueeze(1).to_broadcast([128, 16, 16]), op=ALU.add))
            for a in range(2):
                S(lambda h, a=a: h.tensor_tensor(out=oh[:, a], in0=oh[:, a], in1=pit, op=ALU.mult))
                S(lambda h, a=a: h.tensor_reduce(out=slf[:, a, :], in_=oh[:, a], axis=AX.X, op=ALU.add))
            S(lambda h: h.tensor_copy(out=sli, in_=slf))
            S(lambda h: h.tensor_tensor(out=cmpb, in0=cui.unsqueeze(1).to_broadcast([128, NU, 16]),
                                        in1=cn[:, CN_IOTA:CN_IOTA + NU].unsqueeze(2).to_broadcast([128, NU, 16]), op=ALU.is_le))
            S(lambda h: h.tensor_reduce(out=euf[:, 0:NU], in_=cmpb, axis=AX.X, op=ALU.add))
            S(lambda h: h.tensor_scalar(out=euf[:, 0:NU], in0=euf[:, 0:NU], scalar1=15.0, scalar2=128.0, op0=ALU.min, op1=ALU.mult))
            S(lambda h: h.tensor_scalar(out=euf[:, 0:NU], in0=euf[:, 0:NU], scalar1=cn[:, CN_PIO:CN_PIO + 1], scalar2=None, op0=ALU.add))
            S(lambda h: h.tensor_copy(out=wix[:, 0:NU], in_=euf[:, 0:NU]))
            for a in range(2):
                for i in range(16):
                    P.op("pool", lambda h, a=a, i=i: h.indirect_dma_start(
                        out=tokslot_d, out_offset=bass.IndirectOffsetOnAxis(ap=sli[:, a, i:i + 1].bitcast(U32), axis=0),
                        in_=tokid[:, i:i + 1], in_offset=None), reads=["rt", "tokid", "ts0"], writes=["ts.%d.%d" % (a, i)], dma=True)
            tskeys = ["ts.%d.%d" % (a, i) for a in range(2) for i in range(16)]
            for col in range(NU * 4):
                P.op("sp", lambda h, col=col: h.dma_start(out=uidx[:, col:col + 1], in_=tokslot_d[col * 128:(col + 1) * 128, :]),
                     reads=tskeys + ["ts0"], writes=["uidx%d" % col], dma=True)
            if stop_after == "SL":
                P.barrier()
                P.op("sp", lambda h: h.dma_start(out=dbg_d[:, 0:32], in_=slf.rearrange("p a i -> p (a i)")), dma=True)
                P.op("sp", lambda h: h.dma_start(out=dbg_d[:, 32:64], in_=euf), dma=True)
                P.op("sp", lambda h: h.dma_start(out=dbg_d[:, 64:96], in_=w1.to_broadcast([128, 16]) if False else r_s[4][:, 0:32]), dma=True)
                return True
            P.barrier()

            xgT = [carve_at(bufA_off + q * 2048, 2048, BF16, "p (c n) -> p c n", c=8) for q in range(2)]
            xg = [carve_at(bufA_off + 4096 + q * 512, 512, BF16) for q in range(4)] + [carve(512, BF16) for q in range(4)]
            yst = [carve_at(bufA_off + 6144 + q * 1024, 1024) for q in range(2)]
            H3keys = ["H3.%d" % i for i in range(NT)]

            def pre_tok(u):
                for r in range(4):
                    col = u * 4 + r
                    q = (u % 2) * 4 + r
                    P.op("pool", lambda h, q=q, col=col: h.indirect_dma_start(
                        out=xg[q], out_offset=None, in_=h3_d,
                        in_offset=bass.IndirectOffsetOnAxis(ap=uidx[:, col:col + 1].bitcast(U32), axis=0)),
                        reads=["uidx%d" % col] + H3keys, writes=["xg%d" % q], dma=True)

            def tr(u):
                b = u % 2
                for r in range(4):
                    q = b * 4 + r
                    norm_b(xg[q], "xg%d" % q, 3, xgT[b], "xgT%d.%d" % (b, r), r)

            def pre_w(u):
                b = u % 2
                for (wl, dst, nm, cn_) in ((wgb_d, wg[b], "wg%d" % b, "g"), (wub_d, wu[b], "wu%d" % b, "u")):
                    for hh in range(2):
                        P.op("pool", lambda h, wl=wl, dst=dst, hh=hh: h.indirect_dma_start(
                            out=dst[:, 4 * hh:4 * hh + 4, :].rearrange("p c n -> p (c n)"), out_offset=None, in_=wl[hh],
                            in_offset=bass.IndirectOffsetOnAxis(ap=wix[:, u:u + 1].bitcast(U32), axis=0)),
                            reads=["rt"] + cast_keys(cn_, hh), writes=[nm + ".%d" % hh], dma=True)

            def pre_d(u):
                b = u % 2
                for hh in range(2):
                    P.op("pool", lambda h, hh=hh: h.indirect_dma_start(
                        out=wd[b][:, 2 * hh:2 * hh + 2, :].rearrange("p c n -> p (c n)"), out_offset=None, in_=wdb_d[hh],
                        in_offset=bass.IndirectOffsetOnAxis(ap=wix[:, u:u + 1].bitcast(U32), axis=0)),
                        reads=["rt"] + cast_keys("d", hh), writes=["wd%d.%d" % (b, hh)], dma=True)

            def gate_up(u):
                b = u % 2
                hb = hid[b]
                rdx = ["xgT%d.%d" % (b, r) for r in range(4)]
                for f in range(4):
                    bg_, bu_ = Bank("f"), Bank("f")
                    for (bk, w, wk) in ((bg_, wg[b], "wg%d" % b), (bu_, wu[b], "wu%d" % b)):
                        for c in range(8):
                            P.op("pe", lambda h, c=c, f=f, bk=bk, w=w: h.matmul(bk.t[:, :], lhsT=w[:, c, f * 128:(f + 1) * 128], rhs=xgT[b][:, c, :],
                                                                                start=(c == 0), stop=(c == 7)), reads=rdx + [wk + ".%d" % (c // 4)], writes=[bk.k()])
                    sg = sgb[f % 2]
                    P.op("act", lambda h, sg=sg, bg_=bg_: h.activation(out=sg, in_=bg_.t[:, :], func=AF.Silu), reads=[bg_.k()], writes=["sgb%d" % (f % 2)])
                    P.op("dve", lambda h, f=f, sg=sg, bu_=bu_: h.tensor_tensor(out=hb[:, f, :], in0=bu_.t[:, :], in1=sg, op=ALU.mult),
                         reads=[bu_.k(), "sgb%d" % (f % 2)], writes=["hid%d.%d" % (b, f)])

            def down(u):
                b = u % 2
                hb = hid[b]
                for tt in range(4):
                    ys = yst[tt % 2]
                    yk = "yst%d" % (tt % 2)
                    for n2 in range(2):
                        bk = Bank("f")
                        for f in range(4):
                            P.op("pe", lambda h, f=f, tt=tt, n2=n2, bk=bk: h.matmul(bk.t[:, :], lhsT=hb[:, f, tt * 128:(tt + 1) * 128],
                                                                                   rhs=wd[b][:, f, n2 * 512:(n2 + 1) * 512], start=(f == 0), stop=(f == 3)),
                                 reads=["hid%d.%d" % (b, f), "wd%d.%d" % (b, f // 2)], writes=[bk.k()])
                        if n2 == 0:
                            P.op("act", lambda h, ys=ys, bk=bk: h.activation(out=ys[:, 0:512], in_=bk.t[:, :], func=AF.Copy), reads=[bk.k()], writes=[yk + ".0"])
                        else:
                            P.op("dve", lambda h, ys=ys, bk=bk: h.tensor_copy(out=ys[:, 512:1024], in_=bk.t[:, :]), reads=[bk.k()], writes=[yk + ".1"])
                    row0 = (u * 4 + tt) * 128
                    P.op("sp", lambda h, ys=ys, row0=row0: h.dma_start(out=y_d[row0:row0 + 128, :], in_=ys), reads=[yk + ".0", yk + ".1"],
                         writes=["Y.%d" % (u * 4 + tt)], dma=True)

            pre_tok(0)
            pre_tok(1)
            pre_w(0)
            pre_d(0)
            tr(0)
            for u in range(NU + 1):
                if u + 2 < NU:
                    pre_tok(u + 2)
                if u + 1 < NU:
                    tr(u + 1)
                    pre_w(u + 1)
                if u < NU:
                    gate_up(u)
                if u >= 1:
                    down(u - 1)
                if u + 1 < NU:
                    pre_d(u + 1)
            P.barrier()
            yg = [carve_at(bufA_off + q * 1024, 1024) for q in range(4)]
            fin = rows[:, 0:1024]
            for i in range(NT):
                for a in range(2):
                    q = (2 * i + a) % 4
                    P.op("pool", lambda h, a=a, i=i, q=q: h.indirect_dma_start(
                        out=yg[q], out_offset=None, in_=y_d,
                        in_offset=bass.IndirectOffsetOnAxis(ap=sli[:, a, i:i + 1].bitcast(U32), axis=0)),
                        writes=["yg%d" % q], dma=True)
                    wa = (w1, w2)[a]
                    P.op("dve", lambda h, i=i, q=q, wa=wa: h.scalar_tensor_tensor(out=xres[:, i, :], in0=yg[q], scalar=wa[:, i:i + 1], in1=xres[:, i, :],
                                                                                 op0=ALU.mult, op1=ALU.add), reads=["yg%d" % q, xkey(i)], writes=[xkey(i)])
                stt = rstd3[:, i, :]
                sk_ = "st.%d" % i
                P.op("act", lambda h, i=i, stt=stt: h.activation(out=xn2[i % 2], in_=xres[:, i, :], func=AF.Square, accum_out=stt[:, 0:1]),
                     reads=[xkey(i)], writes=["xn%d" % (i % 2), sk_])
                P.op("act", lambda h, stt=stt: h.activation(out=stt[:, 1:2], in_=stt[:, 0:1], func=AF.Ln, scale=1.0 / D, bias=eps_ap), reads=[sk_, "eps"], writes=[sk_])
                P.op("act", lambda h, stt=stt: h.activation(out=stt[:, 1:2], in_=stt[:, 1:2], func=AF.Exp, scale=-0.5), reads=[sk_], writes=[sk_])
                P.op("dve", lambda h, i=i, stt=stt: h.scalar_tensor_tensor(out=xres[:, i, :], in0=xres[:, i, :], scalar=stt[:, 1:2], in1=fin, op0=ALU.mult, op1=ALU.mult),
                     reads=[xkey(i), sk_, "rows"], writes=[xkey(i)])
                P.op("sp", lambda h, i=i: h.dma_start(out=out_d[tok0 + i * 128:tok0 + (i + 1) * 128, :], in_=xres[:, i, :]),
                     reads=[xkey(i)], writes=["out.%d.%d" % (hf, i)], dma=True)
            P.barrier()
            return False

        for hf_ in range(nhalves):
            if do_half(hf_):
                break
        P.emit()
    return nc


_CACHE = {}


def _host_consts():
    cn = np.zeros((128, CN_N), np.float32)
    cn[:, CN_ID:CN_ID + 128] = np.eye(128, dtype=np.float32)
    cn[:, CN_ONE:CN_ONE + 128] = 1.0
    s = np.arange(64)[:, None]
    t = np.arange(64)[None, :]
    cn[0:64, CN_MASK:CN_MASK + 64] = (s <= t).astype(np.float32)
    rm = np.ones(512, np.float32)
    rm[::64] = 0.0
    cn[:, CN_RM:CN_RM + 512] = rm[None, :]
    kk = np.arange(128)[:, None]
    mm = np.arange(128)[None, :]
    cn[:, CN_U:CN_U + 128] = (kk < mm).astype(np.float32)
    cn[:, CN_IOTA:CN_IOTA + 32] = np.arange(32, dtype=np.float32)[None, :]
    cn[:, CN_PIO] = np.arange(128, dtype=np.float32)
    return cn


def _col(v, nchunk):
    return np.ascontiguousarray(np.asarray(v, np.float32).reshape(nchunk, 128).T)


def _rowlay(name, w, nchunk):
    E, R, n = w.shape
    t = w.reshape(E, nchunk, 128, n).transpose(0, 2, 1, 3).reshape(E * 128, nchunk * n)
    hlf = nchunk * n // 2
    return {name + "0": np.ascontiguousarray(t[:, :hlf]), name + "1": np.ascontiguousarray(t[:, hlf:])}


def make_in_maps(inputs):
    f = lambda k: np.asarray(inputs[k], np.float32)
    pcols = np.zeros((128, PC_N), np.float32)
    for gi, k in enumerate(["mix_norm", "xattn_norm", "mem_norm", "ffn_norm"]):
        pcols[:, PC_G + gi * 8:PC_G + gi * 8 + 8] = _col(f(k)[0], 8)
    cw = f("conv_w")[0]
    for cch in range(4):
        for jj in range(3):
            pcols[:, PC_CONV + cch * 3 + jj] = cw[jj, cch * 128:(cch + 1) * 128]
    lbr = f("hgrn_lb")
    for r in range(2):
        pcols[:, PC_LB + r * 4:PC_LB + r * 4 + 4] = _col(lbr[r], 4)
    pcols[:, PC_HN:PC_HN + 4] = _col(f("hgrn_norm")[0], 4)
    wrc = np.concatenate([f("w_group")[0], f("w_expert")[0]], axis=1)
    wr = np.ascontiguousarray(wrc.reshape(8, 128, 20).transpose(1, 0, 2).reshape(128, 160))
    rows = np.concatenate([f("final_norm").reshape(-1), f("b_group")[0], f("b_expert")[0]])[None, :].astype(np.float32)
    consts = _host_consts()
    x = f("x")
    mem = f("mem")
    shared = dict(
        w_in=np.ascontiguousarray(f("w_in")[0]), w_out=np.ascontiguousarray(f("w_out")[0]),
        w_q=np.ascontiguousarray(f("w_q")[0]), w_kv=np.ascontiguousarray(f("w_kv")[0]), w_o=np.ascontiguousarray(f("w_o")[0]),
        **_rowlay("wgl", f("w_gate")[0], 8), **_rowlay("wul", f("w_up")[0], 8), **_rowlay("wdl", f("w_down")[0], 4),
        pcols=pcols, wr=wr, rows=np.ascontiguousarray(rows), consts=consts)
    maps = []
    for c in range(NCORES):
        m = dict(shared)
        m["x"] = np.ascontiguousarray(x[2 * c:2 * c + 2].reshape(2 * SEQ, D))
        m["mem"] = np.ascontiguousarray(mem[2 * c:2 * c + 2].reshape(512, D))
        maps.append(m)
    return maps


def kernel(**inputs):
    if "nc" not in _CACHE:
        _CACHE["nc"] = build_program()
    nc = _CACHE["nc"]
    maps = make_in_maps(inputs)
    res = run_bass_kernel_spmd(nc, maps, core_ids=list(range(NCORES)))
    outs = [np.asarray(r["out"], np.float32).reshape(2, SEQ, D) for r in res.results]
    return np.concatenate(outs, axis=0)
```

```python
import contextlib
import numpy as np
import concourse.bass as bass
import concourse.mybir as mybir
from concourse.bass_utils import run_bass_kernel_spmd

F32 = mybir.dt.float32
BF16 = mybir.dt.bfloat16
I32 = mybir.dt.int32
U32 = mybir.dt.uint32
AF = mybir.ActivationFunctionType
ALU = mybir.AluOpType
AX = mybir.AxisListType

ENGS = ("pe", "act", "dve", "pool", "sp")
EPS = 1e-6
NCORES = 8
SEQ = 2048
D = 1024
NT = 16
NJ = 4
NEXP = 16


class Prog:
    NDMA_SEMS = 24
    NBG_SEMS = 6

    def __init__(self, nc):
        self.nc = nc
        self.ops = []
        self.last_w = {}
        self.readers = {}
        self.dma_rr = [0, 0]
        self.bg_rr = 0
        self.bg_last = {}
        self.dma_last = {}
        self.last_on = {}

    def alias(self, old_keys, new_key):
        s = set()
        for k in old_keys:
            w = self.last_w.get(k)
            if w is not None:
                s.add(w)
            s.update(self.readers.get(k, ()))
        self.last_w[new_key] = None
        self.readers[new_key] = list(s)

    def op(self, eng, fn, reads=(), writes=(), dma=False, extra=(), bg=False):
        oid = len(self.ops)
        deps = set(extra)
        for k in reads:
            w = self.last_w.get(k)
            if w is not None:
                deps.add(w)
        for k in writes:
            w = self.last_w.get(k)
            if w is not None:
                deps.add(w)
            for r in self.readers.get(k, ()):
                deps.add(r)
        deps.discard(oid)
        rec = dict(id=oid, eng=eng, fn=fn, deps=deps, dma=dma, sig=False, dsem=None)
        if dma and bg:
            s = self.NDMA_SEMS + (self.bg_rr % self.NBG_SEMS)
            self.bg_rr += 1
            rec["dsem"] = s
            prev = self.bg_last.get(s)
            if prev is not None:
                deps.add(prev)
            self.bg_last[s] = oid
        elif dma:
            half = self.NDMA_SEMS // 2
            q = 0 if eng == "pool" else 1
            s = q * half + (self.dma_rr[q] % half)
            self.dma_rr[q] += 1
            rec["dsem"] = s
            prev = self.dma_last.get(s)
            if prev is not None:
                deps.add(prev)
            self.dma_last[s] = oid
        self.ops.append(rec)
        if fn is not None and not bg:
            self.last_on[eng] = oid
        for k in writes:
            self.last_w[k] = oid
            self.readers[k] = []
        for k in reads:
            if k not in writes:
                self.readers.setdefault(k, []).append(oid)
        return oid

    def barrier(self):
        tails = set(self.last_on.values()) | set(self.dma_last.values())
        for e in ENGS:
            self.op(e, None, extra=set(tails))
        keepw = {k: v for k, v in self.last_w.items() if k.startswith("keep:")}
        self.last_w = keepw
        self.readers = {}

    def emit(self):
        nc = self.nc
        ops = self.ops
        for o in ops:
            if o["eng"] == "pe" and not o["dma"]:
                o["deps"] = {d for d in o["deps"] if ops[d]["dma"] or ops[d]["eng"] != "pe"}
        for o in ops:
            for d in o["deps"]:
                ops[d]["sig"] = True
        cnt = {e: 0 for e in ENGS}
        dcnt = {}
        for o in ops:
            if o["dma"]:
                s = o["dsem"]
                dcnt[s] = dcnt.get(s, 0) + 16
                o["ev"] = ("d%d" % s, dcnt[s])
            elif o["sig"]:
                cnt[o["eng"]] += 1
                o["ev"] = (o["eng"], cnt[o["eng"]])
        with contextlib.ExitStack() as st:
            sems = {}
            for e in ENGS:
                sems[e] = st.enter_context(nc.semaphore("sem_" + e))
            for s in range(self.NDMA_SEMS + self.NBG_SEMS):
                sems["d%d" % s] = st.enter_context(nc.semaphore("sem_d%d" % s))
            block = st.enter_context(nc.Block())
            per = {e: [o for o in ops if o["eng"] == e] for e in ENGS}

            def run(e, handle):
                waited = {}
                for o in per[e]:
                    need = {}
                    for d in o["deps"]:
                        sname, val = ops[d]["ev"]
                        if need.get(sname, 0) < val:
                            need[sname] = val
                    for sname, val in need.items():
                        if waited.get(sname, 0) >= val:
                            continue
                        handle.wait_ge(sems[sname], val)
                        waited[sname] = val
                    if o["fn"] is None:
                        continue
                    ins = o["fn"](handle)
                    if o["dma"]:
                        ins.then_inc(sems[o["ev"][0]], 16)
                    elif o["sig"]:
                        ins.then_inc(sems[e], 1)

            @block.tensor
            def _(h):
                run("pe", h)

            @block.scalar
            def _(h):
                run("act", h)

            @block.vector
            def _(h):
                run("dve", h)

            @block.gpsimd
            def _(h):
                run("pool", h)

            @block.sync
            def _(h):
                run("sp", h)
                for s, v in dcnt.items():
                    h.wait_ge(sems["d%d" % s], v)


PC_G = 0
PC_CONV = 32
PC_LB = 44
PC_HN = 52
PC_N = 56
CN_ID = 0
CN_ONE = 128
CN_MASK = 256
CN_RM = 320
CN_U = 832
CN_IOTA = 960
CN_PIO = 992
CN_N = 1024


def build_program(stop_after=None, nhalves=2):
    nc = bass.Bass("TRN2", target_bir_lowering=False)

    def dram(name, shape, kind="ExternalInput"):
        return nc.dram_tensor(name, shape, F32, kind=kind).ap()

    x_d = dram("x", [2 * SEQ, D])
    mem_d = dram("mem", [512, D])
    w_in_d = dram("w_in", [D, 3584])
    w_out_d = dram("w_out", [D, D])
    w_q_d = dram("w_q", [D, D])
    w_kv_d = dram("w_kv", [D, 2 * D])
    w_o_d = dram("w_o", [D, D])
    wgl_d = [dram("wgl%d" % q, [NEXP * 128, 2048]) for q in range(2)]
    wul_d = [dram("wul%d" % q, [NEXP * 128, 2048]) for q in range(2)]
    wdl_d = [dram("wdl%d" % q, [NEXP * 128, 2048]) for q in range(2)]
    wgb_d = [nc.dram_tensor("wgb%d" % q, [NEXP * 128, 2048], BF16, kind="Internal").ap() for q in range(2)]
    wub_d = [nc.dram_tensor("wub%d" % q, [NEXP * 128, 2048], BF16, kind="Internal").ap() for q in range(2)]
    wdb_d = [nc.dram_tensor("wdb%d" % q, [NEXP * 128, 2048], BF16, kind="Internal").ap() for q in range(2)]
    h3_d = nc.dram_tensor("h3s", [2 * SEQ, D], BF16, kind="Internal").ap()
    x2_d = nc.dram_tensor("x2s", [2 * SEQ, D], F32, kind="Internal").ap()
    tokslot_d = nc.dram_tensor("tokslot", [31 * 512, 1], I32, kind="Internal").ap()
    y_d = nc.dram_tensor("ysc", [31 * 512, D], F32, kind="Internal").ap()
    pcols_d = dram("pcols", [128, PC_N])
    wr_d = dram("wr", [128, 160])
    rows_d = dram("rows", [1, 1044])
    consts_d = dram("consts", [128, CN_N])
    out_d = dram("out", [2 * SEQ, D], kind="ExternalOutput")
    dbg_d = dram("dbg", [128, 8192], kind="ExternalOutput") if stop_after else None

    st = contextlib.ExitStack()
    with st:
        NW = 53100
        big = st.enter_context(nc.sbuf_tensor("big", [128, NW], F32))
        psf = [st.enter_context(nc.psum_tensor("psf%d" % i, [128, 512], F32)) for i in range(6)]
        psb = [st.enter_context(nc.psum_tensor("psb%d" % i, [128, 1024], BF16)) for i in range(2)]
        P = Prog(nc)

        top = [0]

        def carve(words, dtype=F32, pattern=None, **kw):
            off = top[0]
            top[0] += words
            assert top[0] <= NW, "SBUF arena overflow %d" % top[0]
            ap = big[:, off:off + words]
            if dtype == BF16:
                ap = ap.bitcast(BF16)
            if pattern:
                ap = ap.rearrange(pattern, **kw)
            return ap

        bank_state = {}
        rr = {"f": 0, "b": 0}

        held = set()

        class Bank:
            def __init__(self, kind, hold=False):
                n = 6 if kind == "f" else 2
                for _ in range(n):
                    self.idx = rr[kind] % n
                    rr[kind] += 1
                    if (kind, self.idx) not in held:
                        break
                else:
                    raise RuntimeError("all PSUM banks held")
                self.kind = kind
                self.gen = rr[kind]
                self.t = psf[self.idx] if kind == "f" else psb[self.idx]
                self.name = "%s%d" % (kind, self.idx)
                self.old = bank_state.get(self.name, [])
                self.keys = {}
                bank_state[self.name] = []
                if hold:
                    held.add((kind, self.idx))

            def done(self):
                held.discard((self.kind, self.idx))

            def k(self, sub=0):
                if sub not in self.keys:
                    key = "ps.%s.%d.%s" % (self.name, self.gen, sub)
                    P.alias(self.old, key)
                    self.keys[sub] = key
                    bank_state[self.name].append(key)
                return self.keys[sub]

        pc = carve(PC_N)
        cn = carve(CN_N)
        identb = carve(64, BF16)
        onesb = carve(64, BF16)
        maskb = carve(32, BF16)
        wr = carve(160, F32, "p (c n) -> p c n", c=8)
        wrg = carve(160, F32, "p (c n) -> p c n", c=8)
        rows = carve(1044)
        hp = carve(16)
        ident = cn[:, CN_ID:CN_ID + 128]
        rmask = cn[:, CN_RM:CN_RM + 512]
        base_top = top[0]

        P.op("sp", lambda h: h.dma_start(out=pc, in_=pcols_d), writes=["pc"], dma=True)
        P.op("sp", lambda h: h.dma_start(out=cn, in_=consts_d), writes=["cn"], dma=True)
        P.op("sp", lambda h: h.dma_start(out=wr.rearrange("p c n -> p (c n)"), in_=wr_d), writes=["wr"], dma=True)
        P.op("sp", lambda h: h.dma_start(out=rows, in_=rows_d.partition_broadcast(128)), writes=["rows"], dma=True)
        P.op("dve", lambda h: h.tensor_copy(out=identb, in_=cn[:, CN_ID:CN_ID + 128]), reads=["cn"], writes=["identb"])
        P.op("dve", lambda h: h.tensor_copy(out=onesb, in_=cn[:, CN_ONE:CN_ONE + 128]), reads=["cn"], writes=["onesb"])
        P.op("dve", lambda h: h.tensor_copy(out=maskb, in_=cn[:, CN_MASK:CN_MASK + 64]), reads=["cn"], writes=["maskb"])
        P.op("dve", lambda h: h.tensor_tensor(out=hp[:, 12:16], in0=pc[:, PC_LB + 4:PC_LB + 8], in1=pc[:, PC_LB:PC_LB + 4],
                                              op=ALU.subtract), reads=["pc"], writes=["hp_t"])
        P.op("act", lambda h: h.activation(out=hp[:, 12:16], in_=hp[:, 12:16], func=AF.Exp), reads=["hp_t"], writes=["hp_t"])
        P.op("dve", lambda h: h.tensor_scalar_add(out=hp[:, 12:16], in0=hp[:, 12:16], scalar1=1.0), reads=["hp_t"], writes=["hp_t"])
        P.op("dve", lambda h: h.reciprocal(out=hp[:, 0:4], in_=hp[:, 12:16]), reads=["hp_t"], writes=["hp_lb"])
        P.op("dve", lambda h: h.tensor_scalar(out=hp[:, 4:8], in0=hp[:, 0:4], scalar1=-1.0, scalar2=1.0, op0=ALU.mult, op1=ALU.add),
             reads=["hp_lb"], writes=["hp_oml"])
        P.op("act", lambda h: h.activation(out=hp[:, 8:12], in_=hp[:, 4:8], func=AF.Ln), reads=["hp_oml"], writes=["hp_ln"])
        for c in range(8):
            P.op("dve", lambda h, c=c: h.tensor_scalar(out=wrg[:, c, :], in0=wr[:, c, :], scalar1=pc[:, PC_G + 24 + c:PC_G + 25 + c],
                                                       scalar2=None, op0=ALU.mult), reads=["wr", "pc"], writes=["wrg"])

        def wview(w2d, c0, c1):
            return w2d.rearrange("(c p) n -> p c n", p=128)[:, :, c0:c1]

        def gbc(gi):
            return pc[:, PC_G + gi * 8:PC_G + gi * 8 + 8].unsqueeze(2).to_broadcast([128, 8, 128])

        def norm_a(src, src_key, stat, stat_key, xn, xn_key):
            ss = stat[:, 0:1]
            rs = stat[:, 1:2]
            P.op("act", lambda h: h.activation(out=xn, in_=src, func=AF.Square, accum_out=ss),
                 reads=[src_key], writes=[xn_key, stat_key])
            P.op("act", lambda h: h.activation(out=rs, in_=ss, func=AF.Ln, scale=1.0 / D, bias=eps_ap), reads=[stat_key, "eps"], writes=[stat_key])
            P.op("act", lambda h: h.activation(out=rs, in_=rs, func=AF.Exp, scale=-0.5), reads=[stat_key], writes=[stat_key])
            P.op("dve", lambda h: h.tensor_scalar(out=xn, in0=src, scalar1=rs, scalar2=None, op0=ALU.mult),
                 reads=[src_key, stat_key], writes=[xn_key])

        def norm_b(xn, xn_key, gi, dst3, dst_key, ti):
            bk = Bank("b")
            for c in range(8):
                P.op("pe", lambda h, c=c: h.transpose(out=bk.t[:, c * 128:(c + 1) * 128], in_=xn[:, c * 128:(c + 1) * 128], identity=identb),
                     reads=[xn_key, "identb"], writes=[bk.k()])
            P.op("dve", lambda h: h.tensor_tensor(out=dst3[:, :, ti * 128:(ti + 1) * 128],
                                                  in0=bk.t[:, :].rearrange("p (c n) -> p c n", c=8), in1=gbc(gi), op=ALU.mult),
                 reads=[bk.k(), "pc"], writes=[dst_key])

        def norm_tile(src, src_key, gi, dst3, dst_key, stat, stat_key, xn, xn_key, ti):
            norm_a(src, src_key, stat, stat_key, xn, xn_key)
            norm_b(xn, xn_key, gi, dst3, dst_key, ti)

        eps_ap = carve(1)
        P.op("dve", lambda h: h.memset(eps_ap, EPS), writes=["eps"])
        one_ap = carve(1)
        P.op("dve", lambda h: h.memset(one_ap, 1.0), writes=["one"])
        base_top = top[0]

        if stop_after == "S":
            P.barrier()
            P.op("sp", lambda h: h.dma_start(out=dbg_d[:, 0:16], in_=hp), dma=True)
            P.op("sp", lambda h: h.dma_start(out=dbg_d[:, 16:1060], in_=rows), dma=True)
            P.op("sp", lambda h: h.dma_start(out=dbg_d[:, 1060:1220], in_=wrg.rearrange("p c n -> p (c n)")), dma=True)
            nhalves = 0

        lgA = carve(2 * NT * 20, F32, "p (i n) -> p i n", i=2 * NT)
        wrb = carve(80, BF16, "p (c n) -> p c n", c=8)
        P.op("dve", lambda h: h.tensor_copy(out=wrb, in_=wr), reads=["wr"], writes=["wrb"])
        base_top = top[0]

        def carve_at(off, words, dtype=F32, pattern=None, **kw):
            sv = top[0]
            top[0] = off
            ap = carve(words, dtype, pattern, **kw)
            top[0] = sv
            return ap

        cast_jobs = []
        for (src, dst, nm) in ((wgl_d, wgb_d, "g"), (wul_d, wub_d, "u"), (wdl_d, wdb_d, "d")):
            for q in range(2):
                for ch in range(8):
                    cast_jobs.append((src[q], dst[q], ch, "keep:wc.%s%d" % (nm, q)))
        cast_pos = [0]

        def emit_casts(n):
            for _ in range(n):
                if cast_pos[0] >= len(cast_jobs):
                    return
                src, dst, ch, key = cast_jobs[cast_pos[0]]
                cast_pos[0] += 1
                P.op("pool", lambda h, src=src, dst=dst, ch=ch: h.dma_start(out=dst[ch * 256:(ch + 1) * 256, :], in_=src[ch * 256:(ch + 1) * 256, :]),
                     writes=[key + ".%d" % ch], dma=True, bg=True)

        def cast_keys(nm, q):
            return ["keep:wc.%s%d.%d" % (nm, q, ch) for ch in range(8)]

        def do_half(hf):
            top[0] = base_top
            bufA_off = top[0]
            bufA = carve(8192, BF16, "p (c n) -> p c n", c=8)
            bufY_off = top[0]
            bufY = carve(8192, BF16, "p (c n) -> p c n", c=8)
            xres_off = top[0]
            xres = carve(16384, F32, "p (i n) -> p i n", i=16)
            rstd3 = carve(32, F32, "p (i n) -> p i n", i=16)
            RT0 = top[0]
            tok0 = hf * SEQ

            def akey(i):
                return "A.%d" % i

            def ykey(c, j):
                return "Y.%d.%d" % (c, j)

            def xkey(i):
                return "X.%d" % i

            win = carve_at(xres_off, 14336, BF16, "p (c n) -> p c n", c=8)
            vtok = carve_at(xres_off + 14336, 2048, BF16, "p (c n) -> p c n", c=8)
            for s in (0, 1, 2, 5, 4, 3, 6):
                P.op("pool", lambda h, s=s: h.dma_start(out=win[:, :, s * 512:(s + 1) * 512], in_=wview(w_in_d, s * 512, (s + 1) * 512)),
                     writes=["win%d" % s], dma=True)

            top[0] = RT0
            xs = [carve(1024) for _ in range(3)]
            xn3 = [carve(512, BF16) for _ in range(3)]
            for i in range(NT + 1):
                if i < NT:
                    xk = "xs%d" % (i % 3)
                    P.op("sp", lambda h, i=i: h.dma_start(out=xs[i % 3], in_=x_d[tok0 + i * 128:tok0 + (i + 1) * 128, :]),
                         writes=[xk], dma=True)
                    norm_a(xs[i % 3], xk, rstd3[:, i, :], "st.%d" % i, xn3[i % 3], "xn%d" % (i % 3))
                if i >= 1:
                    norm_b(xn3[(i - 1) % 3], "xn%d" % ((i - 1) % 3), 0, bufA, akey(i - 1), i - 1)
            P.barrier()
            if stop_after == "A1":
                for c in range(8):
                    P.op("pool", lambda h, c=c: h.dma_start(out=dbg_d[:, c * 1024:(c + 1) * 1024], in_=bufA[:, c, 0:1024]), dma=True)
                return True

            top[0] = RT0
            wout = carve(4096, BF16, "p (c n) -> p c n", c=8)
            P.op("pool", lambda h: h.dma_start(out=wout, in_=wview(w_out_d, 0, D)), writes=["wout"], dma=True)
            Sst = carve(512, F32, "p (h n) -> p h n", h=4)
            ubuf = carve(4 * 514, F32, "p (c n) -> p c n", c=4)
            P.op("pool", lambda h: h.memset(Sst.rearrange("p h n -> p (h n)"), 0.0), writes=["S0", "S1", "S2", "S3"])
            P.op("pool", lambda h: h.memset(ubuf.rearrange("p c n -> p (c n)"), 0.0), writes=["u0", "u1", "u2", "u3"])
            f_t1 = carve(512)
            f_t2 = carve(512)
            DSb = [carve(64, BF16) for _ in range(8)]
            NSET = 2
            TS = []
            for q in range(NSET):
                TS.append(dict(
                    e=carve(512), l2=carve(512), l1=carve(512), lnk=carve(512), d=carve(512), dec=carve(8),
                    qt=carve(256, BF16), khT=carve(256, BF16),
                    khtok=carve(512, BF16, "p (c n) -> p c n", c=8), scm=carve(256, BF16, "p (c n) -> p c n", c=8)))

            def conv_chunk(j, cch):
                T0 = j * 512
                rdA = [akey(4 * j + t) for t in range(4)]
                th = []
                bks = [None, None, None]

                def mm(s):
                    def f():
                        bks[s] = Bank("f")
                        bk = bks[s]
                        for c in range(8):
                            P.op("pe", lambda h, c=c: h.matmul(
                                bk.t[:, :], lhsT=win[:, c, s * 512 + cch * 128:s * 512 + (cch + 1) * 128],
                                rhs=bufA[:, c, T0:T0 + 512], start=(c == 0), stop=(c == 7)),
                                reads=rdA + ["win%d" % s], writes=[bk.k()])
                    return f
                uk = "u%d" % cch
                cw0 = PC_CONV + cch * 3

                def ew1():
                    if j > 0:
                        P.op("pool", lambda h: h.tensor_copy(out=ubuf[:, cch, 0:2], in_=ubuf[:, cch, 512:514]), reads=[uk], writes=[uk])
                    P.op("act", lambda h: h.activation(out=ubuf[:, cch, 2:514], in_=bks[1].t[:, :], func=AF.Copy), reads=[bks[1].k()], writes=[uk])

                def ew2():
                    P.op("dve", lambda h: h.tensor_tensor(out=ubuf[:, cch, 2:514], in0=bks[2].t[:, :], in1=ubuf[:, cch, 2:514], op=ALU.mult),
                         reads=[bks[2].k(), uk], writes=[uk])
                    P.op("dve", lambda h: h.tensor_scalar(out=f_t1, in0=ubuf[:, cch, 2:514], scalar1=pc[:, cw0 + 2:cw0 + 3],
                                                          scalar2=None, op0=ALU.mult), reads=[uk, "pc"], writes=["f_t1"])
                    P.op("dve", lambda h: h.scalar_tensor_tensor(out=f_t2, in0=ubuf[:, cch, 1:513], scalar=pc[:, cw0 + 1:cw0 + 2],
                                                                 in1=f_t1, op0=ALU.mult, op1=ALU.add), reads=[uk, "pc", "f_t1"], writes=["f_t2"])
                    P.op("dve", lambda h: h.scalar_tensor_tensor(out=f_t1, in0=ubuf[:, cch, 0:512], scalar=pc[:, cw0:cw0 + 1],
                                                                 in1=f_t2, op0=ALU.mult, op1=ALU.add), reads=[uk, "pc", "f_t2"], writes=["f_t1"])

                def ew3():
                    P.op("dve", lambda h: h.tensor_tensor(out=bufY[:, cch, T0:T0 + 512], in0=bks[0].t[:, :], in1=f_t1, op=ALU.mult),
                         reads=[bks[0].k(), "f_t1"], writes=[ykey(cch, j)])
                def seq(*fs):
                    def f():
                        for g_ in fs:
                            g_()
                    return f
                return [seq(mm(1), ew1), seq(mm(2), ew2), seq(mm(0), ew3)]

            def v_group(j, c8):
                T0 = j * 512
                rdA = [akey(4 * j + t) for t in range(4)]

                def f():
                    bk = Bank("f")
                    for c in range(8):
                        P.op("pe", lambda h, c=c: h.matmul(
                            bk.t[0:64, :], lhsT=bufA[:, c, T0 + c8 * 64:T0 + (c8 + 1) * 64], rhs=win[:, c, 5 * 512:6 * 512],
                            start=(c == 0), stop=(c == 7)), reads=rdA + ["win5"], writes=[bk.k()])
                    P.op("act", lambda h: h.activation(out=vtok[0:64, c8, :], in_=bk.t[0:64, :], func=AF.Copy),
                         reads=[bk.k()], writes=["vtok%d" % c8])
                return f

            def head_unit(j, hd, q):
                T0 = j * 512
                rdA = [akey(4 * j + t) for t in range(4)]
                t = TS[q]
                K = lambda n: "%s.%d" % (n, q)
                st_ = {}
                lbh = hp[:, hd:hd + 1]
                lnomlh = hp[:, 8 + hd:9 + hd]
                b3 = t["e"].rearrange("p (c n) -> p c n", c=8)
                sk = "S%d" % hd
                E = []

                def proj(name, s):
                    def f():
                        bk = Bank("f")
                        st_[name] = bk
                        for c in range(8):
                            P.op("pe", lambda h, c=c: h.matmul(
                                bk.t[:, :], lhsT=win[:, c, s * 512 + hd * 128:s * 512 + (hd + 1) * 128],
                                rhs=bufA[:, c, T0:T0 + 512], start=(c == 0), stop=(c == 7)),
                                reads=rdA + ["win%d" % s], writes=[bk.k()])
                    return f
                pz_ = proj("z", 4)

                def e1():
                    bz = st_["z"]
                    P.op("act", lambda h: h.activation(out=t["e"], in_=bz.t[:, :], func=AF.Exp, scale=-1.0), reads=[bz.k()], writes=[K("e")])
                    P.op("act", lambda h: h.activation(out=t["l2"], in_=t["e"], func=AF.Ln, bias=one_ap), reads=[K("e"), "one"], writes=[K("l2")])
                    P.op("act", lambda h: h.activation(out=t["l1"], in_=t["e"], func=AF.Ln, scale=lbh, bias=one_ap),
                         reads=[K("e"), "one", "hp_lb"], writes=[K("l1")])

                def e2():
                    bz = st_["z"]
                    P.op("pool", lambda h: h.tensor_tensor(out=t["l1"], in0=t["l1"], in1=t["l2"], op=ALU.subtract), reads=[K("l1"), K("l2")], writes=[K("l1")])
                    P.op("dve", lambda h: h.scalar_tensor_tensor(out=t["lnk"], in0=bz.t[:, :], scalar=-1.0, in1=t["l2"], op0=ALU.mult, op1=ALU.subtract),
                         reads=[bz.k(), K("l2")], writes=[K("lnk")])
                pq_ = proj("q", 3)

                def e3():
                    P.op("dve", lambda h: h.tensor_tensor_scan(out=t["e"], data0=rmask, data1=t["l1"], initial=0.0, op0=ALU.mult, op1=ALU.add),
                         reads=["cn", K("l1"), K("e")], writes=[K("e")])
                    P.op("pool", lambda h: h.tensor_tensor(out=t["d"].rearrange("p (c n) -> p c n", c=8), in0=b3,
                                                           in1=b3[:, :, 63:64].to_broadcast([128, 8, 64]), op=ALU.subtract),
                         reads=[K("e")], writes=[K("d")])

                def e4():
                    bq = st_["q"]
                    P.op("act", lambda h: h.activation(out=t["l2"], in_=t["d"], func=AF.Exp), reads=[K("d"), K("l2")], writes=[K("l2")])
                    P.op("dve", lambda h: h.tensor_tensor(out=t["qt"], in0=bq.t[:, :], in1=t["l2"], op=ALU.mult), reads=[bq.k(), K("l2")], writes=[K("qt")])
                pg_ = proj("g", 6)

                def e5():
                    P.op("pool", lambda h: h.tensor_tensor(out=t["lnk"], in0=t["lnk"], in1=t["d"], op=ALU.subtract), reads=[K("lnk"), K("d")], writes=[K("lnk")])
                    P.op("act", lambda h: h.activation(out=t["khT"], in_=t["lnk"], func=AF.Exp, bias=lnomlh),
                         reads=[K("lnk"), "hp_ln"], writes=[K("khT")])
                    P.op("act", lambda h: h.activation(out=t["dec"], in_=b3[:, :, 63], func=AF.Exp), reads=[K("e")], writes=[K("dec")])

                def e6():
                    bg = st_["g"]
                    P.op("act", lambda h: h.activation(out=t["l1"], in_=bg.t[:, :], func=AF.Exp, scale=-1.0), reads=[bg.k(), K("l1")], writes=[K("l1")])
                    P.op("act", lambda h: h.activation(out=t["l1"], in_=t["l1"], func=AF.Ln, bias=one_ap), reads=[K("l1"), "one"], writes=[K("l1")])
                    P.op("act", lambda h: h.activation(out=t["l1"], in_=t["l1"], func=AF.Exp, scale=-1.0), reads=[K("l1")], writes=[K("l1")])
                    P.op("dve", lambda h: h.tensor_tensor(out=t["l1"], in0=bg.t[:, :], in1=t["l1"], op=ALU.mult), reads=[bg.k(), K("l1")], writes=[K("l1")])

                def grp(*fs):
                    def f():
                        for g_ in fs:
                            g_()
                    return f
                E.append(grp(pz_, e1, e2))
                E.append(grp(pq_, e3, e4))
                E.append(grp(pg_, e5, e6))

                C = []

                def c0():
                    bkk = Bank("b")
                    for c8 in range(8):
                        P.op("pe", lambda h, c8=c8: h.transpose(out=bkk.t[0:64, c8 * 128:(c8 + 1) * 128], in_=t["khT"][:, c8 * 64:(c8 + 1) * 64],
                                                               identity=identb), reads=[K("khT"), "identb"], writes=[bkk.k()])
                    P.op("act", lambda h: h.activation(out=t["khtok"][0:64, :, :].rearrange("p c n -> p (c n)"), in_=bkk.t[0:64, :], func=AF.Copy),
                         reads=[bkk.k()], writes=[K("khtok")])
                    st_["o"] = Bank("f", hold=True)
                    st_["sc"] = Bank("f", hold=True)
                C.append(c0)

                def c1():
                    bsc = st_["sc"]
                    st_["ds0"] = Bank("f", hold=True)
                    st_["ds1"] = Bank("f", hold=True)
                    for c8 in range(8):
                        cs = slice(c8 * 64, (c8 + 1) * 64)
                        P.op("pe", lambda h, cs=cs: h.matmul(bsc.t[0:64, cs], lhsT=t["khT"][:, cs], rhs=t["qt"][:, cs], start=True, stop=True),
                             reads=[K("khT"), K("qt")], writes=[bsc.k(c8)])
                    for c8 in range(8):
                        bds = st_["ds%d" % (c8 // 4)]
                        ds_cols = slice((c8 % 4) * 128, (c8 % 4 + 1) * 128)
                        P.op("pe", lambda h, c8=c8, bds=bds, ds_cols=ds_cols: h.matmul(bds.t[:, ds_cols], lhsT=t["khtok"][0:64, c8, :],
                                                                                        rhs=vtok[0:64, c8, hd * 128:(hd + 1) * 128], start=True, stop=True),
                             reads=[K("khtok"), "vtok%d" % c8], writes=[bds.k(c8 % 4)])
                C.append(c1)

                def c2():
                    bsc = st_["sc"]
                    for c8 in range(8):
                        cs = slice(c8 * 64, (c8 + 1) * 64)
                        P.op("dve", lambda h, c8=c8, cs=cs: h.tensor_tensor(out=t["scm"][0:64, c8, :], in0=bsc.t[0:64, cs], in1=maskb[0:64, :], op=ALU.mult),
                             reads=[bsc.k(c) for c in range(8)] + ["maskb"], writes=[K("scm%d" % c8)])
                    for c8 in range(8):
                        bds = st_["ds%d" % (c8 // 4)]
                        ds_cols = slice((c8 % 4) * 128, (c8 % 4 + 1) * 128)
                        P.op("dve", lambda h, c8=c8: h.tensor_scalar(out=DSb[c8], in0=Sst[:, hd, :], scalar1=t["dec"][:, c8:c8 + 1], scalar2=None, op0=ALU.mult),
                             reads=[sk, K("dec")], writes=["DSb%d" % c8])
                        P.op("dve", lambda h, c8=c8, bds=bds, ds_cols=ds_cols: h.scalar_tensor_tensor(
                            out=Sst[:, hd, :], in0=Sst[:, hd, :], scalar=t["dec"][:, c8:c8 + 1], in1=bds.t[:, ds_cols], op0=ALU.mult, op1=ALU.add),
                            reads=[sk, K("dec")] + [bds.k(c) for c in range(4)], writes=[sk])
                    st_["sc"].done()
                    st_["ds0"].done()
                    st_["ds1"].done()
                C.append(c2)

                def c3():
                    bo = st_["o"]
                    for c8 in range(8):
                        cs = slice(c8 * 64, (c8 + 1) * 64)
                        P.op("pe", lambda h, c8=c8, cs=cs: h.matmul(bo.t[:, cs], lhsT=DSb[c8], rhs=t["qt"][:, cs], start=True, stop=False),
                             reads=["DSb%d" % c8, K("qt")], writes=[bo.k(c8)])
                        P.op("pe", lambda h, c8=c8, cs=cs: h.matmul(bo.t[:, cs], lhsT=vtok[0:64, c8, hd * 128:(hd + 1) * 128], rhs=t["scm"][0:64, c8, :],
                                                                    start=False, stop=True),
                             reads=["vtok%d" % c8, K("scm%d" % c8)], writes=[bo.k(c8)])
                C.append(c3)

                def c9():
                    bo = st_["o"]
                    okeys = [bo.k(c8) for c8 in range(8)]
                    P.op("act", lambda h: h.activation(out=t["khT"], in_=bo.t[:, :], func=AF.Square), reads=okeys + [K("khT")], writes=[K("khT")])
                    bss = Bank("f")
                    P.op("pe", lambda h: h.matmul(bss.t[:, :], lhsT=onesb, rhs=t["khT"], start=True, stop=True), reads=["onesb", K("khT")], writes=[bss.k()])
                    P.op("act", lambda h: h.activation(out=t["l2"], in_=bss.t[:, :], func=AF.Ln, scale=1.0 / 128, bias=eps_ap),
                         reads=[bss.k(), "eps", K("l2")], writes=[K("l2")])
                    P.op("act", lambda h: h.activation(out=t["l2"], in_=t["l2"], func=AF.Exp, scale=-0.5), reads=[K("l2")], writes=[K("l2")])
                    P.op("dve", lambda h: h.tensor_tensor(out=t["d"], in0=bo.t[:, :], in1=t["l2"], op=ALU.mult), reads=okeys + [K("l2"), K("d")], writes=[K("d")])
                    P.op("dve", lambda h: h.scalar_tensor_tensor(out=bufY[:, 4 + hd, T0:T0 + 512], in0=t["d"], scalar=pc[:, PC_HN + hd:PC_HN + hd + 1],
                                                                 in1=t["l1"], op0=ALU.mult, op1=ALU.mult),
                         reads=[K("d"), K("l1"), "pc"], writes=[ykey(4 + hd, j)])
                    bo.done()
                C.append(c9)
                return E, C

            def interleave(primary, fillers):
                n, m = len(primary), len(fillers)
                fi = 0
                for i, f in enumerate(primary):
                    f()
                    want = ((i + 1) * m) // max(n, 1)
                    while fi < want:
                        fillers[fi]()
                        fi += 1
                while fi < m:
                    fillers[fi]()
                    fi += 1

            for cch in range(4):
                for f in conv_chunk(0, cch):
                    f()
            for c8 in range(8):
                v_group(0, c8)()
            units = [(j, hd) for j in range(NJ) for hd in range(4)]
            E0, Cprev = head_unit(0, 0, 0)
            for f in E0:
                f()
            for u in range(len(units)):
                j, hd = units[u]
                fill = []
                if u + 1 < len(units):
                    jn, hn = units[u + 1]
                    En, Cn = head_unit(jn, hn, (u + 1) % NSET)
                    if jn == j:
                        fill += En
                else:
                    En, Cn = [], []
                if j + 1 < NJ:
                    fill += conv_chunk(j + 1, hd)
                for f in Cprev[0:3]:
                    f()
                for f in fill:
                    f()
                for f in Cprev[3:]:
                    f()
                if u + 1 < len(units) and units[u + 1][0] != j:
                    for c8 in range(8):
                        v_group(j + 1, c8)()
                    for f in En:
                        f()
                emit_casts(2)
                Cprev = Cn
            P.barrier()
            if stop_after == "A2":
                for c in (4, 5, 6, 7, 0, 1, 2, 3):
                    for jj in range(2):
                        P.op("pool", lambda h, c=c, jj=jj: h.dma_start(out=dbg_d[:, c * 1024 + jj * 512:c * 1024 + (jj + 1) * 512],
                                                                      in_=bufY[:, c, jj * 512:(jj + 1) * 512]), dma=True)
                return True

            top[0] = RT0
            wout = carve(4096, BF16, "p (c n) -> p c n", c=8)
            xsb = [carve(1024) for _ in range(3)]
            xnb = [carve(512, BF16) for _ in range(3)]
            wq_off = top[0]
            wq = carve(4096, BF16, "p (c n) -> p c n", c=8)
            wo = carve(4096, BF16, "p (c n) -> p c n", c=8)
            P.op("pool", lambda h: h.dma_start(out=wq, in_=wview(w_q_d, 0, D)), writes=["wq"], dma=True)
            P.op("pool", lambda h: h.dma_start(out=wo, in_=wview(w_o_d, 0, D)), writes=["wo"], dma=True)
            emit_casts(4)

            def b1_stage1(i):
                xk = "xsb%d" % (i % 3)
                P.op("sp", lambda h: h.dma_start(out=xsb[i % 3], in_=x_d[tok0 + i * 128:tok0 + (i + 1) * 128, :]), writes=[xk], dma=True)
                for n2 in range(2):
                    bk = Bank("f")
                    for c in range(8):
                        P.op("pe", lambda h, c=c, n2=n2, bk=bk: h.matmul(bk.t[:, :], lhsT=bufY[:, c, i * 128:(i + 1) * 128],
                                                                         rhs=wout[:, c, n2 * 512:(n2 + 1) * 512], start=(c == 0), stop=(c == 7)),
                             reads=["wout"], writes=[bk.k()])
                    P.op("dve", lambda h, n2=n2, bk=bk: h.tensor_tensor(out=xres[:, i, n2 * 512:(n2 + 1) * 512], in0=bk.t[:, :],
                                                                        in1=xsb[i % 3][:, n2 * 512:(n2 + 1) * 512], op=ALU.add),
                         reads=[bk.k(), xk], writes=[xkey(i)])

            b1_stage1(0)
            b1_stage1(1)
            for i in range(NT + 1):
                if i + 2 < NT:
                    b1_stage1(i + 2)
                if i < NT:
                    norm_a(xres[:, i, :], xkey(i), rstd3[:, i, :], "st.%d" % i, xnb[i % 3], "xnb%d" % (i % 3))
                if i >= 1:
                    norm_b(xnb[(i - 1) % 3], "xnb%d" % ((i - 1) % 3), 1, bufA, akey(i - 1), i - 1)
            P.barrier()
            if stop_after == "B1":
                for i in range(8):
                    P.op("pool", lambda h, i=i: h.dma_start(out=dbg_d[:, i * 1024:(i + 1) * 1024], in_=xres[:, i, :]), dma=True)
                return True

            KT = carve_at(RT0, 1024, BF16, "p (c n) -> p c n", c=8)
            Vt = carve_at(RT0 + 1024, 1024, BF16, "p (c n) -> p c n", c=2)
            mstat = carve_at(RT0 + 2048, 4)
            wq = carve_at(wq_off, 4096, BF16, "p (c n) -> p c n", c=8)
            wo = carve_at(wq_off + 4096, 4096, BF16, "p (c n) -> p c n", c=8)
            top[0] = bufY_off
            wkvb = [carve(2048, BF16, "p (c n) -> p c n", c=8) for _ in range(2)]
            mT = carve(1024, BF16, "p (c n) -> p c n", c=8)
            mst = [carve(1024) for _ in range(2)]
            xn2 = [carve(512, BF16) for _ in range(2)]
            assert top[0] <= bufY_off + 8192
            for t in range(2):
                P.op("sp", lambda h, t=t: h.dma_start(out=mst[t], in_=mem_d[hf * 256 + t * 128:hf * 256 + (t + 1) * 128, :]),
                     writes=["mst%d" % t], dma=True)
            for blk in range(2):
                P.op("pool", lambda h, blk=blk: h.dma_start(out=wkvb[blk], in_=wview(w_kv_d, blk * 512, (blk + 1) * 512)), writes=["wkv%d" % blk], dma=True)
            for t in range(2):
                norm_tile(mst[t], "mst%d" % t, 2, mT, "mT", mstat[:, 2 * t:2 * t + 2], "mstat%d" % t, xn2[t], "xn%d" % t, t)
            for blk in range(4):
                wb = wkvb[blk % 2]
                wk = "wkv%d" % (blk % 2)
                if blk >= 2:
                    P.op("pool", lambda h, blk=blk, wb=wb: h.dma_start(out=wb, in_=wview(w_kv_d, blk * 512, (blk + 1) * 512)), writes=[wk], dma=True)
                if blk < 2:
                    for e4 in range(4):
                        ec = blk * 4 + e4
                        bk = Bank("f")
                        for c in range(8):
                            P.op("pe", lambda h, c=c, e4=e4, wb=wb, bk=bk: h.matmul(bk.t[:, 0:256], lhsT=wb[:, c, e4 * 128:(e4 + 1) * 128], rhs=mT[:, c, :],
                                                                                   start=(c == 0), stop=(c == 7)), reads=[wk, "mT"], writes=[bk.k()])
                        P.op("act", lambda h, ec=ec, bk=bk: h.activation(out=KT[:, ec, :], in_=bk.t[:, 0:256], func=AF.Copy), reads=[bk.k()], writes=["KT"])
                else:
                    n2 = blk - 2
                    for mc in range(2):
                        bk = Bank("f")
                        for c in range(8):
                            P.op("pe", lambda h, c=c, mc=mc, wb=wb, bk=bk: h.matmul(bk.t[:, :], lhsT=mT[:, c, mc * 128:(mc + 1) * 128], rhs=wb[:, c, :],
                                                                                   start=(c == 0), stop=(c == 7)), reads=[wk, "mT"], writes=[bk.k()])
                        P.op("act", lambda h, mc=mc, n2=n2, bk=bk: h.activation(out=Vt[:, mc, n2 * 512:(n2 + 1) * 512], in_=bk.t[:, :], func=AF.Copy),
                             reads=[bk.k()], writes=["Vt"])
            P.barrier()
            top[0] = bufY_off
            QT = carve(2048, BF16, "p (c n) -> p c n", c=8)
            OT = carve(2048, BF16, "p (c n) -> p c n", c=8)
            ET = [carve(512, BF16, "p (c n) -> p c n", c=2) for _ in range(2)]
            rden = [carve(512) for _ in range(2)]

            def q_proj(j):
                T0 = j * 512
                rdA = [akey(4 * j + t) for t in range(4)]
                for ec in range(8):
                    bk = Bank("f")
                    for c in range(8):
                        P.op("pe", lambda h, c=c, ec=ec, bk=bk: h.matmul(bk.t[:, :], lhsT=wq[:, c, ec * 128:(ec + 1) * 128], rhs=bufA[:, c, T0:T0 + 512],
                                                                        start=(c == 0), stop=(c == 7)), reads=rdA + ["wq"], writes=[bk.k()])
                    if ec % 2 == 0:
                        P.op("act", lambda h, ec=ec, bk=bk: h.activation(out=QT[:, ec, :], in_=bk.t[:, :], func=AF.Copy, scale=1.0 / 16.0),
                             reads=[bk.k()], writes=["QT%d" % ec])
                    else:
                        P.op("dve", lambda h, ec=ec, bk=bk: h.tensor_scalar(out=QT[:, ec, :], in0=bk.t[:, :], scalar1=1.0 / 16.0, scalar2=None, op0=ALU.mult),
                             reads=[bk.k()], writes=["QT%d" % ec])

            def s_exp(hd):
                et = ET[hd % 2]
                ek = "ET%d" % (hd % 2)
                for mc in range(2):
                    bk = Bank("f")
                    for k2 in range(2):
                        P.op("pe", lambda h, mc=mc, k2=k2, bk=bk: h.matmul(bk.t[:, :], lhsT=KT[:, 2 * hd + k2, mc * 128:(mc + 1) * 128],
                                                                          rhs=QT[:, 2 * hd + k2, :], start=(k2 == 0), stop=(k2 == 1)),
                             reads=["KT", "QT%d" % (2 * hd + k2)], writes=[bk.k()])
                    P.op("act", lambda h, mc=mc, bk=bk: h.activation(out=et[:, mc, :], in_=bk.t[:, :], func=AF.Exp), reads=[bk.k()], writes=[ek + ".%d" % mc])

            def pv(hd):
                et = ET[hd % 2]
                ek = "ET%d" % (hd % 2)
                bden = Bank("f")
                for mc in range(2):
                    P.op("pe", lambda h, mc=mc: h.matmul(bden.t[:, :], lhsT=onesb, rhs=et[:, mc, :], start=(mc == 0), stop=(mc == 1)),
                         reads=["onesb", ek + ".%d" % mc], writes=[bden.k()])
                rd = rden[hd % 2]
                rk = "rden%d" % (hd % 2)
                P.op("dve", lambda h: h.reciprocal(out=rd, in_=bden.t[:, :]), reads=[bden.k()], writes=[rk])
                for k2 in range(2):
                    bk = Bank("f")
                    for mc in range(2):
                        P.op("pe", lambda h, mc=mc, k2=k2, bk=bk: h.matmul(
                            bk.t[:, :], lhsT=Vt[:, mc, (2 * hd + k2) * 128:(2 * hd + k2 + 1) * 128], rhs=et[:, mc, :], start=(mc == 0), stop=(mc == 1)),
                            reads=["Vt", ek + ".%d" % mc], writes=[bk.k()])
                    P.op("dve", lambda h, k2=k2, bk=bk: h.tensor_tensor(out=OT[:, 2 * hd + k2, :], in0=bk.t[:, :], in1=rd, op=ALU.mult),
                         reads=[bk.k(), rk], writes=["OT%d" % (2 * hd + k2)])

            def w_o(j):
                for tt in range(4):
                    i = 4 * j + tt
                    for n2 in range(2):
                        bk = Bank("f")
                        for ec in range(8):
                            P.op("pe", lambda h, ec=ec, tt=tt, n2=n2, bk=bk: h.matmul(bk.t[:, :], lhsT=OT[:, ec, tt * 128:(tt + 1) * 128],
                                                                                     rhs=wo[:, ec, n2 * 512:(n2 + 1) * 512], start=(ec == 0), stop=(ec == 7)),
                                 reads=["OT%d" % ec, "wo"], writes=[bk.k()])
                        P.op("dve", lambda h, i=i, n2=n2, bk=bk: h.tensor_tensor(out=xres[:, i, n2 * 512:(n2 + 1) * 512], in0=bk.t[:, :],
                                                                                 in1=xres[:, i, n2 * 512:(n2 + 1) * 512], op=ALU.add),
                             reads=[bk.k(), xkey(i)], writes=[xkey(i)])

            q_proj(0)
            for j in range(NJ):
                s_exp(0)
                for hd in range(4):
                    if hd + 1 < 4:
                        s_exp(hd + 1)
                    pv(hd)
                if j + 1 < NJ:
                    q_proj(j + 1)
                w_o(j)
                emit_casts(3)
            emit_casts(100)
            P.barrier()
            if stop_after == "B2":
                for i in range(8):
                    P.op("pool", lambda h, i=i: h.dma_start(out=dbg_d[:, i * 1024:(i + 1) * 1024], in_=xres[:, i, :]), dma=True)
                return True

            top[0] = RT0
            xnc = [carve(512, BF16) for _ in range(3)]

            def router(i):
                bk = Bank("f")
                for c in range(8):
                    P.op("pe", lambda h, c=c: h.matmul(bk.t[:, 0:20], lhsT=bufA[:, c, i * 128:(i + 1) * 128], rhs=wrb[:, c, :],
                                                       start=(c == 0), stop=(c == 7)), reads=[akey(i), "wrb"], writes=[bk.k()])
                P.op("dve", lambda h: h.tensor_tensor(out=lgA[:, hf * NT + i, :], in0=bk.t[:, 0:20], in1=rows[:, 1024:1044], op=ALU.add),
                     reads=[bk.k(), "rows"], writes=["keep:lg.%d.%d" % (hf, i)])

            for i in range(NT + 1):
                if i < NT:
                    g_ = hf * NT + i
                    P.op("sp", lambda h, i=i, g_=g_: h.dma_start(out=x2_d[g_ * 128:(g_ + 1) * 128, :], in_=xres[:, i, :]), reads=[xkey(i)],
                         writes=["keep:X2.%d" % g_], dma=True)
                    norm_a(xres[:, i, :], xkey(i), rstd3[:, i, :], "st.%d" % i, xnc[i % 3], "xnc%d" % (i % 3))
                    P.op("sp", lambda h, i=i, g_=g_: h.dma_start(out=h3_d[g_ * 128:(g_ + 1) * 128, :], in_=xnc[i % 3]), reads=["xnc%d" % (i % 3)],
                         writes=["keep:H3.%d" % g_], dma=True)
                if i >= 1:
                    norm_b(xnc[(i - 1) % 3], "xnc%d" % ((i - 1) % 3), 3, bufA, akey(i - 1), i - 1)
                    router(i - 1)
            emit_casts(100)
            P.barrier()
            return False

        def moe_global():
            NTT = 2 * NT
            top[0] = base_top
            wg = [carve(2048, BF16, "p (c n) -> p c n", c=8) for _ in range(2)]
            wu = [carve(2048, BF16, "p (c n) -> p c n", c=8) for _ in range(2)]
            wd = [carve(2048, BF16, "p (c n) -> p c n", c=4) for _ in range(2)]
            xn2 = [carve(512, BF16) for _ in range(2)]
            r_t = [carve(512) for _ in range(4)]
            r_s = [carve(64) for _ in range(6)]
            hid = [carve(1024, BF16, "p (c n) -> p c n", c=4) for _ in range(2)]
            sgb = [carve(256, BF16) for _ in range(2)]
            rstd3 = carve(2 * NTT, F32, "p (i n) -> p i n", i=NTT)
            bufA_off = top[0]
            top[0] += 8192
            lg = lgA
            lgkeys = ["keep:lg.%d.%d" % (hf, i) for hf in range(2) for i in range(NT)]
            P.op("dve", lambda h: h.tensor_copy(out=lg[:, 0, 0:1], in_=lg[:, 0, 0:1]), reads=lgkeys, writes=["lg"])
            LG = lg[:, :, 0:4]
            LE = lg[:, :, 4:20].rearrange("p i (j k) -> p i j k", j=4)
            gmax, gsum, m1, m2, w1, w2 = [r[:, 0:NTT] for r in r_s]
            gsh = r_t[0][:, 0:4 * NTT].rearrange("p (i j) -> p i j", i=NTT)
            gm = r_t[1][:, 0:4 * NTT].rearrange("p (i j) -> p i j", i=NTT)
            tmp4 = r_t[2].rearrange("p (i j k) -> p i j k", i=NTT, j=4)
            esel = r_t[3][:, 0:128].rearrange("p (i k) -> p i k", i=NTT)
            mk1 = r_t[3][:, 128:256].rearrange("p (i k) -> p i k", i=NTT)
            e2 = r_t[3][:, 256:384].rearrange("p (i k) -> p i k", i=NTT)
            mk2 = r_t[3][:, 384:512].rearrange("p (i k) -> p i k", i=NTT)
            cig = r_t[0][:, 128:256].rearrange("p (i k) -> p i k", i=NTT)
            tq = r_t[0][:, 256:384].rearrange("p (i k) -> p i k", i=NTT)

            def bc3(a):
                return a.unsqueeze(2).to_broadcast([128, NTT, 4])

            R = lambda fn, eng="dve": P.op(eng, fn, reads=["lg", "rt"], writes=["rt"])
            R(lambda h: h.tensor_reduce(out=gmax, in_=LG, axis=AX.X, op=ALU.max))
            R(lambda h: h.tensor_tensor(out=gsh, in0=LG, in1=bc3(gmax), op=ALU.subtract))
            R(lambda h: h.tensor_single_scalar(out=gm, in_=gsh, scalar=0.0, op=ALU.is_ge))
            R(lambda h: h.activation(out=gsh, in_=gsh, func=AF.Exp), "act")
            R(lambda h: h.tensor_reduce(out=gsum, in_=gsh, axis=AX.X, op=ALU.add))
            R(lambda h: h.reciprocal(out=gsum, in_=gsum))
            R(lambda h: h.tensor_tensor(out=tmp4, in0=LE, in1=gm.unsqueeze(3).to_broadcast([128, NTT, 4, 4]), op=ALU.mult))
            R(lambda h: h.tensor_reduce(out=esel, in_=tmp4.rearrange("p i j k -> p i k j"), axis=AX.X, op=ALU.add))
            R(lambda h: h.tensor_reduce(out=m1, in_=esel, axis=AX.X, op=ALU.max))
            R(lambda h: h.tensor_tensor(out=mk1, in0=esel, in1=bc3(m1), op=ALU.is_ge))
            R(lambda h: h.scalar_tensor_tensor(out=e2, in0=mk1, scalar=-1e30, in1=esel, op0=ALU.mult, op1=ALU.add))
            R(lambda h: h.tensor_reduce(out=m2, in_=e2, axis=AX.X, op=ALU.max))
            R(lambda h: h.tensor_tensor(out=mk2, in0=e2, in1=bc3(m2), op=ALU.is_ge))
            R(lambda h: h.tensor_tensor(out=w2, in0=m2, in1=m1, op=ALU.subtract))
            R(lambda h: h.activation(out=w2, in_=w2, func=AF.Exp), "act")
            R(lambda h: h.tensor_scalar_add(out=w1, in0=w2, scalar1=1.0))
            R(lambda h: h.reciprocal(out=w1, in_=w1))
            R(lambda h: h.tensor_tensor(out=w2, in0=w2, in1=w1, op=ALU.mult))
            R(lambda h: h.tensor_tensor(out=w1, in0=w1, in1=gsum, op=ALU.mult))
            R(lambda h: h.tensor_tensor(out=w2, in0=w2, in1=gsum, op=ALU.mult))
            R(lambda h: h.tensor_tensor(out=cig, in0=mk1, in1=bc3(w1), op=ALU.mult))
            R(lambda h: h.tensor_tensor(out=tq, in0=mk2, in1=bc3(w2), op=ALU.mult))
            R(lambda h: h.tensor_tensor(out=cig, in0=cig, in1=tq, op=ALU.add))
            NU = 31
            NSLOT = NU * 512
            oh = carve(2 * NTT * 16, F32, "p (a i e) -> p a i e", a=2, i=NTT)
            A_bf = carve(NTT * 8, BF16)
            pit = carve(NTT * 16, F32, "p (i e) -> p i e", i=NTT)
            tot = carve(NTT * 16, F32, "p (i e) -> p i e", i=NTT)
            off = carve(NTT * 16, F32, "p (i e) -> p i e", i=NTT)
            sm = carve(80)
            ne, un, cu, cui, base = [sm[:, k * 16:(k + 1) * 16] for k in range(5)]
            slf = carve(2 * NTT, F32, "p (a i) -> p a i", a=2)
            sli = carve(2 * NTT).bitcast(I32).rearrange("p (a i) -> p a i", a=2)
            sadr = carve(2 * NTT).bitcast(I32).rearrange("p (a i) -> p a i", a=2)
            sadr_i = carve(4 * NTT).bitcast(I32)
            sadr_f = carve(4 * NTT)
            cmpb = carve(NU * 16, F32, "p (s e) -> p s e", s=NU)
            euf = carve(32)
            wix = carve(32).bitcast(I32)
            tokid = carve(NTT).bitcast(I32)
            uidx = carve(NU * 4).bitcast(I32)
            zer = carve(NU * 4).bitcast(I32)
            Ub = carve(64, BF16)
            P.op("dve", lambda h: h.tensor_copy(out=Ub, in_=cn[:, CN_U:CN_U + 128]), reads=["cn"], writes=["Ub"])
            P.op("pool", lambda h: h.iota(tokid, pattern=[[128, NTT]], base=0, channel_multiplier=1), writes=["tokid"])
            P.op("dve", lambda h: h.memset(zer, 0), writes=["zer"])
            P.op("sp", lambda h: h.dma_start(out=tokslot_d.rearrange("(p n) o -> p (n o)", p=128), in_=zer), reads=["zer"], writes=["ts0"], dma=True)
            S = lambda fn, eng="dve": P.op(eng, fn, reads=["rt", "cn", "Ub"], writes=["rt"])
            gm4 = gm.unsqueeze(3).to_broadcast([128, NTT, 4, 4])
            S(lambda h: h.tensor_tensor(out=oh[:, 0].rearrange("p i (j k) -> p i j k", j=4), in0=gm4,
                                        in1=mk1.unsqueeze(2).to_broadcast([128, NTT, 4, 4]), op=ALU.mult))
            S(lambda h: h.tensor_tensor(out=oh[:, 1].rearrange("p i (j k) -> p i j k", j=4), in0=gm4,
                                        in1=mk2.unsqueeze(2).to_broadcast([128, NTT, 4, 4]), op=ALU.mult))
            S(lambda h: h.tensor_tensor(out=A_bf, in0=oh[:, 0].rearrange("p i e -> p (i e)"), in1=oh[:, 1].rearrange("p i e -> p (i e)"), op=ALU.add))
            bkp, bkt = Bank("f"), Bank("f")
            P.op("pe", lambda h: h.matmul(bkp.t[:, 0:NTT * 16], lhsT=Ub, rhs=A_bf, start=True, stop=True), reads=["rt", "Ub"], writes=[bkp.k()])
            P.op("pe", lambda h: h.matmul(bkt.t[:, 0:NTT * 16], lhsT=onesb, rhs=A_bf, start=True, stop=True), reads=["rt", "onesb"], writes=[bkt.k()])
            P.op("dve", lambda h: h.tensor_copy(out=pit.rearrange("p i e -> p (i e)"), in_=bkp.t[:, 0:NTT * 16]), reads=[bkp.k(), "rt"], writes=["rt"])
            P.op("act", lambda h: h.activation(out=tot.rearrange("p i e -> p (i e)"), in_=bkt.t[:, 0:NTT * 16], func=AF.Copy), reads=[bkt.k(), "rt"], writes=["rt"])
            S(lambda h: h.memset(off[:, 0, :], 0.0))
            for i in range(1, NTT):
                S(lambda h, i=i: h.tensor_tensor(out=off[:, i, :], in0=off[:, i - 1, :], in1=tot[:, i - 1, :], op=ALU.add))
            S(lambda h: h.tensor_tensor(out=ne, in0=off[:, NTT - 1, :], in1=tot[:, NTT - 1, :], op=ALU.add))
            S(lambda h: h.tensor_single_scalar(out=un, in_=ne, scalar=0.0, op=ALU.is_gt))
            for thr in (512.0, 1024.0, 1536.0, 2048.0, 2560.0, 3072.0, 3584.0):
                S(lambda h, thr=thr: h.scalar_tensor_tensor(out=un, in0=ne, scalar=thr, in1=un, op0=ALU.is_gt, op1=ALU.add))
            S(lambda h: h.memset(cu[:, 0:1], 0.0))
            for e in range(1, 16):
                S(lambda h, e=e: h.tensor_tensor(out=cu[:, e:e + 1], in0=cu[:, e - 1:e], in1=un[:, e - 1:e], op=ALU.add))
            S(lambda h: h.tensor_tensor(out=cui, in0=cu, in1=un, op=ALU.add))
            S(lambda h: h.tensor_single_scalar(out=base, in_=cu, scalar=512.0, op=ALU.mult))
            S(lambda h: h.tensor_tensor(out=pit, in0=pit, in1=off, op=ALU.add))
            S(lambda h: h.tensor_tensor(out=pit, in0=pit, in1=base.unsqueeze(1).to_broadcast([128, NTT, 16]), op=ALU.add))
            for a in range(2):
                S(lambda h, a=a: h.tensor_tensor(out=oh[:, a], in0=oh[:, a], in1=pit, op=ALU.mult))
                S(lambda h, a=a: h.tensor_reduce(out=slf[:, a, :], in_=oh[:, a], axis=AX.X, op=ALU.add))
            S(lambda h: h.tensor_copy(out=sli, in_=slf))
            S(lambda h: h.tensor_single_scalar(out=sadr_i[:, 0:2 * NTT], in_=sli.rearrange("p a i -> p (a i)"), scalar=7, op=ALU.arith_shift_right))
            S(lambda h: h.tensor_single_scalar(out=sadr_i[:, 2 * NTT:4 * NTT], in_=sli.rearrange("p a i -> p (a i)"), scalar=127, op=ALU.bitwise_and))
            S(lambda h: h.tensor_copy(out=sadr_f, in_=sadr_i))
            S(lambda h: h.scalar_tensor_tensor(out=sadr_f[:, 0:2 * NTT], in0=sadr_f[:, 2 * NTT:4 * NTT], scalar=float(NU * 4), in1=sadr_f[:, 0:2 * NTT], op0=ALU.mult, op1=ALU.add))
            S(lambda h: h.tensor_copy(out=sadr.rearrange("p a i -> p (a i)"), in_=sadr_f[:, 0:2 * NTT]))
            S(lambda h: h.tensor_tensor(out=cmpb, in0=cui.unsqueeze(1).to_broadcast([128, NU, 16]),
                                        in1=cn[:, CN_IOTA:CN_IOTA + NU].unsqueeze(2).to_broadcast([128, NU, 16]), op=ALU.is_le))
            S(lambda h: h.tensor_reduce(out=euf[:, 0:NU], in_=cmpb, axis=AX.X, op=ALU.add))
            S(lambda h: h.tensor_scalar(out=euf[:, 0:NU], in0=euf[:, 0:NU], scalar1=15.0, scalar2=128.0, op0=ALU.min, op1=ALU.mult))
            S(lambda h: h.tensor_scalar(out=euf[:, 0:NU], in0=euf[:, 0:NU], scalar1=cn[:, CN_PIO:CN_PIO + 1], scalar2=None, op0=ALU.add))
            S(lambda h: h.tensor_copy(out=wix[:, 0:NU], in_=euf[:, 0:NU]))
            for a in range(2):
                for i in range(NTT):
                    P.op("pool", lambda h, a=a, i=i: h.indirect_dma_start(
                        out=tokslot_d, out_offset=bass.IndirectOffsetOnAxis(ap=sadr[:, a, i:i + 1].bitcast(U32), axis=0),
                        in_=tokid[:, i:i + 1], in_offset=None), reads=["rt", "tokid", "ts0"], writes=["ts.%d.%d" % (a, i)], dma=True)
            tskeys = ["ts.%d.%d" % (a, i) for a in range(2) for i in range(NTT)]
            P.op("sp", lambda h: h.dma_start(out=uidx, in_=tokslot_d.rearrange("(p n) o -> p (n o)", p=128)), reads=tskeys + ["ts0"], writes=["uidx"], dma=True)
            P.barrier()

            xgT = [carve_at(bufA_off + q * 2048, 2048, BF16, "p (c n) -> p c n", c=8) for q in range(2)]
            xg = [carve_at(bufA_off + 4096 + q * 512, 512, BF16) for q in range(4)] + [carve(512, BF16) for q in range(4)]
            yst = [carve_at(bufA_off + 6144 + q * 1024, 1024) for q in range(2)]
            H3keys = ["keep:H3.%d" % i for i in range(NTT)]

            def pre_tok(u):
                for r in range(4):
                    col = u * 4 + r
                    q = (u % 2) * 4 + r
                    P.op("pool", lambda h, q=q, col=col: h.indirect_dma_start(
                        out=xg[q], out_offset=None, in_=h3_d,
                        in_offset=bass.IndirectOffsetOnAxis(ap=uidx[:, col:col + 1].bitcast(U32), axis=0)),
                        reads=["uidx"] + H3keys, writes=["xg%d" % q], dma=True)

            def tr(u):
                b = u % 2
                for r in range(4):
                    q = b * 4 + r
                    norm_b(xg[q], "xg%d" % q, 3, xgT[b], "xgT%d.%d" % (b, r), r)

            def pre_w(u):
                b = u % 2
                for (wl, dst, nm, cn_) in ((wgb_d, wg[b], "wg%d" % b, "g"), (wub_d, wu[b], "wu%d" % b, "u")):
                    for hh in range(2):
                        P.op("pool", lambda h, wl=wl, dst=dst, hh=hh: h.indirect_dma_start(
                            out=dst[:, 4 * hh:4 * hh + 4, :].rearrange("p c n -> p (c n)"), out_offset=None, in_=wl[hh],
                            in_offset=bass.IndirectOffsetOnAxis(ap=wix[:, u:u + 1].bitcast(U32), axis=0)),
                            reads=["rt"] + cast_keys(cn_, hh), writes=[nm + ".%d" % hh], dma=True)

            def pre_d(u):
                b = u % 2
                for hh in range(2):
                    P.op("pool", lambda h, hh=hh: h.indirect_dma_start(
                        out=wd[b][:, 2 * hh:2 * hh + 2, :].rearrange("p c n -> p (c n)"), out_offset=None, in_=wdb_d[hh],
                        in_offset=bass.IndirectOffsetOnAxis(ap=wix[:, u:u + 1].bitcast(U32), axis=0)),
                        reads=["rt"] + cast_keys("d", hh), writes=["wd%d.%d" % (b, hh)], dma=True)

            def gate_up(u):
                b = u % 2
                hb = hid[b]
                rdx = ["xgT%d.%d" % (b, r) for r in range(4)]
                for f in range(4):
                    bg_, bu_ = Bank("f"), Bank("f")
                    for (bk, w, wk) in ((bg_, wg[b], "wg%d" % b), (bu_, wu[b], "wu%d" % b)):
                        for c in range(8):
                            P.op("pe", lambda h, c=c, f=f, bk=bk, w=w: h.matmul(bk.t[:, :], lhsT=w[:, c, f * 128:(f + 1) * 128], rhs=xgT[b][:, c, :],
                                                                                start=(c == 0), stop=(c == 7)), reads=rdx + [wk + ".%d" % (c // 4)], writes=[bk.k()])
                    sg = sgb[f % 2]
                    P.op("act", lambda h, sg=sg, bg_=bg_: h.activation(out=sg, in_=bg_.t[:, :], func=AF.Silu), reads=[bg_.k()], writes=["sgb%d" % (f % 2)])
                    P.op("dve", lambda h, f=f, sg=sg, bu_=bu_: h.tensor_tensor(out=hb[:, f, :], in0=bu_.t[:, :], in1=sg, op=ALU.mult),
                         reads=[bu_.k(), "sgb%d" % (f % 2)], writes=["hid%d.%d" % (b, f)])

            def down(u):
                b = u % 2
                hb = hid[b]
                for tt in range(4):
                    ys = yst[tt % 2]
                    yk = "yst%d" % (tt % 2)
                    for n2 in range(2):
                        bk = Bank("f")
                        for f in range(4):
                            P.op("pe", lambda h, f=f, tt=tt, n2=n2, bk=bk: h.matmul(bk.t[:, :], lhsT=hb[:, f, tt * 128:(tt + 1) * 128],
                                                                                   rhs=wd[b][:, f, n2 * 512:(n2 + 1) * 512], start=(f == 0), stop=(f == 3)),
                                 reads=["hid%d.%d" % (b, f), "wd%d.%d" % (b, f // 2)], writes=[bk.k()])
                        if n2 == 0:
                            P.op("act", lambda h, ys=ys, bk=bk: h.activation(out=ys[:, 0:512], in_=bk.t[:, :], func=AF.Copy), reads=[bk.k()], writes=[yk + ".0"])
                        else:
                            P.op("dve", lambda h, ys=ys, bk=bk: h.tensor_copy(out=ys[:, 512:1024], in_=bk.t[:, :]), reads=[bk.k()], writes=[yk + ".1"])
                    row0 = (u * 4 + tt) * 128
                    P.op("sp", lambda h, ys=ys, row0=row0: h.dma_start(out=y_d[row0:row0 + 128, :], in_=ys), reads=[yk + ".0", yk + ".1"],
                         writes=["Y.%d" % (u * 4 + tt)], dma=True)

            pre_tok(0)
            pre_tok(1)
            pre_w(0)
            pre_d(0)
            tr(0)
            for u in range(NU + 1):
                if u + 2 < NU:
                    pre_tok(u + 2)
                if u + 1 < NU:
                    tr(u + 1)
                    pre_w(u + 1)
                if u < NU:
                    gate_up(u)
                if u >= 1:
                    down(u - 1)
                if u + 1 < NU:
                    pre_d(u + 1)
            P.barrier()
            yg = [carve_at(bufA_off + q * 1024, 1024) for q in range(4)]
            x2s = [carve(1024) for _ in range(3)]
            fin = rows[:, 0:1024]
            for i in range(NTT):
                xb = x2s[i % 3]
                xk = "x2s%d" % (i % 3)
                P.op("sp", lambda h, i=i, xb=xb: h.dma_start(out=xb, in_=x2_d[i * 128:(i + 1) * 128, :]), reads=["keep:X2.%d" % i], writes=[xk], dma=True)
                for a in range(2):
                    q = (2 * i + a) % 4
                    P.op("pool", lambda h, a=a, i=i, q=q: h.indirect_dma_start(
                        out=yg[q], out_offset=None, in_=y_d,
                        in_offset=bass.IndirectOffsetOnAxis(ap=sli[:, a, i:i + 1].bitcast(U32), axis=0)),
                        writes=["yg%d" % q], dma=True)
                    wa = (w1, w2)[a]
                    P.op("dve", lambda h, i=i, q=q, wa=wa, xb=xb: h.scalar_tensor_tensor(out=xb, in0=yg[q], scalar=wa[:, i:i + 1], in1=xb,
                                                                                        op0=ALU.mult, op1=ALU.add), reads=["yg%d" % q, xk], writes=[xk])
                stt = rstd3[:, i, :]
                sk_ = "st.%d" % i
                P.op("act", lambda h, i=i, stt=stt, xb=xb: h.activation(out=xn2[i % 2], in_=xb, func=AF.Square, accum_out=stt[:, 0:1]),
                     reads=[xk], writes=["xn%d" % (i % 2), sk_])
                P.op("act", lambda h, stt=stt: h.activation(out=stt[:, 1:2], in_=stt[:, 0:1], func=AF.Ln, scale=1.0 / D, bias=eps_ap), reads=[sk_, "eps"], writes=[sk_])
                P.op("act", lambda h, stt=stt: h.activation(out=stt[:, 1:2], in_=stt[:, 1:2], func=AF.Exp, scale=-0.5), reads=[sk_], writes=[sk_])
                P.op("dve", lambda h, stt=stt, xb=xb: h.scalar_tensor_tensor(out=xb, in0=xb, scalar=stt[:, 1:2], in1=fin, op0=ALU.mult, op1=ALU.mult),
                     reads=[xk, sk_, "rows"], writes=[xk])
                P.op("sp", lambda h, i=i, xb=xb: h.dma_start(out=out_d[i * 128:(i + 1) * 128, :], in_=xb),
                     reads=[xk], writes=["out.%d" % i], dma=True)
            P.barrier()

        stopped = False
        for hf_ in range(nhalves):
            if do_half(hf_):
                stopped = True
                break
        if not stopped and nhalves == 2:
            moe_global()
        P.emit()
    return nc


_CACHE = {}


def _host_consts():
    cn = np.zeros((128, CN_N), np.float32)
    cn[:, CN_ID:CN_ID + 128] = np.eye(128, dtype=np.float32)
    cn[:, CN_ONE:CN_ONE + 128] = 1.0
    s = np.arange(64)[:, None]
    t = np.arange(64)[None, :]
    cn[0:64, CN_MASK:CN_MASK + 64] = (s <= t).astype(np.float32)
    rm = np.ones(512, np.float32)
    rm[::64] = 0.0
    cn[:, CN_RM:CN_RM + 512] = rm[None, :]
    kk = np.arange(128)[:, None]
    mm = np.arange(128)[None, :]
    cn[:, CN_U:CN_U + 128] = (kk < mm).astype(np.float32)
    cn[:, CN_IOTA:CN_IOTA + 32] = np.arange(32, dtype=np.float32)[None, :]
    cn[:, CN_PIO] = np.arange(128, dtype=np.float32)
    return cn


def _col(v, nchunk):
    return np.ascontiguousarray(np.asarray(v, np.float32).reshape(nchunk, 128).T)


def _rowlay(name, w, nchunk):
    E, R, n = w.shape
    t = w.reshape(E, nchunk, 128, n).transpose(0, 2, 1, 3).reshape(E * 128, nchunk * n)
    hlf = nchunk * n // 2
    return {name + "0": np.ascontiguousarray(t[:, :hlf]), name + "1": np.ascontiguousarray(t[:, hlf:])}


def make_in_maps(inputs):
    f = lambda k: np.asarray(inputs[k], np.float32)
    pcols = np.zeros((128, PC_N), np.float32)
    for gi, k in enumerate(["mix_norm", "xattn_norm", "mem_norm", "ffn_norm"]):
        pcols[:, PC_G + gi * 8:PC_G + gi * 8 + 8] = _col(f(k)[0], 8)
    cw = f("conv_w")[0]
    for cch in range(4):
        for jj in range(3):
            pcols[:, PC_CONV + cch * 3 + jj] = cw[jj, cch * 128:(cch + 1) * 128]
    lbr = f("hgrn_lb")
    for r in range(2):
        pcols[:, PC_LB + r * 4:PC_LB + r * 4 + 4] = _col(lbr[r], 4)
    pcols[:, PC_HN:PC_HN + 4] = _col(f("hgrn_norm")[0], 4)
    wrc = np.concatenate([f("w_group")[0], f("w_expert")[0]], axis=1)
    wr = np.ascontiguousarray(wrc.reshape(8, 128, 20).transpose(1, 0, 2).reshape(128, 160))
    rows = np.concatenate([f("final_norm").reshape(-1), f("b_group")[0], f("b_expert")[0]])[None, :].astype(np.float32)
    consts = _host_consts()
    x = f("x")
    mem = f("mem")
    shared = dict(
        w_in=np.ascontiguousarray(f("w_in")[0]), w_out=np.ascontiguousarray(f("w_out")[0]),
        w_q=np.ascontiguousarray(f("w_q")[0]), w_kv=np.ascontiguousarray(f("w_kv")[0]), w_o=np.ascontiguousarray(f("w_o")[0]),
        **_rowlay("wgl", f("w_gate")[0], 8), **_rowlay("wul", f("w_up")[0], 8), **_rowlay("wdl", f("w_down")[0], 4),
        pcols=pcols, wr=wr, rows=np.ascontiguousarray(rows), consts=consts)
    maps = []
    for c in range(NCORES):
        m = dict(shared)
        m["x"] = np.ascontiguousarray(x[2 * c:2 * c + 2].reshape(2 * SEQ, D))
        m["mem"] = np.ascontiguousarray(mem[2 * c:2 * c + 2].reshape(512, D))
        maps.append(m)
    return maps


def kernel(**inputs):
    if "nc" not in _CACHE:
        _CACHE["nc"] = build_program()
    nc = _CACHE["nc"]
    maps = make_in_maps(inputs)
    res = run_bass_kernel_spmd(nc, maps, core_ids=list(range(NCORES)))
    outs = [np.asarray(r["out"], np.float32).reshape(2, SEQ, D) for r in res.results]
    return np.concatenate(outs, axis=0)
```
